# Optimizing a Trainium2 kernel written in Bass

```python
import jax, jax.numpy as jnp
from jax import lax
import numpy as np

D_MODEL = 1024
BATCH = 8
SEQ = 4096
DEPTH = 2

GRID_W = 64
CTX_LEN = 256
ROPE_BASE = 10000.0
EPS = 1e-6
N_MOD = 6

MLA_HEADS = 8
MLA_NOPE = 64
MLA_ROPE = 32
MLA_QK = MLA_NOPE + MLA_ROPE
MLA_V = 64
MLA_Q_RANK = 384
MLA_KV_RANK = 256
Q_BLOCK = 128

SWA_HEADS = 8
SWA_KV_HEADS = 2
SWA_GROUP = SWA_HEADS // SWA_KV_HEADS
SWA_DIM = 64
WINDOW = 128
WIN_BLOCK = 128
N_NEIGH = 2 * (WINDOW // WIN_BLOCK) + 1

D_MIX = MLA_HEADS * MLA_V + SWA_HEADS * SWA_DIM
Q_COLS = MLA_Q_RANK + SWA_HEADS * SWA_DIM
KV_COLS = MLA_KV_RANK + MLA_ROPE + 2 * SWA_KV_HEADS * SWA_DIM
D_IN = Q_COLS + KV_COLS

PEER_HEADS = 8
PEER_NKEYS = 128
PEER_EXPERTS = PEER_NKEYS * PEER_NKEYS
PEER_DQ = 256
PEER_DHALF = PEER_DQ // 2
PEER_TOPK = 16
PEER_CHUNK = 128

kernel_name = "hymba_mla_swa_peer_dit_block"


def rms_norm(x, g):
    x32 = x.astype(jnp.float32)
    y = x32 * lax.rsqrt(jnp.mean(x32 * x32, axis=-1, keepdims=True) + EPS)
    return (y * g.astype(jnp.float32)).astype(x.dtype)


def modulate(x, g, shift, scale):
    return rms_norm(x, g) * (1 + scale) + shift


def axial_cos_sin(rows, cols, rot_dim):
    quarter = rot_dim // 4
    inv = ROPE_BASE ** (-jnp.arange(quarter, dtype=jnp.float32) / quarter)
    ar = rows[:, None] * inv
    ac = cols[:, None] * inv
    ang = jnp.concatenate([ar, ar, ac, ac], axis=-1)
    return jnp.cos(ang), jnp.sin(ang)


def apply_rope(x, cs):
    cos, sin = cs
    x32 = x.astype(jnp.float32)
    a, b, c, d = jnp.split(x32, 4, axis=-1)
    rot = jnp.concatenate([-b, a, -d, c], axis=-1)
    return (x32 * cos[:, None, :] + rot * sin[:, None, :]).astype(x.dtype)


def rope_tail(x, cs):
    return jnp.concatenate([x[..., :MLA_NOPE], apply_rope(x[..., MLA_NOPE:], cs)], axis=-1)


def split_kv_cols(p):
    b0 = MLA_KV_RANK
    b1 = b0 + MLA_ROPE
    b2 = b1 + SWA_KV_HEADS * SWA_DIM
    return p[..., :b0], p[..., b0:b1], p[..., b1:b2], p[..., b2:]


def mla_queries(qa, qa_g, wuq, qn_g, cs):
    B, T = qa.shape[:2]
    q = (rms_norm(qa, qa_g) @ wuq).reshape(B, T, MLA_HEADS, MLA_QK)
    q = rms_norm(q, qn_g)
    return q if cs is None else rope_tail(q, cs)


def mla_keys_values(kva, kr, kva_g, wukv, kn_g, cs):
    B, T = kva.shape[:2]
    kv = (rms_norm(kva, kva_g) @ wukv).reshape(B, T, MLA_HEADS, MLA_NOPE + MLA_V)
    k_rope = jnp.broadcast_to(kr[:, :, None, :], (B, T, MLA_HEADS, MLA_ROPE))
    k = rms_norm(jnp.concatenate([kv[..., :MLA_NOPE], k_rope], axis=-1), kn_g)
    if cs is not None:
        k = rope_tail(k, cs)
    return k, kv[..., MLA_NOPE:]


def mla_latent_attend(q, k, v, kc, vc):
    B, S = q.shape[:2]
    nb = S // Q_BLOCK
    k_all = jnp.concatenate([kc, k], axis=1)
    v_all = jnp.concatenate([vc, v], axis=1)
    qb = q.reshape(B, nb, Q_BLOCK, MLA_HEADS, MLA_QK).transpose(1, 0, 2, 3, 4)
    scale = MLA_QK ** -0.5

    def block(qblk):
        s = jnp.einsum('bqhd,bkhd->bhqk', qblk, k_all).astype(jnp.float32) * scale
        p = jax.nn.softmax(s, axis=-1).astype(v_all.dtype)
        return jnp.einsum('bhqk,bkhd->bqhd', p, v_all)

    o = lax.map(block, qb)
    return o.transpose(1, 0, 2, 3, 4).reshape(B, S, MLA_HEADS * MLA_V)


def mla_ctx_attend(q, kc, vc):
    B, C = q.shape[:2]
    s = jnp.einsum('bqhd,bkhd->bhqk', q, kc).astype(jnp.float32) * (MLA_QK ** -0.5)
    p = jax.nn.softmax(s, axis=-1).astype(vc.dtype)
    return jnp.einsum('bhqk,bkhd->bqhd', p, vc).reshape(B, C, MLA_HEADS * MLA_V)


def swa_queries(sq, qn_g, cs):
    B, T = sq.shape[:2]
    q = rms_norm(sq.reshape(B, T, SWA_HEADS, SWA_DIM), qn_g)
    return q if cs is None else apply_rope(q, cs)


def swa_keys_values(sk, sv, kn_g, cs):
    B, T = sk.shape[:2]
    k = rms_norm(sk.reshape(B, T, SWA_KV_HEADS, SWA_DIM), kn_g)
    if cs is not None:
        k = apply_rope(k, cs)
    return k, sv.reshape(B, T, SWA_KV_HEADS, SWA_DIM)


def window_attend(q, k, v, kc, vc, sink):
    B, S = q.shape[:2]
    nb = S // WIN_BLOCK
    nk = N_NEIGH * WIN_BLOCK
    C = kc.shape[1]
    scale = SWA_DIM ** -0.5
    qb = q.reshape(B, nb, WIN_BLOCK, SWA_KV_HEADS, SWA_GROUP, SWA_DIM).transpose(1, 0, 2, 3, 4, 5)
    pad = ((0, 0), (WINDOW, WINDOW), (0, 0), (0, 0))
    kp, vp = jnp.pad(k, pad), jnp.pad(v, pad)

    def band(xp):
        parts = [xp[:, i * WIN_BLOCK: i * WIN_BLOCK + S].reshape(B, nb, WIN_BLOCK, SWA_KV_HEADS, SWA_DIM)
                 for i in range(N_NEIGH)]
        return jnp.concatenate(parts, axis=2).transpose(1, 0, 2, 3, 4)

    kb, vb = band(kp), band(vp)
    qi = jnp.arange(WIN_BLOCK)[:, None]
    kj = jnp.arange(nk)[None, :]
    k_abs = jnp.arange(nb)[:, None, None] * WIN_BLOCK - WINDOW + kj
    mask = (jnp.abs(kj - WINDOW - qi) <= WINDOW) & (k_abs >= 0) & (k_abs < S)
    sink_col = sink.reshape(SWA_KV_HEADS, SWA_GROUP, 1, 1).astype(jnp.float32)

    def block(args):
        qblk, kblk, vblk, m = args
        s_ctx = jnp.einsum('bqgrd,bcgd->bgrqc', qblk, kc).astype(jnp.float32) * scale
        s_loc = jnp.einsum('bqgrd,bkgd->bgrqk', qblk, kblk).astype(jnp.float32) * scale
        s_loc = jnp.where(m, s_loc, -jnp.inf)
        s_sink = jnp.broadcast_to(sink_col, s_ctx.shape[:-1] + (1,))
        p = jax.nn.softmax(jnp.concatenate([s_ctx, s_loc, s_sink], axis=-1), axis=-1).astype(vblk.dtype)
        return (jnp.einsum('bgrqc,bcgd->bqgrd', p[..., :C], vc)
                + jnp.einsum('bgrqk,bkgd->bqgrd', p[..., C:C + nk], vblk))

    o = lax.map(block, (qb, kb, vb, mask))
    return o.transpose(1, 0, 2, 3, 4, 5).reshape(B, S, SWA_HEADS * SWA_DIM)


def swa_ctx_attend(q, kc, vc, sink):
    B, C = q.shape[:2]
    qg = q.reshape(B, C, SWA_KV_HEADS, SWA_GROUP, SWA_DIM)
    s = jnp.einsum('bqgrd,bkgd->bgrqk', qg, kc).astype(jnp.float32) * (SWA_DIM ** -0.5)
    s_sink = jnp.broadcast_to(sink.reshape(SWA_KV_HEADS, SWA_GROUP, 1, 1).astype(jnp.float32),
                              s.shape[:-1] + (1,))
    p = jax.nn.softmax(jnp.concatenate([s, s_sink], axis=-1), axis=-1)[..., :C].astype(vc.dtype)
    return jnp.einsum('bgrqk,bkgd->bqgrd', p, vc).reshape(B, C, SWA_HEADS * SWA_DIM)


def peer(h, wq, sub_keys, u, v):
    shp = h.shape
    chunks = h.reshape(-1, PEER_CHUNK, shp[-1])

    def chunk(xc):
        q = (xc @ wq).reshape(PEER_CHUNK, PEER_HEADS, 2, PEER_DHALF)
        s = jnp.einsum('thpd,hpkd->thpk', q, sub_keys).astype(jnp.float32)
        sv, si = lax.top_k(s, PEER_TOPK)
        cand = sv[:, :, 0, :, None] + sv[:, :, 1, None, :]
        cv, ci = lax.top_k(cand.reshape(PEER_CHUNK, PEER_HEADS, PEER_TOPK * PEER_TOPK), PEER_TOPK)
        i1 = jnp.take_along_axis(si[:, :, 0], ci // PEER_TOPK, axis=-1)
        i2 = jnp.take_along_axis(si[:, :, 1], ci % PEER_TOPK, axis=-1)
        idx = i1 * PEER_NKEYS + i2
        g = jax.nn.softmax(cv, axis=-1)
        u_sel = u[idx]
        v_sel = v[idx]
        a = jax.nn.gelu(jnp.einsum('td,thkd->thk', xc, u_sel).astype(jnp.float32), approximate=False)
        return jnp.einsum('thk,thkd->td', (g * a).astype(xc.dtype), v_sel)

    return lax.map(chunk, chunks).reshape(shp)


def setup_inputs(seed: int = 0) -> dict:
    key = jax.random.key(seed)
    ks = jax.random.split(key, 24)
    f32 = jnp.float32

    def nrm(k, shape, s):
        return s * jax.random.normal(k, shape, f32)

    L, D = DEPTH, D_MODEL
    return {
        "x": nrm(ks[0], (BATCH, SEQ, D), 1.0),
        "c": nrm(ks[1], (BATCH, D), 1.0),
        "ctx": nrm(ks[2], (BATCH, CTX_LEN, D), 1.0),
        "c_ctx": nrm(ks[3], (D,), 1.0),
        "ada_w": nrm(ks[4], (L, D, N_MOD * D), D ** -0.5),
        "ada_b": nrm(ks[5], (L, N_MOD * D), 0.01),
        "norm1_g": 1.0 + nrm(ks[6], (L, D), 0.02),
        "norm2_g": 1.0 + nrm(ks[7], (L, D), 0.02),
        "w_in": nrm(ks[8], (L, D, D_IN), D ** -0.5),
        "mla_qa_g": 1.0 + nrm(ks[9], (L, MLA_Q_RANK), 0.02),
        "mla_wuq": nrm(ks[10], (L, MLA_Q_RANK, MLA_HEADS * MLA_QK), MLA_Q_RANK ** -0.5),
        "mla_kva_g": 1.0 + nrm(ks[11], (L, MLA_KV_RANK), 0.02),
        "mla_wukv": nrm(ks[12], (L, MLA_KV_RANK, MLA_HEADS * (MLA_NOPE + MLA_V)), MLA_KV_RANK ** -0.5),
        "mla_qn_g": 1.0 + nrm(ks[13], (L, MLA_QK), 0.02),
        "mla_kn_g": 1.0 + nrm(ks[14], (L, MLA_QK), 0.02),
        "swa_qn_g": 1.0 + nrm(ks[15], (L, SWA_DIM), 0.02),
        "swa_kn_g": 1.0 + nrm(ks[16], (L, SWA_DIM), 0.02),
        "swa_sink": nrm(ks[17], (L, SWA_HEADS), 0.5),
        "w_out": nrm(ks[18], (L, D_MIX, D), D_MIX ** -0.5),
        "peer_wq": nrm(ks[19], (L, D, PEER_HEADS * PEER_DQ), D ** -0.5),
        "peer_keys": nrm(ks[20], (L, PEER_HEADS, 2, PEER_NKEYS, PEER_DHALF), PEER_DHALF ** -0.5),
        "peer_u": nrm(ks[21], (L, PEER_EXPERTS, D), D ** -0.5),
        "peer_v": nrm(ks[22], (L, PEER_EXPERTS, D), PEER_HEADS ** -0.5),
    }


def reference(x, c, ctx, c_ctx, ada_w, ada_b, norm1_g, norm2_g, w_in, mla_qa_g, mla_wuq,
              mla_kva_g, mla_wukv, mla_qn_g, mla_kn_g, swa_qn_g, swa_kn_g, swa_sink, w_out,
              peer_wq, peer_keys, peer_u, peer_v):
    B, S, D = x.shape
    ROWS = S // GRID_W
    rows = jnp.repeat(jnp.arange(ROWS, dtype=jnp.float32), GRID_W)
    cols = jnp.tile(jnp.arange(GRID_W, dtype=jnp.float32), ROWS)
    cs_mla = axial_cos_sin(rows, cols, MLA_ROPE)
    cs_swa = axial_cos_sin(rows, cols, SWA_DIM)
    s_c = jax.nn.silu(c)
    s_cctx = jax.nn.silu(c_ctx)

    h_lat, h_ctx = x, ctx
    for l in range(DEPTH):
        last = l == DEPTH - 1
        mod = (s_c @ ada_w[l] + ada_b[l]).reshape(B, N_MOD, D)
        sh1, sc1, g1, sh2, sc2, g2 = [mod[:, i][:, None, :] for i in range(N_MOD)]
        mod_c = (s_cctx @ ada_w[l] + ada_b[l]).reshape(N_MOD, D)

        a_lat = modulate(h_lat, norm1_g[l], sh1, sc1)
        a_ctx = modulate(h_ctx, norm1_g[l], mod_c[0], mod_c[1])
        p_lat = a_lat @ w_in[l]
        p_ctx = a_ctx @ (w_in[l][:, Q_COLS:] if last else w_in[l])

        qa, sq = p_lat[..., :MLA_Q_RANK], p_lat[..., MLA_Q_RANK:Q_COLS]
        kva, kr, sk, sv = split_kv_cols(p_lat[..., Q_COLS:])
        kva_c, kr_c, sk_c, sv_c = split_kv_cols(p_ctx[..., -KV_COLS:])

        q_m = mla_queries(qa, mla_qa_g[l], mla_wuq[l], mla_qn_g[l], cs_mla)
        k_m, v_m = mla_keys_values(kva, kr, mla_kva_g[l], mla_wukv[l], mla_kn_g[l], cs_mla)
        kc_m, vc_m = mla_keys_values(kva_c, kr_c, mla_kva_g[l], mla_wukv[l], mla_kn_g[l], None)
        o_mla = mla_latent_attend(q_m, k_m, v_m, kc_m, vc_m)

        q_s = swa_queries(sq, swa_qn_g[l], cs_swa)
        k_s, v_s = swa_keys_values(sk, sv, swa_kn_g[l], cs_swa)
        kc_s, vc_s = swa_keys_values(sk_c, sv_c, swa_kn_g[l], None)
        o_swa = window_attend(q_s, k_s, v_s, kc_s, vc_s, swa_sink[l])

        h_lat = h_lat + g1 * (jnp.concatenate([o_mla, o_swa], axis=-1) @ w_out[l])

        if not last:
            qa_c, sq_c = p_ctx[..., :MLA_Q_RANK], p_ctx[..., MLA_Q_RANK:Q_COLS]
            qc_m = mla_queries(qa_c, mla_qa_g[l], mla_wuq[l], mla_qn_g[l], None)
            qc_s = swa_queries(sq_c, swa_qn_g[l], None)
            oc = jnp.concatenate([mla_ctx_attend(qc_m, kc_m, vc_m),
                                  swa_ctx_attend(qc_s, kc_s, vc_s, swa_sink[l])], axis=-1)
            h_ctx = h_ctx + mod_c[2] * (oc @ w_out[l])

        b_lat = modulate(h_lat, norm2_g[l], sh2, sc2)
        h_lat = h_lat + g2 * peer(b_lat, peer_wq[l], peer_keys[l], peer_u[l], peer_v[l])
        if not last:
            b_ctx = modulate(h_ctx, norm2_g[l], mod_c[3], mod_c[4])
            h_ctx = h_ctx + mod_c[5] * peer(b_ctx, peer_wq[l], peer_keys[l], peer_u[l], peer_v[l])

    return h_lat
```

```python
import contextlib
import numpy as np
import concourse.bass as bass
import concourse.mybir as mybir
from concourse.bass_utils import run_bass_kernel_spmd

F32, BF16, U32 = mybir.dt.float32, mybir.dt.bfloat16, mybir.dt.uint32
AF = mybir.ActivationFunctionType
ALU = mybir.AluOpType
AX = mybir.AxisListType

L = 2
D = 1024
SEQ = 4096
CTX = 256
T = SEQ + CTX
NT = T // 128
EPS = 1e-6
DIN = 1440
NEG = -1.0e30


class Buf:
    __slots__ = ("w", "r")

    def __init__(self):
        self.w = None
        self.r = {}


class Sched:
    def __init__(self, nc, es):
        self.nc = nc
        self.eng = {"pe": nc.tensor, "act": nc.scalar, "dve": nc.vector, "pool": nc.gpsimd, "sp": nc.sync}
        self.sem = {}
        self.cnt = {}
        for e in ("pe", "act", "dve", "pool"):
            self.sem[e] = es.enter_context(nc.semaphore("s_" + e))
            self.cnt[e] = 0
        self.R = 32
        self.dsem = [es.enter_context(nc.semaphore("d%d" % i)) for i in range(self.R)]
        self.dlast = [None] * self.R
        self.dn = 0
        self.seen = {e: {} for e in self.eng}
        self.pe_pending = False
        self.rec = None
        self.bgsems = [es.enter_context(nc.semaphore("bg%d" % i)) for i in range(8)]
        self.bgn = 0

    def _wait(self, e, tok):
        key, sem, val = tok
        if e == "pe" and key == "pe":
            return
        if self.seen[e].get(key, 0) >= val:
            return
        self.eng[e].wait_ge(sem, val)
        self.seen[e][key] = val

    def _deps(self, e, reads, writes):
        for b in reads:
            if b.w is not None:
                self._wait(e, b.w)
        for b in writes:
            if b.w is not None:
                self._wait(e, b.w)
            for t in list(b.r.values()):
                self._wait(e, t)

    def _commit(self, tok, reads, writes):
        for b in reads:
            b.r[tok[0]] = tok
        for b in writes:
            b.w = tok
            b.r = {}

    def op(self, *a, **k):
        if self.rec is not None:
            self.rec.append(lambda: self._op(*a, **k))
        else:
            self._op(*a, **k)

    def dma(self, *a, **k):
        if self.rec is not None:
            self.rec.append(lambda: self._dma(*a, **k))
        else:
            self._dma(*a, **k)

    def _op(self, e, fn, R=(), W=(), inc=True):
        self._deps(e, R, W)
        inst = fn(self.eng[e])
        if inc:
            self.cnt[e] += 1
            inst.then_inc(self.sem[e], 1)
            tok = (e, self.sem[e], self.cnt[e])
        else:
            assert e == "pe"
            tok = (e, self.sem[e], self.cnt[e] + 1)
        self._commit(tok, R, W)

    def dma_untracked(self, out, in_, q):
        k = self.bgn % len(self.bgsems)
        if self.bgn >= len(self.bgsems):
            self.eng[q].wait_ge(self.bgsems[k], 16 * (self.bgn // len(self.bgsems)))
        inst = self.eng[q].dma_start(out=out, in_=in_)
        self.bgn += 1
        inst.then_inc(self.bgsems[k], 16)

    def _dma(self, out, in_, R=(), W=(), q="sp"):
        i = self.dn % self.R
        if self.dlast[i] is not None:
            self._wait(q, self.dlast[i])
        self._deps(q, R, W)
        inst = self.eng[q].dma_start(out=out, in_=in_)
        val = 16 * (self.dn // self.R + 1)
        inst.then_inc(self.dsem[i], 16)
        tok = ("d%d" % i, self.dsem[i], val)
        self.dlast[i] = tok
        self.dn += 1
        self._commit(tok, R, W)

    def barrier(self, engines=("pe", "act", "dve", "pool", "sp"), bg=False):
        toks = []
        if bg and self.bgn > 0:
            nb_ = len(self.bgsems)
            for k in range(nb_):
                cnt_ = len([x for x in range(self.bgn) if x % nb_ == k])
                if cnt_:
                    toks.append(("bg%d" % k, self.bgsems[k], 16 * cnt_))
        for e in ("pe", "act", "dve", "pool"):
            if self.cnt[e] > 0:
                toks.append((e, self.sem[e], self.cnt[e]))
        for t in self.dlast:
            if t is not None:
                toks.append(t)
        for e in engines:
            for t in toks:
                if not (e == t[0]):
                    self._wait(e, t)
                elif e != "pe":
                    self._wait(e, t)


class TT:
    def __init__(self, t):
        self.t = t
        self.b = Buf()

    def __getitem__(self, k):
        return self.t[k]


class _Stop(Exception):
    pass


_LAST = {}


def build_dbg(debug, stop):
    try:
        return build(debug, stop)
    except _Stop:
        _LAST["es"].close()
        return _LAST["nc"]


def build(debug=None, stop=None):
    nc = bass.Bass("TRN2", target_bir_lowering=False)
    es = contextlib.ExitStack()
    _LAST["nc"] = nc
    _LAST["es"] = es

    def din(name, shape, dt=F32):
        return nc.dram_tensor(name, list(shape), dt, kind="ExternalInput").ap()

    dbg_names = set(debug or [])

    def dscr(name, shape, dt):
        kind = "ExternalOutput" if name in dbg_names else "Internal"
        return nc.dram_tensor(name, list(shape), dt, kind=kind).ap()

    x_d = din("x", [SEQ, D])
    ctx_d = din("ctx", [CTX, D])
    cvec_d = din("cvec", [2, D])
    ada_w = din("ada_w", [L, D, 6 * D])
    ada_b = din("ada_b", [L, 6 * D])
    n1g = din("norm1_g", [L, D])
    n2g = din("norm2_g", [L, D])
    w_in = din("w_in", [L, D, DIN])
    qa_g = din("qa_g_t", [L, 128, 3])
    wuq = din("mla_wuq", [L, 384, 768])
    kva_g = din("kva_g_t", [L, 128, 2])
    wukv = din("mla_wukv", [L, 256, 1024])
    mqn_g = din("mla_qn_g", [L, 96])
    mkn_g = din("mla_kn_g", [L, 96])
    sqn_g = din("swa_qn_g", [L, 64])
    skn_g = din("swa_kn_g", [L, 64])
    sink_d = din("swa_sink", [L, 8])
    w_out = din("w_out", [L, D, D])
    wq_d = din("peer_wq", [L, D, 2048])
    keysT_d = din("keysT", [L, 128, 2048])
    ut_d = din("peer_uT", [L, 128, 128, 1024])
    v_d = din("peer_v", [L, 16384, D])
    ident_d = din("ident", [128, 128])
    iota_d = din("iota", [128, 128])
    tri_d = din("tri", [2, 128, 128])
    sel_d = din("sel", [2, 256])
    csm_d = din("cs_mla", [T, 64])
    css_d = din("cs_swa", [T, 128])
    y_d = nc.dram_tensor("y", [SEQ, D], F32, kind="ExternalOutput").ap()

    h_s = dscr("h_s", [T, D], F32)
    qTm_s = dscr("qTm_s", [8, 96, T], BF16)
    kTm_s = dscr("kTm_s", [8, 96, T], BF16)
    Vm_s = dscr("Vm_s", [T, 520], BF16)
    qTs_s = dscr("qTs_s", [8, 64, T], BF16)
    kTs_s = dscr("kTs_s", [2, 64, T], BF16)
    Vs_s = dscr("Vs_s", [T, 130], BF16)
    om_s = dscr("om_s", [T, D], BF16)
    utb_s = dscr("utb_s", [L, 128, 128, 1024], BF16)
    vb_s = dscr("vb_s", [L, 16384, D], BF16)

    S = Sched(nc, es)

    uid = [0]

    def sb(name, shape, dt, stack=None):
        uid[0] += 1
        return TT((stack or es).enter_context(nc.sbuf_tensor("%s_%d" % (name, uid[0]), list(shape), dt)))

    ps = [TT(es.enter_context(nc.psum_tensor("ps%d" % i, [128, 512], F32))) for i in range(8)]

    def psb(i):
        return ps[i][:].bitcast(BF16)

    ident_f = sb("ident_f", [128, 128], F32)
    ident_b = sb("ident_b", [128, 128], BF16)
    iota_f = sb("iota_f", [128, 128], F32)
    iota_b = sb("iota_b", [128, 128], BF16)
    tri_f = sb("tri_f", [128, 2, 128], F32)
    tri_b = sb("tri_b", [128, 2, 128], BF16)
    sel_f = sb("sel_f", [2, 256], F32)
    epsc = sb("epsc", [128, 1], F32)
    S.dma(ident_f[:], ident_d[:, :], W=[ident_f.b])
    S.dma(iota_f[:], iota_d[:, :], W=[iota_f.b])
    S.dma(tri_f[:], tri_d.rearrange("a p q -> p a q"), W=[tri_f.b])
    S.dma(sel_f[:], sel_d[:, :], W=[sel_f.b])
    S.op("dve", lambda e: e.tensor_copy(ident_b[:], ident_f[:]), R=[ident_f.b], W=[ident_b.b])
    S.op("dve", lambda e: e.tensor_copy(iota_b[:], iota_f[:]), R=[iota_f.b], W=[iota_b.b])
    S.op("dve", lambda e: e.tensor_copy(tri_b[:], tri_f[:]), R=[tri_f.b], W=[tri_b.b])
    S.op("dve", lambda e: e.memset(epsc[:], EPS), W=[epsc.b])

    modT = sb("modT", [128, 4, 8, 2], F32)
    grow = sb("grow", [2, 2 * D], F32)
    sT = sb("sT", [128, 8, 2], F32)
    esink = sb("esink", [128, 8], F32)
    tmp_es = contextlib.ExitStack()
    s2row = sb("s2row", [2, D], F32, tmp_es)

    S.dma(s2row[:], cvec_d[:, :], W=[s2row.b])
    S.op("act", lambda e: e.activation(out=s2row[:], in_=s2row[:], func=AF.Silu), R=[s2row.b], W=[s2row.b])
    for dc in range(8):
        S.op("pe", lambda e, dc=dc: e.transpose(ps[0][:, dc * 2:dc * 2 + 2], s2row[0:2, dc * 128:(dc + 1) * 128], ident_f[0:2, 0:2]),
             R=[s2row.b, ident_f.b], W=[ps[0].b])
    S.op("dve", lambda e: e.tensor_copy(sT[:].rearrange("p a b -> p (a b)"), ps[0][:, 0:16]), R=[ps[0].b], W=[sT.b])
    S.barrier()
    tmp_es.close()

    bg_list = []
    for l in range(L):
        for c in range(0, 128, 8):
            bg_list.append((utb_s[l, c:c + 8].rearrange("c p n -> (c p) n"), ut_d[l, c:c + 8].rearrange("c p n -> (c p) n")))
            bg_list.append((vb_s[l, c * 128:(c + 8) * 128, :], v_d[l, c * 128:(c + 8) * 128, :]))

    def bg_issue(n):
        for _ in range(n):
            if bg_list:
                o_, i_ = bg_list.pop(0)
                S.dma_untracked(o_, i_, "pool")

    def rstd_of(ph, x3, xb, H, Dh, tmp, ssq, rstd):
        tv = tmp[:, 0:H * Dh].rearrange("p (h d) -> p h d", h=H)
        S.op("dve", lambda e: e.tensor_tensor(out=tv, in0=x3, in1=x3, op=ALU.mult), R=[xb], W=[tmp.b])
        S.op("dve", lambda e: e.tensor_reduce(out=ssq[:, 0:H], in_=tv, axis=AX.X, op=ALU.add), R=[tmp.b], W=[ssq.b])
        S.op("act", lambda e: e.activation(out=rstd[:, 0:H], in_=ssq[:, 0:H], func=AF.Sqrt, scale=1.0 / Dh, bias=epsc[:, 0:1]),
             R=[ssq.b, epsc.b], W=[rstd.b])
        S.op("dve", lambda e: e.reciprocal(out=rstd[:, 0:H], in_=rstd[:, 0:H]), R=[rstd.b], W=[rstd.b])

    def rope(x5, xb, cst, cstb, H, q, t1, t2, out5, outb):
        cos4 = cst[:, 0, :].rearrange("p (a b q) -> p a b q", a=2, b=2)
        sin4 = cst[:, 1, :].rearrange("p (a b q) -> p a b q", a=2, b=2)
        n = H * 4 * q
        t1v = t1[:, 0:n].rearrange("p (h a b q) -> p h a b q", h=H, a=2, b=2)
        t2v = t2[:, 0:n].rearrange("p (h a b q) -> p h a b q", h=H, a=2, b=2)
        for b_ in range(2):
            cb = cos4[:, :, b_, :].unsqueeze(1).to_broadcast([128, H, 2, q])
            sbn = sin4[:, :, b_, :].unsqueeze(1).to_broadcast([128, H, 2, q])
            S.op("dve", lambda e, b_=b_, cb=cb: e.tensor_tensor(out=t1v[:, :, :, b_, :], in0=x5[:, :, :, b_, :], in1=cb, op=ALU.mult),
                 R=[xb, cstb], W=[t1.b])
            S.op("dve", lambda e, b_=b_, sbn=sbn: e.tensor_tensor(out=t2v[:, :, :, b_, :], in0=x5[:, :, :, 1 - b_, :], in1=sbn, op=ALU.mult),
                 R=[xb, cstb], W=[t2.b])
        for b_ in range(2):
            S.op("dve", lambda e, b_=b_: e.tensor_tensor(out=out5[:, :, :, b_, :], in0=t1v[:, :, :, b_, :], in1=t2v[:, :, :, b_, :], op=ALU.add),
                 R=[t1.b, t2.b], W=[outb])

    def load_w_bf16(ph, dst2d, src2d, ncols, stg, scale_ap=None, scale_b=None, k=[0]):
        for c0 in range(0, ncols, 2048):
            c1 = min(ncols, c0 + 2048)
            st = stg[k[0] % 2]
            k[0] += 1
            S.dma(st[:, 0:c1 - c0], src2d[:, c0:c1], W=[st.b])
            if scale_ap is None:
                S.op("act", lambda e, st=st, c0=c0, c1=c1: e.activation(out=dst2d[0][:, c0:c1], in_=st[:, 0:c1 - c0], func=AF.Copy),
                     R=[st.b], W=[dst2d[1]])
            else:
                S.op("dve", lambda e, st=st, c0=c0, c1=c1: e.tensor_scalar(out=dst2d[0][:, c0:c1], in0=st[:, 0:c1 - c0], scalar1=scale_ap,
                                                                         scalar2=None, op0=ALU.mult),
                     R=[st.b, scale_b], W=[dst2d[1]])

    def chk(l, phn):
        if stop is not None and stop == (l, phn):
            raise _Stop()

    for l in (range(L) if stop is None else range(stop[0] + 1)):
        last = l == L - 1
        with contextlib.ExitStack() as ph:
            aw = [sb("aw%d" % i, [128, 3072], F32, ph) for i in range(2)]
            modrow = sb("modrow", [2, 6 * D], F32, ph)
            abr = sb("abr", [2, 6 * D], F32, ph)
            ng = sb("ng", [2, 2, D], F32, ph)
            vrow = sb("vrow", [2, 4, D], F32, ph)
            S.dma(abr[:], ada_b[l:l + 1, :].to_broadcast([2, 6 * D]), W=[abr.b])
            S.dma(ng[:, 0, :], n1g[l:l + 1, :].to_broadcast([2, D]), W=[ng.b])
            S.dma(ng[:, 1, :], n2g[l:l + 1, :].to_broadcast([2, D]), W=[ng.b])
            S.dma(esink[:], sink_d[l:l + 1, :].to_broadcast([128, 8]), W=[esink.b])
            S.op("act", lambda e: e.activation(out=esink[:], in_=esink[:], func=AF.Exp), R=[esink.b], W=[esink.b])
            k = 0
            for half in range(2):
                for dc in range(8):
                    a = aw[k % 2]
                    k += 1
                    S.dma(a[:], ada_w[l, dc * 128:(dc + 1) * 128, half * 3072:(half + 1) * 3072], W=[a.b])
                    for cb in range(6):
                        S.op("pe", lambda e, a=a, cb=cb, dc=dc: e.matmul(ps[cb][0:2, :], lhsT=sT[:, dc, :], rhs=a[:, cb * 512:(cb + 1) * 512],
                                                                       start=(dc == 0), stop=(dc == 7)),
                             R=[a.b, sT.b], W=[ps[cb].b])
                for cb in range(6):
                    c0 = half * 3072 + cb * 512
                    S.op("dve", lambda e, cb=cb, c0=c0: e.tensor_tensor(out=modrow[:, c0:c0 + 512], in0=ps[cb][0:2, :], in1=abr[:, c0:c0 + 512], op=ALU.add),
                         R=[ps[cb].b, abr.b], W=[modrow.b])
            for j, (sci, shi) in enumerate(((1, 0), (4, 3))):
                S.op("dve", lambda e, j=j, sci=sci: e.scalar_tensor_tensor(out=vrow[:, 2 * j, :], in0=modrow[:, sci * D:(sci + 1) * D], scalar=1.0,
                                                                          in1=ng[:, j, :], op0=ALU.add, op1=ALU.mult),
                     R=[modrow.b, ng.b], W=[vrow.b])
                S.op("dve", lambda e, j=j, shi=shi: e.tensor_copy(vrow[:, 2 * j + 1, :], modrow[:, shi * D:(shi + 1) * D]),
                     R=[modrow.b], W=[vrow.b])
            S.op("dve", lambda e: e.tensor_copy(grow[:, 0:D], modrow[:, 2 * D:3 * D]), R=[modrow.b], W=[grow.b])
            S.op("dve", lambda e: e.tensor_copy(grow[:, D:2 * D], modrow[:, 5 * D:6 * D]), R=[modrow.b], W=[grow.b])
            for v in range(4):
                for dc in range(8):
                    o = (v * 8 + dc) * 2
                    S.op("pe", lambda e, v=v, dc=dc, o=o: e.transpose(ps[6][:, o:o + 2], vrow[0:2, v, dc * 128:(dc + 1) * 128], ident_f[0:2, 0:2]),
                         R=[vrow.b, ident_f.b], W=[ps[6].b])
            S.op("dve", lambda e: e.tensor_copy(modT[:].rearrange("p a b c -> p (a b c)"), ps[6][:, 0:64]), R=[ps[6].b], W=[modT.b])
            S.barrier()
        chk(l, "A")

        def gate_bcast(dst, gi, j):
            for hf in range(2):
                S.op("pe", lambda e, hf=hf: e.matmul(ps[6 + hf][:, :], lhsT=sel_f[0:2, j * 128:(j + 1) * 128],
                                                    rhs=grow[0:2, gi * D + hf * 512: gi * D + (hf + 1) * 512], start=True, stop=True),
                     R=[sel_f.b, grow.b], W=[ps[6 + hf].b])
                S.op("act", lambda e, hf=hf: e.activation(out=dst[:, hf * 512:(hf + 1) * 512], in_=ps[6 + hf][:, :], func=AF.Copy),
                     R=[ps[6 + hf].b], W=[dst.b])

        def modulate_T(ht, xn, sqj, ssq, rstd, aT_ap, aT_b, vA, j, psbank):
            rstd_of(None, ht[:].rearrange("p (h d) -> p h d", h=1), ht.b, 1, D, sqj, ssq, rstd)
            S.op("dve", lambda e: e.tensor_scalar(out=xn[:], in0=ht[:], scalar1=rstd[:, 0:1], scalar2=None, op0=ALU.mult),
                 R=[ht.b, rstd.b], W=[xn.b])
            pv = psb(psbank)
            for dc in range(8):
                S.op("pe", lambda e, dc=dc: e.transpose(pv[:, dc * 128:(dc + 1) * 128], xn[:, dc * 128:(dc + 1) * 128], ident_b[:]),
                     R=[xn.b, ident_b.b], W=[ps[psbank].b])
            for dc in range(8):
                S.op("dve", lambda e, dc=dc: e.tensor_scalar(out=aT_ap[:, dc, :], in0=pv[:, dc * 128:(dc + 1) * 128],
                                                            scalar1=modT[:, vA, dc, j:j + 1], scalar2=modT[:, vA + 1, dc, j:j + 1],
                                                            op0=ALU.mult, op1=ALU.add),
                     R=[ps[psbank].b, modT.b], W=[aT_b])

        with contextlib.ExitStack() as ph:
            stg = [sb("stg%d" % i, [128, 2048], F32, ph) for i in range(2)]
            w_in_sb = sb("w_in_sb", [128, 8, DIN], BF16, ph)
            wuq_sb = sb("wuq_sb", [128, 3, 768], BF16, ph)
            wukv_sb = sb("wukv_sb", [128, 2, 1024], BF16, ph)
            gq = sb("gq", [128, 3], F32, ph)
            gkv = sb("gkv", [128, 2], F32, ph)
            g_mq = sb("g_mq", [128, 96], F32, ph)
            g_mk = sb("g_mk", [128, 96], F32, ph)
            g_sq = sb("g_sq", [128, 64], F32, ph)
            g_sk = sb("g_sk", [128, 64], F32, ph)
            S.dma(gq[:], qa_g[l], W=[gq.b])
            S.dma(gkv[:], kva_g[l], W=[gkv.b])
            S.dma(g_mq[:], mqn_g[l:l + 1, :].to_broadcast([128, 96]), W=[g_mq.b])
            S.dma(g_mk[:], mkn_g[l:l + 1, :].to_broadcast([128, 96]), W=[g_mk.b])
            S.dma(g_sq[:], sqn_g[l:l + 1, :].to_broadcast([128, 64]), W=[g_sq.b])
            S.dma(g_sk[:], skn_g[l:l + 1, :].to_broadcast([128, 64]), W=[g_sk.b])
            for dc in range(8):
                load_w_bf16(ph, (w_in_sb[:, dc, :], w_in_sb.b), w_in[l, dc * 128:(dc + 1) * 128, :], DIN, stg)
            for kc in range(3):
                load_w_bf16(ph, (wuq_sb[:, kc, :], wuq_sb.b), wuq[l, kc * 128:(kc + 1) * 128, :], 768, stg, gq[:, kc:kc + 1], gq.b)
            for kc in range(2):
                load_w_bf16(ph, (wukv_sb[:, kc, :], wukv_sb.b), wukv[l, kc * 128:(kc + 1) * 128, :], 1024, stg, gkv[:, kc:kc + 1], gkv.b)

            ht = [sb("ht%d" % i, [128, D], F32, ph) for i in range(2)]
            cstm = [sb("cstm%d" % i, [128, 2, 32], F32, ph) for i in range(2)]
            csts = [sb("csts%d" % i, [128, 2, 64], F32, ph) for i in range(2)]
            def mkset(par):
                d_ = {}
                d_["sqj"] = sb("sqj%d" % par, [128, D], F32, ph)
                d_["t1"] = sb("t1%d" % par, [128, D], F32, ph)
                d_["t2"] = sb("t2%d" % par, [128, D], F32, ph)
                d_["xn"] = sb("xn%d" % par, [128, D], BF16, ph)
                d_["aT"] = sb("aT%d" % par, [128, 8, 128], BF16, ph)
                d_["p_sb"] = sb("p_sb%d" % par, [128, DIN], F32, ph)
                d_["qan"] = sb("qan%d" % par, [128, 384], BF16, ph)
                d_["kvan"] = sb("kvan%d" % par, [128, 256], BF16, ph)
                d_["lT"] = sb("lT%d" % par, [128, 5, 128], BF16, ph)
                d_["qm"] = sb("qm%d" % par, [128, 8, 96], F32, ph)
                d_["kv"] = sb("kv%d" % par, [128, 8, 128], F32, ph)
                d_["kf"] = sb("kf%d" % par, [128, 8, 96], F32, ph)
                d_["qn"] = sb("qn%d" % par, [128, 8, 96], F32, ph)
                d_["kn"] = sb("kn%d" % par, [128, 8, 96], F32, ph)
                d_["sqn"] = sb("sqn%d" % par, [128, 8, 64], F32, ph)
                d_["skn"] = sb("skn%d" % par, [128, 2, 64], F32, ph)
                d_["qo"] = sb("qo%d" % par, [128, 8, 96], BF16, ph)
                d_["ko"] = sb("ko%d" % par, [128, 8, 96], BF16, ph)
                d_["sqo"] = sb("sqo%d" % par, [128, 8, 64], BF16, ph)
                d_["sko"] = sb("sko%d" % par, [128, 2, 64], BF16, ph)
                d_["qT_sb"] = sb("qT_sb%d" % par, [128, 8, 128], BF16, ph)
                d_["kT_sb"] = sb("kT_sb%d" % par, [128, 8, 128], BF16, ph)
                d_["sqT_sb"] = sb("sqT_sb%d" % par, [128, 8, 128], BF16, ph)
                d_["skT_sb"] = sb("skT_sb%d" % par, [128, 2, 128], BF16, ph)
                return d_

            BS = [mkset(0), mkset(1)]
            Vm_sb = [sb("Vm_sb%d" % i, [128, 8, 65], BF16, ph) for i in range(2)]
            Vs_sb = [sb("Vs_sb%d" % i, [128, 2, 65], BF16, ph) for i in range(2)]
            for par_ in range(2):
                BS[par_]["ssq"] = sb("ssq%d" % par_, [128, 16], F32, ph)
                BS[par_]["rstd"] = sb("rstd%d" % par_, [128, 16], F32, ph)
            for i in range(2):
                S.op("pool", lambda e, i=i: e.memset(Vm_sb[i][:], 1.0), W=[Vm_sb[i].b])
                S.op("pool", lambda e, i=i: e.memset(Vs_sb[i][:], 1.0), W=[Vs_sb[i].b])

            def loads(i):
                hb = ht[i % 2]
                if l == 0:
                    src = ctx_d[i * 128:(i + 1) * 128, :] if i < 2 else x_d[(i - 2) * 128:(i - 1) * 128, :]
                else:
                    src = h_s[i * 128:(i + 1) * 128, :]
                S.dma(hb[:], src, W=[hb.b])
                S.dma(cstm[i % 2][:].rearrange("p a b -> p (a b)"), csm_d[i * 128:(i + 1) * 128, :], W=[cstm[i % 2].b])
                S.dma(csts[i % 2][:].rearrange("p a b -> p (a b)"), css_d[i * 128:(i + 1) * 128, :], W=[csts[i % 2].b])

            def tile_body(i, par):
                d_ = BS[par]
                sqj, t1, t2, xn, aT, p_sb, qan, kvan, lT, qm, kv, kf, qn, kn, sqn, skn, qo, ko, sqo, sko, qT_sb, kT_sb, sqT_sb, skT_sb, ssq, rstd = d_["sqj"], d_["t1"], d_["t2"], d_["xn"], d_["aT"], d_["p_sb"], d_["qan"], d_["kvan"], d_["lT"], d_["qm"], d_["kv"], d_["kf"], d_["qn"], d_["kn"], d_["sqn"], d_["skn"], d_["qo"], d_["ko"], d_["sqo"], d_["sko"], d_["qT_sb"], d_["kT_sb"], d_["sqT_sb"], d_["skT_sb"], d_["ssq"], d_["rstd"]
                ps_ = ps[4 * par:] + ps[:4 * par]
                psb_ = lambda k_: psb((k_ + 4 * par) % 8)
                j = 1 if i < 2 else 0
                hb = ht[i % 2]
                cm = cstm[i % 2]
                cs_ = csts[i % 2]
                tsl = slice(i * 128, (i + 1) * 128)
                modulate_T(hb, xn, sqj, ssq, rstd, aT[:], aT.b, 0, j, (4 * par) % 8)
                for cb, (c0, c1) in enumerate(((0, 512), (512, 1024), (1024, DIN))):
                    for dc in range(8):
                        S.op("pe", lambda e, cb=cb, c0=c0, c1=c1, dc=dc: e.matmul(ps_[1 + cb][:, 0:c1 - c0], lhsT=aT[:, dc, :], rhs=w_in_sb[:, dc, c0:c1],
                                                                              start=(dc == 0), stop=(dc == 7)),
                             R=[aT.b, w_in_sb.b], W=[ps_[1 + cb].b], inc=(dc == 7))
                    S.op("act", lambda e, cb=cb, c0=c0, c1=c1: e.activation(out=p_sb[:, c0:c1], in_=ps_[1 + cb][:, 0:c1 - c0], func=AF.Copy),
                         R=[ps_[1 + cb].b], W=[p_sb.b])
                rstd_of(ph, p_sb[:, 0:384].rearrange("p (h d) -> p h d", h=1), p_sb.b, 1, 384, sqj, ssq, rstd)
                S.op("dve", lambda e: e.tensor_scalar(out=qan[:], in0=p_sb[:, 0:384], scalar1=rstd[:, 0:1], scalar2=None, op0=ALU.mult),
                     R=[p_sb.b, rstd.b], W=[qan.b])
                rstd_of(ph, p_sb[:, 896:1152].rearrange("p (h d) -> p h d", h=1), p_sb.b, 1, 256, sqj, ssq, rstd)
                S.op("dve", lambda e: e.tensor_scalar(out=kvan[:], in0=p_sb[:, 896:1152], scalar1=rstd[:, 0:1], scalar2=None, op0=ALU.mult),
                     R=[p_sb.b, rstd.b], W=[kvan.b])
                pv4 = psb_(4)
                for kc in range(3):
                    S.op("pe", lambda e, kc=kc: e.transpose(pv4[:, kc * 128:(kc + 1) * 128], qan[:, kc * 128:(kc + 1) * 128], ident_b[:]),
                         R=[qan.b, ident_b.b], W=[ps_[4].b])
                for kc in range(2):
                    S.op("pe", lambda e, kc=kc: e.transpose(pv4[:, (3 + kc) * 128:(4 + kc) * 128], kvan[:, kc * 128:(kc + 1) * 128], ident_b[:]),
                         R=[kvan.b, ident_b.b], W=[ps_[4].b])
                S.op("act", lambda e: e.activation(out=lT[:].rearrange("p a b -> p (a b)"), in_=pv4[:, 0:640], func=AF.Copy), R=[ps_[4].b], W=[lT.b])
                for cb, (c0, c1) in enumerate(((0, 512), (512, 768))):
                    for kc in range(3):
                        S.op("pe", lambda e, cb=cb, c0=c0, c1=c1, kc=kc: e.matmul(ps_[5 + cb][:, 0:c1 - c0], lhsT=lT[:, kc, :], rhs=wuq_sb[:, kc, c0:c1],
                                                                              start=(kc == 0), stop=(kc == 2)),
                             R=[lT.b, wuq_sb.b], W=[ps_[5 + cb].b], inc=(kc == 2))
                    S.op("act", lambda e, cb=cb, c0=c0, c1=c1: e.activation(out=qm[:].rearrange("p a b -> p (a b)")[:, c0:c1], in_=ps_[5 + cb][:, 0:c1 - c0], func=AF.Copy),
                         R=[ps_[5 + cb].b], W=[qm.b])
                for cb in range(2):
                    for kc in range(2):
                        S.op("pe", lambda e, cb=cb, kc=kc: e.matmul(ps_[1 + cb][:, :], lhsT=lT[:, 3 + kc, :], rhs=wukv_sb[:, kc, cb * 512:(cb + 1) * 512],
                                                                start=(kc == 0), stop=(kc == 1)),
                             R=[lT.b, wukv_sb.b], W=[ps_[1 + cb].b], inc=(kc == 1))
                    S.op("act", lambda e, cb=cb: e.activation(out=kv[:].rearrange("p a b -> p (a b)")[:, cb * 512:(cb + 1) * 512], in_=ps_[1 + cb][:, :], func=AF.Copy),
                         R=[ps_[1 + cb].b], W=[kv.b])
                rstd_of(ph, qm[:], qm.b, 8, 96, sqj, ssq, rstd)
                S.op("dve", lambda e: e.tensor_tensor(out=qn[:], in0=qm[:], in1=rstd[:, 0:8].unsqueeze(2).to_broadcast([128, 8, 96]), op=ALU.mult),
                     R=[qm.b, rstd.b], W=[qn.b])
                S.op("dve", lambda e: e.tensor_tensor(out=qn[:], in0=qn[:], in1=g_mq[:].unsqueeze(1).to_broadcast([128, 8, 96]), op=ALU.mult),
                     R=[qn.b, g_mq.b], W=[qn.b])
                S.op("dve", lambda e: e.tensor_copy(qo[:, :, 0:64], qn[:, :, 0:64]), R=[qn.b], W=[qo.b])
                rope(qn[:, :, 64:96].rearrange("p h (a b q) -> p h a b q", a=2, b=2), qn.b, cm, cm.b, 8, 8, t1, t2,
                     qo[:, :, 64:96].rearrange("p h (a b q) -> p h a b q", a=2, b=2), qo.b)
                S.op("dve", lambda e: e.tensor_copy(kf[:, :, 0:64], kv[:, :, 0:64]), R=[kv.b], W=[kf.b])
                S.op("dve", lambda e: e.tensor_copy(kf[:, :, 64:96], p_sb[:, 1152:1184].unsqueeze(1).to_broadcast([128, 8, 32])), R=[p_sb.b], W=[kf.b])
                rstd_of(ph, kf[:], kf.b, 8, 96, sqj, ssq, rstd)
                S.op("dve", lambda e: e.tensor_tensor(out=kn[:], in0=kf[:], in1=rstd[:, 0:8].unsqueeze(2).to_broadcast([128, 8, 96]), op=ALU.mult),
                     R=[kf.b, rstd.b], W=[kn.b])
                S.op("dve", lambda e: e.tensor_tensor(out=kn[:], in0=kn[:], in1=g_mk[:].unsqueeze(1).to_broadcast([128, 8, 96]), op=ALU.mult),
                     R=[kn.b, g_mk.b], W=[kn.b])
                S.op("dve", lambda e: e.tensor_copy(ko[:, :, 0:64], kn[:, :, 0:64]), R=[kn.b], W=[ko.b])
                rope(kn[:, :, 64:96].rearrange("p h (a b q) -> p h a b q", a=2, b=2), kn.b, cm, cm.b, 8, 8, t1, t2,
                     ko[:, :, 64:96].rearrange("p h (a b q) -> p h a b q", a=2, b=2), ko.b)
                vmb = Vm_sb[i % 2]
                S.op("act", lambda e, vmb=vmb: e.activation(out=vmb[:, :, 0:64], in_=kv[:, :, 64:128], func=AF.Copy), R=[kv.b], W=[vmb.b])
                S.dma(Vm_s[tsl, :], vmb[:].rearrange("p a b -> p (a b)"), R=[vmb.b])
                sq3 = p_sb[:, 384:896].rearrange("p (h d) -> p h d", h=8)
                rstd_of(ph, sq3, p_sb.b, 8, 64, sqj, ssq, rstd)
                S.op("dve", lambda e: e.tensor_tensor(out=sqn[:], in0=sq3, in1=rstd[:, 0:8].unsqueeze(2).to_broadcast([128, 8, 64]), op=ALU.mult),
                     R=[p_sb.b, rstd.b], W=[sqn.b])
                S.op("dve", lambda e: e.tensor_tensor(out=sqn[:], in0=sqn[:], in1=g_sq[:].unsqueeze(1).to_broadcast([128, 8, 64]), op=ALU.mult),
                     R=[sqn.b, g_sq.b], W=[sqn.b])
                rope(sqn[:].rearrange("p h (a b q) -> p h a b q", a=2, b=2), sqn.b, cs_, cs_.b, 8, 16, t1, t2,
                     sqo[:].rearrange("p h (a b q) -> p h a b q", a=2, b=2), sqo.b)
                sk3 = p_sb[:, 1184:1312].rearrange("p (h d) -> p h d", h=2)
                rstd_of(ph, sk3, p_sb.b, 2, 64, sqj, ssq, rstd)
                S.op("dve", lambda e: e.tensor_tensor(out=skn[:], in0=sk3, in1=rstd[:, 0:2].unsqueeze(2).to_broadcast([128, 2, 64]), op=ALU.mult),
                     R=[p_sb.b, rstd.b], W=[skn.b])
                S.op("dve", lambda e: e.tensor_tensor(out=skn[:], in0=skn[:], in1=g_sk[:].unsqueeze(1).to_broadcast([128, 2, 64]), op=ALU.mult),
                     R=[skn.b, g_sk.b], W=[skn.b])
                rope(skn[:].rearrange("p h (a b q) -> p h a b q", a=2, b=2), skn.b, cs_, cs_.b, 2, 16, t1, t2,
                     sko[:].rearrange("p h (a b q) -> p h a b q", a=2, b=2), sko.b)
                vsb = Vs_sb[i % 2]
                S.op("act", lambda e, vsb=vsb: e.activation(out=vsb[:, :, 0:64], in_=p_sb[:, 1312:1440].rearrange("p (h d) -> p h d", h=2), func=AF.Copy),
                     R=[p_sb.b], W=[vsb.b])
                S.dma(Vs_s[tsl, :], vsb[:].rearrange("p a b -> p (a b)"), R=[vsb.b])
                for (src, dstT, nh, dh, bank, scr) in ((qo, qT_sb, 8, 96, 7, qTm_s), (ko, kT_sb, 8, 96, 0, kTm_s),
                                                       (sqo, sqT_sb, 8, 64, 4, qTs_s), (sko, skT_sb, 2, 64, 3, kTs_s)):
                    pvv = psb_(bank)
                    for h in range(nh):
                        S.op("pe", lambda e, src=src, h=h, dh=dh, pvv=pvv: e.transpose(pvv[0:dh, h * 128:(h + 1) * 128], src[:, h, :], ident_b[:]),
                             R=[src.b, ident_b.b], W=[ps_[bank].b])
                    S.op("act", lambda e, dstT=dstT, nh=nh, dh=dh, pvv=pvv: e.activation(out=dstT[0:dh, 0:nh, :].rearrange("p a b -> p (a b)"),
                                                                                       in_=pvv[0:dh, 0:nh * 128], func=AF.Copy),
                         R=[ps_[bank].b], W=[dstT.b])
                    S.dma(scr[:, :, tsl].rearrange("h d t -> d h t"), dstT[0:dh, 0:nh, :], R=[dstT.b])
                if i + 2 < NT:
                    loads(i + 2)

            loads(0)
            loads(1)
            for pr in range(NT // 2):
                bg_issue(2)
                recs = []
                for par in range(2):
                    S.rec = []
                    tile_body(2 * pr + par, par)
                    recs.append(S.rec)
                    S.rec = None
                for n_ in range(max(len(recs[0]), len(recs[1]))):
                    for par in range(2):
                        if n_ < len(recs[par]):
                            recs[par][n_]()
            S.barrier()
        chk(l, "B")

        with contextlib.ExitStack() as ph:
            kT_all = [sb("kTa%d" % h, [128, T], BF16, ph) for h in range(8)]
            Vall = sb("Vall", [128, NT, 520], BF16, ph)
            kTs_all = sb("kTs_all", [64, 2, T], BF16, ph)
            Vs_all = sb("Vs_all", [128, NT, 130], BF16, ph)
            qTg = [sb("qTg%d" % i, [128, 8, 512], BF16, ph) for i in range(2)]
            sqTg = [sb("sqTg%d" % i, [64, 8, 512], BF16, ph) for i in range(2)]
            PT = [sb("PT%d" % i, [128, 512], BF16, ph) for i in range(4)]
            obuf = [sb("obuf0", [128, 4, D], BF16, ph)]
            rden = sb("rden", [128, 4], F32, ph)
            for h in range(8):
                S.dma(kT_all[h][0:96, :], kTm_s[h], W=[kT_all[h].b])
            S.dma(Vall[:], Vm_s.rearrange("(k p) e -> p k e", p=128), W=[Vall.b])
            S.dma(kTs_all[:], kTs_s.rearrange("g d t -> d g t"), W=[kTs_all.b])
            S.dma(Vs_all[:], Vs_s.rearrange("(k p) e -> p k e", p=128), W=[Vs_all.b])
            groups = ([] if last else [(0, 256)]) + [(256 + 512 * g, 512) for g in range(8)]
            ctr = {"s": 0, "p": 0, "o": 0}

            def qloads(gi):
                t0, nq = groups[gi]
                S.dma(qTg[gi % 2][0:96, :, 0:nq], qTm_s[:, :, t0:t0 + nq].rearrange("h d t -> d h t"), W=[qTg[gi % 2].b])
                S.dma(sqTg[gi % 2][:, :, 0:nq], qTs_s[:, :, t0:t0 + nq].rearrange("h d t -> d h t"), W=[sqTg[gi % 2].b])

            qloads(0)
            for gi, (t0, nq) in enumerate(groups):
                bg_issue(4)
                if gi + 1 < len(groups):
                    qloads(gi + 1)
                isctx = t0 < 256
                nblk = nq // 128
                qg = qTg[gi % 2]
                sg = sqTg[gi % 2]
                ob = obuf[0]
                kts = [0, 1] if isctx else list(range(NT))
                its = []
                for h in range(8):
                    for ki, kt in enumerate(kts):
                        its.append(("m", h, ki, kt, len(kts), None, None))
                for blk in range(nblk):
                    ti = t0 // 128 + blk
                    if isctx:
                        kl = [0, 1]
                    else:
                        kl = [0, 1] + ([ti - 1] if ti - 1 >= 2 else []) + [ti] + ([ti + 1] if ti + 1 < NT else [])
                    for g2 in range(2):
                        for ki, kt in enumerate(kl):
                            its.append(("s", g2, ki, kt, len(kl), blk, ti))

                def emitS(n):
                    kind, a, ki, kt, nk, blk, ti = its[n]
                    pS = ps[n % 4]
                    if kind == "m":
                        S.op("pe", lambda e: e.matmul(pS[:, 0:nq], lhsT=kT_all[a][0:96, kt * 128:(kt + 1) * 128], rhs=qg[0:96, a, 0:nq], start=True, stop=True),
                             R=[kT_all[a].b, qg.b], W=[pS.b])
                    else:
                        S.op("pe", lambda e: e.matmul(pS[:, :].rearrange("p (a b) -> p a b", a=4), lhsT=kTs_all[:, a, kt * 128:(kt + 1) * 128],
                                                      rhs=sg[:, 4 * a:4 * a + 4, blk * 128:(blk + 1) * 128], start=True, stop=True),
                             R=[kTs_all.b, sg.b], W=[pS.b])

                po_of = {}

                def emitRest(n):
                    kind, a, ki, kt, nk, blk, ti = its[n]
                    pS = ps[n % 4]
                    pt = PT[n % 4]
                    if ki == 0:
                        po_of["cur"] = ps[4 + ctr["o"] % 2]
                        ctr["o"] += 1
                    po = po_of["cur"]
                    if kind == "m":
                        S.op("act", lambda e: e.activation(out=pt[:, 0:nq], in_=pS[:, 0:nq], func=AF.Exp, scale=96.0 ** -0.5), R=[pS.b], W=[pt.b])
                        for qs in range(nblk):
                            S.op("pe", lambda e, qs=qs: e.matmul(po[:, qs * 65:(qs + 1) * 65], lhsT=pt[:, qs * 128:(qs + 1) * 128], rhs=Vall[:, kt, a * 65:(a + 1) * 65],
                                                                start=(ki == 0 and qs == 0), stop=(ki == nk - 1), skip_group_check=True),
                                 R=[pt.b, Vall.b], W=[po.b], inc=(qs == nblk - 1))
                        if ki == nk - 1:
                            po3 = po[:, 0:nblk * 65].rearrange("p (a b) -> p a b", b=65)
                            S.op("dve", lambda e: e.reciprocal(out=rden[:, 0:nblk], in_=po3[:, :, 64]), R=[po.b], W=[rden.b])
                            S.op("dve", lambda e: e.tensor_tensor(out=ob[:, 0:nblk, a * 64:(a + 1) * 64], in0=po3[:, :, 0:64],
                                                                  in1=rden[:, 0:nblk].unsqueeze(2).to_broadcast([128, nblk, 64]), op=ALU.mult),
                                 R=[po.b, rden.b], W=[ob.b])
                    else:
                        S.op("act", lambda e: e.activation(out=pt[:, :], in_=pS[:, :], func=AF.Exp, scale=0.125), R=[pS.b], W=[pt.b])
                        if (not isctx) and kt >= 2 and kt != ti:
                            mi = 0 if kt == ti - 1 else 1
                            S.op("pool", lambda e: e.tensor_tensor(out=pt[:, :].rearrange("p (a b) -> p a b", a=4), in0=pt[:, :].rearrange("p (a b) -> p a b", a=4),
                                                                   in1=tri_b[:, mi, :].unsqueeze(1).to_broadcast([128, 4, 128]), op=ALU.mult),
                                 R=[pt.b, tri_b.b], W=[pt.b])
                        for r in range(4):
                            S.op("pe", lambda e, r=r: e.matmul(po[:, r * 65:(r + 1) * 65], lhsT=pt[:, r * 128:(r + 1) * 128], rhs=Vs_all[:, kt, a * 65:(a + 1) * 65],
                                                              start=(ki == 0 and r == 0), stop=(ki == nk - 1), skip_group_check=True),
                                 R=[pt.b, Vs_all.b], W=[po.b], inc=(r == 3))
                        if ki == nk - 1:
                            po3 = po[:, 0:260].rearrange("p (a b) -> p a b", b=65)
                            S.op("dve", lambda e: e.tensor_tensor(out=rden[:, 0:4], in0=po3[:, :, 64], in1=esink[:, 4 * a:4 * a + 4], op=ALU.add),
                                 R=[po.b, esink.b], W=[rden.b])
                            S.op("dve", lambda e: e.reciprocal(out=rden[:, 0:4], in_=rden[:, 0:4]), R=[rden.b], W=[rden.b])
                            S.op("dve", lambda e: e.tensor_tensor(out=ob[:, blk, 512 + a * 256:512 + (a + 1) * 256].rearrange("p (a b) -> p a b", a=4), in0=po3[:, :, 0:64],
                                                                  in1=rden[:, 0:4].unsqueeze(2).to_broadcast([128, 4, 64]), op=ALU.mult),
                                 R=[po.b, rden.b], W=[ob.b])

                LA = 2
                for n in range(min(LA, len(its))):
                    emitS(n)
                for n in range(len(its)):
                    if n + LA < len(its):
                        emitS(n + LA)
                    emitRest(n)
                S.dma(om_s[t0:t0 + nq, :].rearrange("(b p) c -> p b c", p=128), ob[:, 0:nblk, :], R=[ob.b])
            S.barrier()
        chk(l, "C")

        with contextlib.ExitStack() as ph:
            stg = [sb("stgd%d" % i, [128, 2048], F32, ph) for i in range(2)]
            w_out_sb = sb("w_out_sb", [128, 8, D], BF16, ph)
            for kc in range(8):
                load_w_bf16(ph, (w_out_sb[:, kc, :], w_out_sb.b), w_out[l, kc * 128:(kc + 1) * 128, :], D, stg)
            gb = [sb("gbd%d" % j, [128, D], F32, ph) for j in range(2)]
            gate_bcast(gb[0], 0, 0)
            gate_bcast(gb[1], 0, 1)
            htd = [sb("htd%d" % i, [128, D], F32, ph) for i in range(2)]
            omb = [sb("omb%d" % i, [128, D], BF16, ph) for i in range(2)]
            oT = sb("oT", [128, 8, 128], BF16, ph)
            h1 = [sb("h1d%d" % i, [128, D], F32, ph) for i in range(2)]
            tiles = list(range(2 if last else 0, NT))

            def loadsD(i):
                if l == 0:
                    src = ctx_d[i * 128:(i + 1) * 128, :] if i < 2 else x_d[(i - 2) * 128:(i - 1) * 128, :]
                else:
                    src = h_s[i * 128:(i + 1) * 128, :]
                S.dma(htd[i % 2][:], src, W=[htd[i % 2].b])
                S.dma(omb[i % 2][:], om_s[i * 128:(i + 1) * 128, :], W=[omb[i % 2].b])

            loadsD(tiles[0])
            for n_, i in enumerate(tiles):
                if n_ + 1 < len(tiles):
                    loadsD(tiles[n_ + 1])
                j = 1 if i < 2 else 0
                pv = psb(0)
                for kc in range(8):
                    S.op("pe", lambda e, kc=kc, i=i: e.transpose(pv[:, kc * 128:(kc + 1) * 128], omb[i % 2][:, kc * 128:(kc + 1) * 128], ident_b[:]),
                         R=[omb[i % 2].b, ident_b.b], W=[ps[0].b])
                S.op("act", lambda e: e.activation(out=oT[:].rearrange("p a b -> p (a b)"), in_=pv[:, :], func=AF.Copy), R=[ps[0].b], W=[oT.b])
                hh = h1[i % 2]
                for hf in range(2):
                    for kc in range(8):
                        S.op("pe", lambda e, hf=hf, kc=kc: e.matmul(ps[1 + hf][:, :], lhsT=oT[:, kc, :], rhs=w_out_sb[:, kc, hf * 512:(hf + 1) * 512],
                                                                start=(kc == 0), stop=(kc == 7)),
                             R=[oT.b, w_out_sb.b], W=[ps[1 + hf].b], inc=(kc == 7))
                    S.op("dve", lambda e, hf=hf, j=j, hh=hh: e.tensor_tensor(out=hh[:, hf * 512:(hf + 1) * 512], in0=ps[1 + hf][:, :],
                                                                          in1=gb[j][:, hf * 512:(hf + 1) * 512], op=ALU.mult),
                         R=[ps[1 + hf].b, gb[j].b], W=[hh.b])
                S.op("pool", lambda e, hh=hh, i=i: e.tensor_tensor(out=hh[:], in0=hh[:], in1=htd[i % 2][:], op=ALU.add), R=[hh.b, htd[i % 2].b], W=[hh.b])
                S.dma(h_s[i * 128:(i + 1) * 128, :], hh[:], R=[hh.b])
            bg_issue(len(bg_list))
            S.barrier(bg=True)
        chk(l, "D")

        with contextlib.ExitStack() as ph:
            s_sb = sb("s_sb", [128, 2048], F32, ph)
            cand = sb("cand", [128, 2048], F32, ph)
            stg = [s_sb, cand]
            wq_sb = sb("wq_sb", [128, 8, 2048], BF16, ph)
            keysT_sb = sb("keysT_sb", [128, 2048], BF16, ph)
            for dc in range(8):
                load_w_bf16(ph, (wq_sb[:, dc, :], wq_sb.b), wq_d[l, dc * 128:(dc + 1) * 128, :], 2048, stg)
            load_w_bf16(ph, (keysT_sb[:, :], keysT_sb.b), keysT_d[l], 2048, stg)
            gbe = sb("gbe", [128, D], F32, ph)
            gate_bcast(gbe, 1, 0 if last else 1)
            WtS = sb("WtS", [128, 256, 128], BF16, ph)
            Ab = [sb("Ab%d" % i, [128, 8, 128], BF16, ph) for i in range(2)]
            Bb = [sb("Bb%d" % i, [128, 8, 128], BF16, ph) for i in range(2)]
            utc = [sb("utc%d" % i, [128, 8, 128], BF16, ph) for i in range(6)]
            vc = [sb("vc%d" % i, [128, D], BF16, ph) for i in range(4)]
            h1e = [[sb("h1e%d_%d" % (p_, i), [128, D], F32, ph) for i in range(2)] for p_ in range(2)]
            xn = sb("xne", [128, D], BF16, ph)
            bT = [sb("bT%d" % p_, [128, 8, 256], BF16, ph) for p_ in range(2)]
            qTe = sb("qTe", [128, 16, 256], BF16, ph)
            sv = sb("sv", [128, 16, 16], F32, ph)
            si_u = sb("si_u", [128, 16, 16], U32, ph)
            si_f = sb("si_f", [128, 16, 16], F32, ph)
            cv = sb("cv", [128, 8, 16], F32, ph)
            ci_u = sb("ci_u", [128, 8, 16], U32, ph)
            k_u = sb("k_u", [128, 2, 128], U32, ph)
            k_f = sb("k_f", [128, 2, 128], F32, ph)
            IG = sb("IG", [128, 3, 128], F32, ph)
            IGT = [sb("IGT%d" % p_, [128, 3, 256], BF16, ph) for p_ in range(2)]
            gsum = sb("gsum", [128, 8], F32, ph)
            ssq = sb("ssqe", [128, 16], F32, ph)
            rstd = sb("rstde", [128, 16], F32, ph)
            ga = [sb("ga%d" % i, [128, 256], BF16, ph) for i in range(2)]
            GT = [sb("GT%d" % i, [128, 256], BF16, ph) for i in range(3)]
            iota16 = iota_f[:, 0:16]
            S.barrier()
            s_b = [Buf() for _ in range(16)]
            c_b = [Buf() for _ in range(16)]
            sv_b = [Buf() for _ in range(16)]
            siu_b = [Buf() for _ in range(16)]
            cv_b = [Buf() for _ in range(8)]
            ciu_b = [Buf() for _ in range(8)]
            s3 = s_sb[:].rearrange("p (a b) -> p a b", a=16)
            c3s = cand[:].rearrange("p (a b) -> p a b", a=16)
            cg3 = cand[:].rearrange("p (h a) -> p h a", h=8)
            sg3 = s_sb[:].rearrange("p (h a) -> p h a", h=8)

            def front(g, par):
                j = 1 if g == 0 else 0
                bTp = bT[par]
                for tt in range(2):
                    i = g * 2 + tt
                    S.dma(h1e[par][tt][:], h_s[i * 128:(i + 1) * 128, :], W=[h1e[par][tt].b])
                    modulate_T(h1e[par][tt], xn, xn, ssq, rstd, bTp[:, :, tt * 128:(tt + 1) * 128], bTp.b, 2, j, 7)
                    yield
                for hp in range(16):
                    pq = ps[7]
                    for dc in range(8):
                        S.op("pe", lambda e, dc=dc: e.matmul(pq[:, 0:256], lhsT=wq_sb[:, dc, hp * 128:(hp + 1) * 128], rhs=bTp[:, dc, :],
                                                            start=(dc == 0), stop=(dc == 7)),
                             R=[wq_sb.b, bTp.b], W=[pq.b], inc=(dc == 7))
                    if hp % 2 == 0:
                        S.op("act", lambda e: e.activation(out=qTe[:, hp, :], in_=pq[:, 0:256], func=AF.Copy), R=[pq.b], W=[qTe.b])
                    else:
                        S.op("dve", lambda e: e.tensor_copy(qTe[:, hp, :], pq[:, 0:256]), R=[pq.b], W=[qTe.b])
                    yield
                for tt in range(2):
                    for qd in range(4):
                        pb = ps[7]
                        for k4 in range(4):
                            hp = qd * 4 + k4
                            S.op("pe", lambda e, hp=hp, k4=k4: e.matmul(pb[:, k4 * 128:(k4 + 1) * 128], lhsT=qTe[:, hp, tt * 128:(tt + 1) * 128],
                                                                      rhs=keysT_sb[:, hp * 128:(hp + 1) * 128], start=True, stop=True),
                                 R=[qTe.b, keysT_sb.b], W=[pb.b], inc=(k4 == 3))
                        S.op("act", lambda e: e.activation(out=s_sb[:, qd * 512:(qd + 1) * 512], in_=pb[:, :], func=AF.Copy), R=[pb.b], W=s_b[qd * 4:qd * 4 + 4])
                        yield
                    for hp in range(16):
                        S.op("dve", lambda e, hp=hp: e.max(out=sv[:, hp, 0:8], in_=s3[:, hp, :]), R=[s_b[hp]], W=[sv_b[hp]])
                        if hp % 4 == 3:
                            yield
                    for hp in range(16):
                        S.op("dve", lambda e, hp=hp: e.max_index(out=si_u[:, hp, 0:8], in_max=sv[:, hp, 0:8], in_values=s3[:, hp, :]), R=[s_b[hp], sv_b[hp]], W=[siu_b[hp]])
                        if hp % 4 == 3:
                            yield
                    for hp in range(16):
                        S.op("dve", lambda e, hp=hp: e.match_replace(out=c3s[:, hp, :], in_to_replace=sv[:, hp, 0:8], in_values=s3[:, hp, :], imm_value=NEG),
                             R=[s_b[hp], sv_b[hp]], W=[c_b[hp]])
                        if hp % 4 == 3:
                            yield
                    for hp in range(16):
                        S.op("dve", lambda e, hp=hp: e.max(out=sv[:, hp, 8:16], in_=c3s[:, hp, :]), R=[c_b[hp]], W=[sv_b[hp]])
                        if hp % 4 == 3:
                            yield
                    for hp in range(16):
                        S.op("dve", lambda e, hp=hp: e.max_index(out=si_u[:, hp, 8:16], in_max=sv[:, hp, 8:16], in_values=c3s[:, hp, :]), R=[c_b[hp], sv_b[hp]], W=[siu_b[hp]])
                        if hp % 4 == 3:
                            yield
                    S.op("dve", lambda e: e.tensor_copy(si_f[:], si_u[:]), R=siu_b, W=[si_f.b])
                    sv4 = sv[:].rearrange("p (h a) k -> p h a k", a=2)
                    si4 = si_f[:].rearrange("p (h a) k -> p h a k", a=2)
                    c4 = cand[:].rearrange("p (h a b) -> p h a b", h=8, a=16)
                    S.op("dve", lambda e: e.tensor_tensor(out=c4, in0=sv4[:, :, 0, :].unsqueeze(3).to_broadcast([128, 8, 16, 16]),
                                                          in1=sv4[:, :, 1, :].unsqueeze(2).to_broadcast([128, 8, 16, 16]), op=ALU.add),
                         R=sv_b, W=c_b)
                    yield
                    hb = lambda lst, h: [lst[2 * h], lst[2 * h + 1]]
                    for h in range(8):
                        S.op("dve", lambda e, h=h: e.max(out=cv[:, h, 0:8], in_=cg3[:, h, :]), R=hb(c_b, h), W=[cv_b[h]])
                        if h % 4 == 3:
                            yield
                    for h in range(8):
                        S.op("dve", lambda e, h=h: e.max_index(out=ci_u[:, h, 0:8], in_max=cv[:, h, 0:8], in_values=cg3[:, h, :]), R=hb(c_b, h) + [cv_b[h]], W=[ciu_b[h]])
                        if h % 4 == 3:
                            yield
                    for h in range(8):
                        S.op("dve", lambda e, h=h: e.match_replace(out=sg3[:, h, :], in_to_replace=cv[:, h, 0:8], in_values=cg3[:, h, :], imm_value=NEG),
                             R=hb(c_b, h) + [cv_b[h]], W=hb(s_b, h))
                        if h % 4 == 3:
                            yield
                    for h in range(8):
                        S.op("dve", lambda e, h=h: e.max(out=cv[:, h, 8:16], in_=sg3[:, h, :]), R=hb(s_b, h), W=[cv_b[h]])
                        if h % 4 == 3:
                            yield
                    for h in range(8):
                        S.op("dve", lambda e, h=h: e.max_index(out=ci_u[:, h, 8:16], in_max=cv[:, h, 8:16], in_values=sg3[:, h, :]), R=hb(s_b, h) + [cv_b[h]], W=[ciu_b[h]])
                        if h % 4 == 3:
                            yield
                    ciu2 = ci_u[:].rearrange("p a b -> p (a b)")
                    S.op("dve", lambda e: e.tensor_scalar(out=k_u[:, 0, :], in0=ciu2, scalar1=4, scalar2=None, op0=ALU.logical_shift_right), R=ciu_b, W=[k_u.b])
                    S.op("dve", lambda e: e.tensor_scalar(out=k_u[:, 1, :], in0=ciu2, scalar1=15, scalar2=None, op0=ALU.bitwise_and), R=ciu_b, W=[k_u.b])
                    S.op("dve", lambda e: e.tensor_copy(k_f[:], k_u[:]), R=[k_u.b], W=[k_f.b])
                    yield
                    for a in range(2):
                        kk = k_f[:, a, :].rearrange("p (h k) -> p h k", h=8)
                        e4 = s_sb[:].rearrange("p (h a b) -> p h a b", h=8, a=16)
                        S.op("dve", lambda e: e.tensor_tensor(out=e4, in0=kk.unsqueeze(3).to_broadcast([128, 8, 16, 16]),
                                                              in1=iota16.unsqueeze(1).unsqueeze(1).to_broadcast([128, 8, 16, 16]), op=ALU.is_equal),
                             R=[k_f.b, iota_f.b], W=s_b)
                        yield
                        S.op("dve", lambda e: e.tensor_tensor(out=e4, in0=e4, in1=si4[:, :, a, :].unsqueeze(2).to_broadcast([128, 8, 16, 16]), op=ALU.mult),
                             R=s_b + [si_f.b], W=s_b)
                        yield
                        S.op("dve", lambda e: e.tensor_reduce(out=IG[:, a, :], in_=s_sb[:].rearrange("p (a b) -> p a b", b=16), axis=AX.X, op=ALU.add),
                             R=s_b, W=[IG.b])
                        yield
                    g3 = IG[:, 2, :].rearrange("p (h k) -> p h k", h=8)
                    S.op("dve", lambda e: e.tensor_tensor(out=g3, in0=cv[:], in1=cv[:, :, 0:1].to_broadcast([128, 8, 16]), op=ALU.subtract), R=cv_b, W=[IG.b])
                    S.op("act", lambda e: e.activation(out=IG[:, 2, :], in_=IG[:, 2, :], func=AF.Exp), R=[IG.b], W=[IG.b])
                    S.op("dve", lambda e: e.tensor_reduce(out=gsum[:], in_=g3, axis=AX.X, op=ALU.add), R=[IG.b], W=[gsum.b])
                    S.op("dve", lambda e: e.reciprocal(out=gsum[:], in_=gsum[:]), R=[gsum.b], W=[gsum.b])
                    S.op("dve", lambda e: e.tensor_tensor(out=g3, in0=g3, in1=gsum[:].unsqueeze(2).to_broadcast([128, 8, 16]), op=ALU.mult), R=[IG.b, gsum.b], W=[IG.b])
                    yield
                    pbt = ps[7]
                    for a in range(3):
                        S.op("pe", lambda e, a=a: e.transpose(pbt[:, a * 128:(a + 1) * 128], IG[:, a, :], ident_f[:]), R=[IG.b, ident_f.b], W=[pbt.b])
                    S.op("act", lambda e: e.activation(out=IGT[par][:, :, tt * 128:(tt + 1) * 128], in_=pbt[:, 0:384].rearrange("p (a b) -> p a b", a=3), func=AF.Copy),
                         R=[pbt.b], W=[IGT[par].b])
                    yield

            def build_wt(par):
                IGTp = IGT[par]
                for tb in range(32):
                    A_ = Ab[tb % 2]
                    B_ = Bb[tb % 2]
                    ab_ = AB_b[tb % 2]
                    for t8 in range(8):
                        t = tb * 8 + t8
                        S.op("dve", lambda e, t8=t8, t=t: e.tensor_scalar(out=A_[:, t8, :], in0=iota_b[:], scalar1=IGTp[:, 0, t:t + 1], scalar2=IGTp[:, 2, t:t + 1],
                                                                        op0=ALU.is_equal, op1=ALU.mult),
                             R=[IGTp.b, iota_b.b], W=[ab_[0][t8]])
                        S.op("pool" if t8 % 2 == 1 else "dve",
                             lambda e, t8=t8, t=t: e.tensor_scalar(out=B_[:, t8, :], in0=iota_b[:], scalar1=IGTp[:, 1, t:t + 1], scalar2=None, op0=ALU.is_equal),
                             R=[IGTp.b, iota_b.b], W=[ab_[1][t8]])
                    for q4 in range(2):
                        pw = ps[6 + (tb * 2 + q4) % 2]
                        for t4 in range(4):
                            t = q4 * 4 + t4
                            S.op("pe", lambda e, t=t, t4=t4: e.matmul(pw[:, t4 * 128:(t4 + 1) * 128], lhsT=B_[:, t, :], rhs=A_[:, t, :], start=True, stop=True),
                                 R=[ab_[0][t], ab_[1][t]], W=[pw.b], inc=(t4 == 3))
                        tk0 = tb * 8 + q4 * 4
                        S.op("act", lambda e: e.activation(out=WtS[:, tk0:tk0 + 4, :].rearrange("p a b -> p (a b)"), in_=pw[:, :], func=AF.Copy),
                             R=[pw.b], W=[WtS.b])

            AB_b = [[[Buf() for _ in range(8)] for _ in range(2)] for _ in range(2)]
            ngrp = T // 256
            glist = list(range(1 if last else 0, ngrp))
            for _ in front(glist[0], 0):
                pass
            for gi_, g in enumerate(glist):
                par = gi_ % 2
                j = 1 if g == 0 else 0
                bTp = bT[par]
                build_wt(par)
                nxt = front(glist[gi_ + 1], 1 - par) if gi_ + 1 < len(glist) else iter(())

                def uload(c):
                    S.dma(utc[c % 6][:].rearrange("p a b -> p (a b)"), utb_s[l, c], W=[utc[c % 6].b])

                def vload(c):
                    S.dma(vc[c % 4][:], vb_s[l, c * 128:(c + 1) * 128, :], W=[vc[c % 4].b])

                def emitA(c):
                    u_ = utc[c % 6]
                    pa = ps[4 + c % 3]
                    for dc in range(8):
                        S.op("pe", lambda e, dc=dc: e.matmul(pa[:, 0:256], lhsT=u_[:, dc, :], rhs=bTp[:, dc, :], start=(dc == 0), stop=(dc == 7)),
                             R=[u_.b, bTp.b], W=[pa.b], inc=(dc == 7))

                for c in range(5):
                    uload(c)
                for c in range(3):
                    vload(c)
                def emitMid(c):
                    pa = ps[4 + c % 3]
                    S.op("act", lambda e: e.activation(out=ga[c % 2][:], in_=pa[:, 0:256], func=AF.Gelu), R=[pa.b], W=[ga[c % 2].b])
                    S.op("pool", lambda e: e.tensor_tensor(out=GT[c % 3][:], in0=ga[c % 2][:], in1=WtS[:, :, c], op=ALU.mult), R=[ga[c % 2].b, WtS.b], W=[GT[c % 3].b])

                emitA(0)
                emitA(1)
                emitMid(0)
                for c in range(128):
                    if c + 5 < 128:
                        uload(c + 5)
                    if c + 3 < 128:
                        vload(c + 3)
                    if c + 2 < 128:
                        emitA(c + 2)
                    if c + 1 < 128:
                        emitMid(c + 1)
                    v_ = vc[c % 4]
                    for tt in range(2):
                        for hf in range(2):
                            S.op("pe", lambda e, tt=tt, hf=hf: e.matmul(ps[tt * 2 + hf][:, :], lhsT=GT[c % 3][:, tt * 128:(tt + 1) * 128], rhs=v_[:, hf * 512:(hf + 1) * 512],
                                                                      start=(c == 0), stop=(c == 127), skip_group_check=True),
                                 R=[GT[c % 3].b, v_.b], W=[ps[tt * 2 + hf].b], inc=(tt == 1 and hf == 1))
                    if c >= 2:
                        next(nxt, None)
                        if c % 2 == 0:
                            next(nxt, None)
                for _ in nxt:
                    pass
                for tt in range(2):
                    i = g * 2 + tt
                    y_ = h1e[par][tt]
                    for hf in range(2):
                        pb_ = ps[tt * 2 + hf]
                        S.op("dve", lambda e, hf=hf, pb_=pb_: e.tensor_tensor(out=pb_[:, :], in0=pb_[:, :], in1=gbe[:, hf * 512:(hf + 1) * 512], op=ALU.mult),
                             R=[pb_.b, gbe.b], W=[pb_.b])
                        S.op("dve", lambda e, hf=hf, pb_=pb_: e.tensor_tensor(out=y_[:, hf * 512:(hf + 1) * 512], in0=pb_[:, :], in1=y_[:, hf * 512:(hf + 1) * 512], op=ALU.add),
                             R=[pb_.b, y_.b], W=[y_.b])
                    if last:
                        S.dma(y_d[(i - 2) * 128:(i - 1) * 128, :], y_[:], R=[y_.b])
                    else:
                        S.dma(h_s[i * 128:(i + 1) * 128, :], y_[:], R=[y_.b])
                if g == 0:
                    gate_bcast(gbe, 1, 0)
            S.barrier()
        chk(l, "E")

    S.barrier()
    es.close()
    return nc


def _consts():
    ident = np.eye(128, dtype=np.float32)
    iota = np.tile(np.arange(128, dtype=np.float32)[None, :], (128, 1))
    jj = np.arange(128)[:, None]
    ii = np.arange(128)[None, :]
    tri = np.stack([(ii <= jj), (jj <= ii)]).astype(np.float32)
    sel = np.zeros((2, 256), np.float32)
    sel[0, 0:128] = 1.0
    sel[1, 128:256] = 1.0

    def tables(rot):
        qd = rot // 4
        inv = (np.float32(10000.0) ** (-np.arange(qd, dtype=np.float32) / np.float32(qd))).astype(np.float32)
        t = np.arange(SEQ)
        rows = (t // 64).astype(np.float32)
        cols = (t % 64).astype(np.float32)
        ar = rows[:, None] * inv
        ac = cols[:, None] * inv
        ang = np.concatenate([ar, ar, ac, ac], -1).astype(np.float32)
        cos = np.cos(ang).astype(np.float32)
        sin = np.sin(ang).astype(np.float32)
        sgn = np.concatenate([-np.ones(qd), np.ones(qd), -np.ones(qd), np.ones(qd)]).astype(np.float32)
        cs = np.zeros((T, 2, rot), np.float32)
        cs[:CTX, 0, :] = 1.0
        cs[CTX:, 0, :] = cos
        cs[CTX:, 1, :] = sin * sgn
        return cs.reshape(T, 2 * rot)

    return dict(ident=ident, iota=iota, tri=tri, sel=sel, cs_mla=tables(32), cs_swa=tables(64))


def _in_maps(inp):
    f = lambda a: np.ascontiguousarray(np.asarray(a, dtype=np.float32))
    shared = dict(_consts())
    for k in ("ada_w", "ada_b", "norm1_g", "norm2_g", "w_in", "mla_wuq", "mla_wukv", "mla_qn_g", "mla_kn_g",
              "swa_qn_g", "swa_kn_g", "swa_sink", "w_out", "peer_wq", "peer_v"):
        shared[k] = f(inp[k])
    shared["qa_g_t"] = f(np.asarray(inp["mla_qa_g"]).reshape(L, 3, 128).transpose(0, 2, 1))
    shared["kva_g_t"] = f(np.asarray(inp["mla_kva_g"]).reshape(L, 2, 128).transpose(0, 2, 1))
    shared["keysT"] = f(np.asarray(inp["peer_keys"]).transpose(0, 4, 1, 2, 3).reshape(L, 128, 2048))
    u = np.asarray(inp["peer_u"], dtype=np.float32).reshape(L, 128, 128, 8, 128)
    shared["peer_uT"] = np.ascontiguousarray(u.transpose(0, 1, 4, 3, 2)).reshape(L, 128, 128, 1024)
    x = np.asarray(inp["x"], dtype=np.float32)
    c = np.asarray(inp["c"], dtype=np.float32)
    ctx = np.asarray(inp["ctx"], dtype=np.float32)
    cc = np.asarray(inp["c_ctx"], dtype=np.float32)
    maps = []
    for b in range(8):
        m = dict(shared)
        m["x"] = np.ascontiguousarray(x[b])
        m["ctx"] = np.ascontiguousarray(ctx[b])
        m["cvec"] = np.ascontiguousarray(np.stack([c[b], cc]))
        maps.append(m)
    return maps


def kernel(**inputs):
    nc = build()
    res = run_bass_kernel_spmd(nc, _in_maps(inputs), core_ids=list(range(8)))
    return np.stack([np.asarray(r["y"], dtype=np.float32) for r in res.results], axis=0)
```

```python
import contextlib
import numpy as np
import concourse.bass as bass
import concourse.mybir as mybir
from concourse.bass_utils import run_bass_kernel_spmd

F32, BF16, U32 = mybir.dt.float32, mybir.dt.bfloat16, mybir.dt.uint32
AF = mybir.ActivationFunctionType
ALU = mybir.AluOpType
AX = mybir.AxisListType

L = 2
D = 1024
SEQ = 4096
CTX = 256
T = SEQ + CTX
NT = T // 128
EPS = 1e-6
DIN = 1440
NEG = -1.0e30


class Buf:
    __slots__ = ("w", "r")

    def __init__(self):
        self.w = None
        self.r = {}


class Sched:
    def __init__(self, nc, es):
        self.nc = nc
        self.eng = {"pe": nc.tensor, "act": nc.scalar, "dve": nc.vector, "pool": nc.gpsimd, "sp": nc.sync}
        self.sem = {}
        self.cnt = {}
        for e in ("pe", "act", "dve", "pool"):
            self.sem[e] = es.enter_context(nc.semaphore("s_" + e))
            self.cnt[e] = 0
        self.R = 32
        self.dsem = [es.enter_context(nc.semaphore("d%d" % i)) for i in range(self.R)]
        self.dlast = [None] * self.R
        self.dn = 0
        self.seen = {e: {} for e in self.eng}
        self.pe_pending = False
        self.rec = None
        self.bgsems = [es.enter_context(nc.semaphore("bg%d" % i)) for i in range(8)]
        self.bgn = 0

    def _wait(self, e, tok):
        key, sem, val = tok
        if e == "pe" and key == "pe":
            return
        if self.seen[e].get(key, 0) >= val:
            return
        self.eng[e].wait_ge(sem, val)
        self.seen[e][key] = val

    def _deps(self, e, reads, writes):
        for b in reads:
            if b.w is not None:
                self._wait(e, b.w)
        for b in writes:
            if b.w is not None:
                self._wait(e, b.w)
            for t in list(b.r.values()):
                self._wait(e, t)

    def _commit(self, tok, reads, writes):
        for b in reads:
            b.r[tok[0]] = tok
        for b in writes:
            b.w = tok
            b.r = {}

    def op(self, *a, **k):
        if self.rec is not None:
            self.rec.append(lambda: self._op(*a, **k))
        else:
            self._op(*a, **k)

    def dma(self, *a, **k):
        if self.rec is not None:
            self.rec.append(lambda: self._dma(*a, **k))
        else:
            self._dma(*a, **k)

    def _op(self, e, fn, R=(), W=(), inc=True):
        self._deps(e, R, W)
        inst = fn(self.eng[e])
        if inc:
            self.cnt[e] += 1
            inst.then_inc(self.sem[e], 1)
            tok = (e, self.sem[e], self.cnt[e])
        else:
            assert e == "pe"
            tok = (e, self.sem[e], self.cnt[e] + 1)
        self._commit(tok, R, W)

    def dma_untracked(self, out, in_, q):
        k = self.bgn % len(self.bgsems)
        if self.bgn >= len(self.bgsems):
            self.eng[q].wait_ge(self.bgsems[k], 16 * (self.bgn // len(self.bgsems)))
        inst = self.eng[q].dma_start(out=out, in_=in_)
        self.bgn += 1
        inst.then_inc(self.bgsems[k], 16)

    def _dma(self, out, in_, R=(), W=(), q="sp"):
        i = self.dn % self.R
        if self.dlast[i] is not None:
            self._wait(q, self.dlast[i])
        self._deps(q, R, W)
        inst = self.eng[q].dma_start(out=out, in_=in_)
        val = 16 * (self.dn // self.R + 1)
        inst.then_inc(self.dsem[i], 16)
        tok = ("d%d" % i, self.dsem[i], val)
        self.dlast[i] = tok
        self.dn += 1
        self._commit(tok, R, W)

    def barrier(self, engines=("pe", "act", "dve", "pool", "sp"), bg=False):
        toks = []
        if bg and self.bgn > 0:
            nb_ = len(self.bgsems)
            for k in range(nb_):
                cnt_ = len([x for x in range(self.bgn) if x % nb_ == k])
                if cnt_:
                    toks.append(("bg%d" % k, self.bgsems[k], 16 * cnt_))
        for e in ("pe", "act", "dve", "pool"):
            if self.cnt[e] > 0:
                toks.append((e, self.sem[e], self.cnt[e]))
        for t in self.dlast:
            if t is not None:
                toks.append(t)
        for e in engines:
            for t in toks:
                if not (e == t[0]):
                    self._wait(e, t)
                elif e != "pe":
                    self._wait(e, t)


class TT:
    def __init__(self, t):
        self.t = t
        self.b = Buf()

    def __getitem__(self, k):
        return self.t[k]


class _Stop(Exception):
    pass


_LAST = {}


def build_dbg(debug, stop):
    try:
        return build(debug, stop)
    except _Stop:
        _LAST["es"].close()
        return _LAST["nc"]


def build(debug=None, stop=None):
    nc = bass.Bass("TRN2", target_bir_lowering=False)
    es = contextlib.ExitStack()
    _LAST["nc"] = nc
    _LAST["es"] = es

    def din(name, shape, dt=F32):
        return nc.dram_tensor(name, list(shape), dt, kind="ExternalInput").ap()

    dbg_names = set(debug or [])

    def dscr(name, shape, dt):
        kind = "ExternalOutput" if name in dbg_names else "Internal"
        return nc.dram_tensor(name, list(shape), dt, kind=kind).ap()

    x_d = din("x", [SEQ, D])
    ctx_d = din("ctx", [CTX, D])
    cvec_d = din("cvec", [2, D])
    ada_w = din("ada_w", [L, D, 6 * D])
    ada_b = din("ada_b", [L, 6 * D])
    n1g = din("norm1_g", [L, D])
    n2g = din("norm2_g", [L, D])
    w_in = din("w_in", [L, D, DIN])
    qa_g = din("qa_g_t", [L, 128, 3])
    wuq = din("mla_wuq", [L, 384, 768])
    kva_g = din("kva_g_t", [L, 128, 2])
    wukv = din("mla_wukv", [L, 256, 1024])
    mqn_g = din("mla_qn_g", [L, 96])
    mkn_g = din("mla_kn_g", [L, 96])
    sqn_g = din("swa_qn_g", [L, 64])
    skn_g = din("swa_kn_g", [L, 64])
    sink_d = din("swa_sink", [L, 8])
    w_out = din("w_out", [L, D, D])
    wq_d = din("peer_wq", [L, D, 2048])
    keysT_d = din("keysT", [L, 128, 2048])
    ut_d = din("peer_uT", [L, 128, 128, 1024])
    v_d = din("peer_v", [L, 16384, D])
    ident_d = din("ident", [128, 128])
    iota_d = din("iota", [128, 128])
    tri_d = din("tri", [2, 128, 128])
    sel_d = din("sel", [2, 256])
    csm_d = din("cs_mla", [T, 64])
    css_d = din("cs_swa", [T, 128])
    y_d = nc.dram_tensor("y", [SEQ, D], F32, kind="ExternalOutput").ap()

    h_s = dscr("h_s", [T, D], F32)
    qTm_s = dscr("qTm_s", [8, 96, T], BF16)
    kTm_s = dscr("kTm_s", [8, 96, T], BF16)
    Vm_s = dscr("Vm_s", [T, 520], BF16)
    qTs_s = dscr("qTs_s", [8, 64, T], BF16)
    kTs_s = dscr("kTs_s", [2, 64, T], BF16)
    Vs_s = dscr("Vs_s", [T, 130], BF16)
    om_s = dscr("om_s", [T, D], BF16)
    utb_s = dscr("utb_s", [L, 128, 128, 1024], BF16)
    vb_s = dscr("vb_s", [L, 16384, D], BF16)

    S = Sched(nc, es)

    uid = [0]

    def sb(name, shape, dt, stack=None):
        uid[0] += 1
        return TT((stack or es).enter_context(nc.sbuf_tensor("%s_%d" % (name, uid[0]), list(shape), dt)))

    ps = [TT(es.enter_context(nc.psum_tensor("ps%d" % i, [128, 512], F32))) for i in range(8)]

    def psb(i):
        return ps[i][:].bitcast(BF16)

    ident_f = sb("ident_f", [128, 128], F32)
    ident_b = sb("ident_b", [128, 128], BF16)
    iota_f = sb("iota_f", [128, 128], F32)
    iota_b = sb("iota_b", [128, 128], BF16)
    tri_f = sb("tri_f", [128, 2, 128], F32)
    tri_b = sb("tri_b", [128, 2, 128], BF16)
    sel_f = sb("sel_f", [2, 256], F32)
    epsc = sb("epsc", [128, 1], F32)
    S.dma(ident_f[:], ident_d[:, :], W=[ident_f.b])
    S.dma(iota_f[:], iota_d[:, :], W=[iota_f.b])
    S.dma(tri_f[:], tri_d.rearrange("a p q -> p a q"), W=[tri_f.b])
    S.dma(sel_f[:], sel_d[:, :], W=[sel_f.b])
    S.op("dve", lambda e: e.tensor_copy(ident_b[:], ident_f[:]), R=[ident_f.b], W=[ident_b.b])
    S.op("dve", lambda e: e.tensor_copy(iota_b[:], iota_f[:]), R=[iota_f.b], W=[iota_b.b])
    S.op("dve", lambda e: e.tensor_copy(tri_b[:], tri_f[:]), R=[tri_f.b], W=[tri_b.b])
    S.op("dve", lambda e: e.memset(epsc[:], EPS), W=[epsc.b])

    modT = sb("modT", [128, 4, 8, 2], F32)
    grow = sb("grow", [2, 2 * D], F32)
    sT = sb("sT", [128, 8, 2], F32)
    esink = sb("esink", [128, 8], F32)
    tmp_es = contextlib.ExitStack()
    s2row = sb("s2row", [2, D], F32, tmp_es)

    S.dma(s2row[:], cvec_d[:, :], W=[s2row.b])
    S.op("act", lambda e: e.activation(out=s2row[:], in_=s2row[:], func=AF.Silu), R=[s2row.b], W=[s2row.b])
    for dc in range(8):
        S.op("pe", lambda e, dc=dc: e.transpose(ps[0][:, dc * 2:dc * 2 + 2], s2row[0:2, dc * 128:(dc + 1) * 128], ident_f[0:2, 0:2]),
             R=[s2row.b, ident_f.b], W=[ps[0].b])
    S.op("dve", lambda e: e.tensor_copy(sT[:].rearrange("p a b -> p (a b)"), ps[0][:, 0:16]), R=[ps[0].b], W=[sT.b])
    S.barrier()
    tmp_es.close()

    bg_list = []
    for l in range(L):
        for c in range(0, 128, 8):
            bg_list.append((utb_s[l, c:c + 8].rearrange("c p n -> (c p) n"), ut_d[l, c:c + 8].rearrange("c p n -> (c p) n")))
            bg_list.append((vb_s[l, c * 128:(c + 8) * 128, :], v_d[l, c * 128:(c + 8) * 128, :]))

    def bg_issue(n):
        for _ in range(n):
            if bg_list:
                o_, i_ = bg_list.pop(0)
                S.dma_untracked(o_, i_, "pool")

    def rstd_of(ph, x3, xb, H, Dh, tmp, ssq, rstd):
        tv = tmp[:, 0:H * Dh].rearrange("p (h d) -> p h d", h=H)
        S.op("dve", lambda e: e.tensor_tensor(out=tv, in0=x3, in1=x3, op=ALU.mult), R=[xb], W=[tmp.b])
        S.op("dve", lambda e: e.tensor_reduce(out=ssq[:, 0:H], in_=tv, axis=AX.X, op=ALU.add), R=[tmp.b], W=[ssq.b])
        S.op("act", lambda e: e.activation(out=rstd[:, 0:H], in_=ssq[:, 0:H], func=AF.Sqrt, scale=1.0 / Dh, bias=epsc[:, 0:1]),
             R=[ssq.b, epsc.b], W=[rstd.b])
        S.op("dve", lambda e: e.reciprocal(out=rstd[:, 0:H], in_=rstd[:, 0:H]), R=[rstd.b], W=[rstd.b])

    def rope(x5, xb, cst, cstb, H, q, t1, t2, out5, outb):
        cos4 = cst[:, 0, :].rearrange("p (a b q) -> p a b q", a=2, b=2)
        sin4 = cst[:, 1, :].rearrange("p (a b q) -> p a b q", a=2, b=2)
        n = H * 4 * q
        t1v = t1[:, 0:n].rearrange("p (h a b q) -> p h a b q", h=H, a=2, b=2)
        t2v = t2[:, 0:n].rearrange("p (h a b q) -> p h a b q", h=H, a=2, b=2)
        for b_ in range(2):
            cb = cos4[:, :, b_, :].unsqueeze(1).to_broadcast([128, H, 2, q])
            sbn = sin4[:, :, b_, :].unsqueeze(1).to_broadcast([128, H, 2, q])
            S.op("dve", lambda e, b_=b_, cb=cb: e.tensor_tensor(out=t1v[:, :, :, b_, :], in0=x5[:, :, :, b_, :], in1=cb, op=ALU.mult),
                 R=[xb, cstb], W=[t1.b])
            S.op("dve", lambda e, b_=b_, sbn=sbn: e.tensor_tensor(out=t2v[:, :, :, b_, :], in0=x5[:, :, :, 1 - b_, :], in1=sbn, op=ALU.mult),
                 R=[xb, cstb], W=[t2.b])
        for b_ in range(2):
            S.op("dve", lambda e, b_=b_: e.tensor_tensor(out=out5[:, :, :, b_, :], in0=t1v[:, :, :, b_, :], in1=t2v[:, :, :, b_, :], op=ALU.add),
                 R=[t1.b, t2.b], W=[outb])

    def load_w_bf16(ph, dst2d, src2d, ncols, stg, scale_ap=None, scale_b=None, k=[0]):
        for c0 in range(0, ncols, 2048):
            c1 = min(ncols, c0 + 2048)
            st = stg[k[0] % 2]
            k[0] += 1
            S.dma(st[:, 0:c1 - c0], src2d[:, c0:c1], W=[st.b])
            if scale_ap is None:
                S.op("act", lambda e, st=st, c0=c0, c1=c1: e.activation(out=dst2d[0][:, c0:c1], in_=st[:, 0:c1 - c0], func=AF.Copy),
                     R=[st.b], W=[dst2d[1]])
            else:
                S.op("dve", lambda e, st=st, c0=c0, c1=c1: e.tensor_scalar(out=dst2d[0][:, c0:c1], in0=st[:, 0:c1 - c0], scalar1=scale_ap,
                                                                         scalar2=None, op0=ALU.mult),
                     R=[st.b, scale_b], W=[dst2d[1]])

    def chk(l, phn):
        if stop is not None and stop == (l, phn):
            raise _Stop()

    for l in (range(L) if stop is None else range(stop[0] + 1)):
        last = l == L - 1
        with contextlib.ExitStack() as ph:
            aw = [sb("aw%d" % i, [128, 3072], F32, ph) for i in range(4)]
            modrow = sb("modrow", [2, 6 * D], F32, ph)
            abr = sb("abr", [2, 6 * D], F32, ph)
            ng = sb("ng", [2, 2, D], F32, ph)
            vrow = sb("vrow", [2, 4, D], F32, ph)
            S.dma(abr[:], ada_b[l:l + 1, :].to_broadcast([2, 6 * D]), W=[abr.b])
            S.dma(ng[:, 0, :], n1g[l:l + 1, :].to_broadcast([2, D]), W=[ng.b])
            S.dma(ng[:, 1, :], n2g[l:l + 1, :].to_broadcast([2, D]), W=[ng.b])
            S.dma(esink[:], sink_d[l:l + 1, :].to_broadcast([128, 8]), W=[esink.b])
            S.op("act", lambda e: e.activation(out=esink[:], in_=esink[:], func=AF.Exp), R=[esink.b], W=[esink.b])
            k = 0
            for half in range(2):
                for dc in range(8):
                    a = aw[k % 4]
                    k += 1
                    S.dma(a[:], ada_w[l, dc * 128:(dc + 1) * 128, half * 3072:(half + 1) * 3072], W=[a.b])
                    for cb in range(6):
                        S.op("pe", lambda e, a=a, cb=cb, dc=dc: e.matmul(ps[cb][0:2, :], lhsT=sT[:, dc, :], rhs=a[:, cb * 512:(cb + 1) * 512],
                                                                       start=(dc == 0), stop=(dc == 7)),
                             R=[a.b, sT.b], W=[ps[cb].b])
                for cb in range(6):
                    c0 = half * 3072 + cb * 512
                    S.op("dve", lambda e, cb=cb, c0=c0: e.tensor_tensor(out=modrow[:, c0:c0 + 512], in0=ps[cb][0:2, :], in1=abr[:, c0:c0 + 512], op=ALU.add),
                         R=[ps[cb].b, abr.b], W=[modrow.b])
            for j, (sci, shi) in enumerate(((1, 0), (4, 3))):
                S.op("dve", lambda e, j=j, sci=sci: e.scalar_tensor_tensor(out=vrow[:, 2 * j, :], in0=modrow[:, sci * D:(sci + 1) * D], scalar=1.0,
                                                                          in1=ng[:, j, :], op0=ALU.add, op1=ALU.mult),
                     R=[modrow.b, ng.b], W=[vrow.b])
                S.op("dve", lambda e, j=j, shi=shi: e.tensor_copy(vrow[:, 2 * j + 1, :], modrow[:, shi * D:(shi + 1) * D]),
                     R=[modrow.b], W=[vrow.b])
            S.op("dve", lambda e: e.tensor_copy(grow[:, 0:D], modrow[:, 2 * D:3 * D]), R=[modrow.b], W=[grow.b])
            S.op("dve", lambda e: e.tensor_copy(grow[:, D:2 * D], modrow[:, 5 * D:6 * D]), R=[modrow.b], W=[grow.b])
            for v in range(4):
                for dc in range(8):
                    o = (v * 8 + dc) * 2
                    S.op("pe", lambda e, v=v, dc=dc, o=o: e.transpose(ps[6][:, o:o + 2], vrow[0:2, v, dc * 128:(dc + 1) * 128], ident_f[0:2, 0:2]),
                         R=[vrow.b, ident_f.b], W=[ps[6].b])
            S.op("dve", lambda e: e.tensor_copy(modT[:].rearrange("p a b c -> p (a b c)"), ps[6][:, 0:64]), R=[ps[6].b], W=[modT.b])
            S.barrier()
        chk(l, "A")

        def gate_bcast(dst, gi, j):
            for hf in range(2):
                S.op("pe", lambda e, hf=hf: e.matmul(ps[6 + hf][:, :], lhsT=sel_f[0:2, j * 128:(j + 1) * 128],
                                                    rhs=grow[0:2, gi * D + hf * 512: gi * D + (hf + 1) * 512], start=True, stop=True),
                     R=[sel_f.b, grow.b], W=[ps[6 + hf].b])
                S.op("act", lambda e, hf=hf: e.activation(out=dst[:, hf * 512:(hf + 1) * 512], in_=ps[6 + hf][:, :], func=AF.Copy),
                     R=[ps[6 + hf].b], W=[dst.b])

        def modulate_T(ht, xn, sqj, ssq, rstd, aT_ap, aT_b, vA, j, psbank):
            rstd_of(None, ht[:].rearrange("p (h d) -> p h d", h=1), ht.b, 1, D, sqj, ssq, rstd)
            S.op("dve", lambda e: e.tensor_scalar(out=xn[:], in0=ht[:], scalar1=rstd[:, 0:1], scalar2=None, op0=ALU.mult),
                 R=[ht.b, rstd.b], W=[xn.b])
            pv = psb(psbank)
            for dc in range(8):
                S.op("pe", lambda e, dc=dc: e.transpose(pv[:, dc * 128:(dc + 1) * 128], xn[:, dc * 128:(dc + 1) * 128], ident_b[:]),
                     R=[xn.b, ident_b.b], W=[ps[psbank].b])
            for dc in range(8):
                S.op("dve", lambda e, dc=dc: e.tensor_scalar(out=aT_ap[:, dc, :], in0=pv[:, dc * 128:(dc + 1) * 128],
                                                            scalar1=modT[:, vA, dc, j:j + 1], scalar2=modT[:, vA + 1, dc, j:j + 1],
                                                            op0=ALU.mult, op1=ALU.add),
                     R=[ps[psbank].b, modT.b], W=[aT_b])

        with contextlib.ExitStack() as ph:
            stg = [sb("stg%d" % i, [128, 2048], F32, ph) for i in range(2)]
            w_in_sb = sb("w_in_sb", [128, 8, DIN], BF16, ph)
            wuq_sb = sb("wuq_sb", [128, 3, 768], BF16, ph)
            wukv_sb = sb("wukv_sb", [128, 2, 1024], BF16, ph)
            gq = sb("gq", [128, 3], F32, ph)
            gkv = sb("gkv", [128, 2], F32, ph)
            g_mq = sb("g_mq", [128, 96], F32, ph)
            g_mk = sb("g_mk", [128, 96], F32, ph)
            g_sq = sb("g_sq", [128, 64], F32, ph)
            g_sk = sb("g_sk", [128, 64], F32, ph)
            S.dma(gq[:], qa_g[l], W=[gq.b])
            S.dma(gkv[:], kva_g[l], W=[gkv.b])
            S.dma(g_mq[:], mqn_g[l:l + 1, :].to_broadcast([128, 96]), W=[g_mq.b])
            S.dma(g_mk[:], mkn_g[l:l + 1, :].to_broadcast([128, 96]), W=[g_mk.b])
            S.dma(g_sq[:], sqn_g[l:l + 1, :].to_broadcast([128, 64]), W=[g_sq.b])
            S.dma(g_sk[:], skn_g[l:l + 1, :].to_broadcast([128, 64]), W=[g_sk.b])
            for dc in range(8):
                load_w_bf16(ph, (w_in_sb[:, dc, :], w_in_sb.b), w_in[l, dc * 128:(dc + 1) * 128, :], DIN, stg)
            for kc in range(3):
                load_w_bf16(ph, (wuq_sb[:, kc, :], wuq_sb.b), wuq[l, kc * 128:(kc + 1) * 128, :], 768, stg, gq[:, kc:kc + 1], gq.b)
            for kc in range(2):
                load_w_bf16(ph, (wukv_sb[:, kc, :], wukv_sb.b), wukv[l, kc * 128:(kc + 1) * 128, :], 1024, stg, gkv[:, kc:kc + 1], gkv.b)

            ht = [sb("ht%d" % i, [128, D], F32, ph) for i in range(2)]
            cstm = [sb("cstm%d" % i, [128, 2, 32], F32, ph) for i in range(2)]
            csts = [sb("csts%d" % i, [128, 2, 64], F32, ph) for i in range(2)]
            def mkset(par):
                d_ = {}
                d_["sqj"] = sb("sqj%d" % par, [128, D], F32, ph)
                d_["t1"] = sb("t1%d" % par, [128, D], F32, ph)
                d_["t2"] = sb("t2%d" % par, [128, D], F32, ph)
                d_["xn"] = sb("xn%d" % par, [128, D], BF16, ph)
                d_["aT"] = sb("aT%d" % par, [128, 8, 128], BF16, ph)
                d_["p_sb"] = sb("p_sb%d" % par, [128, DIN], F32, ph)
                d_["qan"] = sb("qan%d" % par, [128, 384], BF16, ph)
                d_["kvan"] = sb("kvan%d" % par, [128, 256], BF16, ph)
                d_["lT"] = sb("lT%d" % par, [128, 5, 128], BF16, ph)
                d_["qm"] = sb("qm%d" % par, [128, 8, 96], F32, ph)
                d_["kv"] = sb("kv%d" % par, [128, 8, 128], F32, ph)
                d_["kf"] = sb("kf%d" % par, [128, 8, 96], F32, ph)
                d_["qn"] = sb("qn%d" % par, [128, 8, 96], F32, ph)
                d_["kn"] = sb("kn%d" % par, [128, 8, 96], F32, ph)
                d_["sqn"] = sb("sqn%d" % par, [128, 8, 64], F32, ph)
                d_["skn"] = sb("skn%d" % par, [128, 2, 64], F32, ph)
                d_["qo"] = sb("qo%d" % par, [128, 8, 96], BF16, ph)
                d_["ko"] = sb("ko%d" % par, [128, 8, 96], BF16, ph)
                d_["sqo"] = sb("sqo%d" % par, [128, 8, 64], BF16, ph)
                d_["sko"] = sb("sko%d" % par, [128, 2, 64], BF16, ph)
                d_["qT_sb"] = sb("qT_sb%d" % par, [128, 8, 128], BF16, ph)
                d_["kT_sb"] = sb("kT_sb%d" % par, [128, 8, 128], BF16, ph)
                d_["sqT_sb"] = sb("sqT_sb%d" % par, [128, 8, 128], BF16, ph)
                d_["skT_sb"] = sb("skT_sb%d" % par, [128, 2, 128], BF16, ph)
                return d_

            BS = [mkset(0), mkset(1)]
            Vm_sb = [sb("Vm_sb%d" % i, [128, 8, 65], BF16, ph) for i in range(2)]
            Vs_sb = [sb("Vs_sb%d" % i, [128, 2, 65], BF16, ph) for i in range(2)]
            for par_ in range(2):
                BS[par_]["ssq"] = sb("ssq%d" % par_, [128, 16], F32, ph)
                BS[par_]["rstd"] = sb("rstd%d" % par_, [128, 16], F32, ph)
            for i in range(2):
                S.op("pool", lambda e, i=i: e.memset(Vm_sb[i][:], 1.0), W=[Vm_sb[i].b])
                S.op("pool", lambda e, i=i: e.memset(Vs_sb[i][:], 1.0), W=[Vs_sb[i].b])

            def loads(i):
                hb = ht[i % 2]
                if l == 0:
                    src = ctx_d[i * 128:(i + 1) * 128, :] if i < 2 else x_d[(i - 2) * 128:(i - 1) * 128, :]
                else:
                    src = h_s[i * 128:(i + 1) * 128, :]
                S.dma(hb[:], src, W=[hb.b])
                S.dma(cstm[i % 2][:].rearrange("p a b -> p (a b)"), csm_d[i * 128:(i + 1) * 128, :], W=[cstm[i % 2].b])
                S.dma(csts[i % 2][:].rearrange("p a b -> p (a b)"), css_d[i * 128:(i + 1) * 128, :], W=[csts[i % 2].b])

            def tile_body(i, par):
                d_ = BS[par]
                sqj, t1, t2, xn, aT, p_sb, qan, kvan, lT, qm, kv, kf, qn, kn, sqn, skn, qo, ko, sqo, sko, qT_sb, kT_sb, sqT_sb, skT_sb, ssq, rstd = d_["sqj"], d_["t1"], d_["t2"], d_["xn"], d_["aT"], d_["p_sb"], d_["qan"], d_["kvan"], d_["lT"], d_["qm"], d_["kv"], d_["kf"], d_["qn"], d_["kn"], d_["sqn"], d_["skn"], d_["qo"], d_["ko"], d_["sqo"], d_["sko"], d_["qT_sb"], d_["kT_sb"], d_["sqT_sb"], d_["skT_sb"], d_["ssq"], d_["rstd"]
                ps_ = ps[4 * par:] + ps[:4 * par]
                psb_ = lambda k_: psb((k_ + 4 * par) % 8)
                j = 1 if i < 2 else 0
                hb = ht[i % 2]
                cm = cstm[i % 2]
                cs_ = csts[i % 2]
                tsl = slice(i * 128, (i + 1) * 128)
                modulate_T(hb, xn, sqj, ssq, rstd, aT[:], aT.b, 0, j, (4 * par) % 8)
                for cb, (c0, c1) in enumerate(((0, 512), (512, 1024), (1024, DIN))):
                    for dc in range(8):
                        S.op("pe", lambda e, cb=cb, c0=c0, c1=c1, dc=dc: e.matmul(ps_[1 + cb][:, 0:c1 - c0], lhsT=aT[:, dc, :], rhs=w_in_sb[:, dc, c0:c1],
                                                                              start=(dc == 0), stop=(dc == 7)),
                             R=[aT.b, w_in_sb.b], W=[ps_[1 + cb].b], inc=(dc == 7))
                    S.op("act", lambda e, cb=cb, c0=c0, c1=c1: e.activation(out=p_sb[:, c0:c1], in_=ps_[1 + cb][:, 0:c1 - c0], func=AF.Copy),
                         R=[ps_[1 + cb].b], W=[p_sb.b])
                rstd_of(ph, p_sb[:, 0:384].rearrange("p (h d) -> p h d", h=1), p_sb.b, 1, 384, sqj, ssq, rstd)
                S.op("dve", lambda e: e.tensor_scalar(out=qan[:], in0=p_sb[:, 0:384], scalar1=rstd[:, 0:1], scalar2=None, op0=ALU.mult),
                     R=[p_sb.b, rstd.b], W=[qan.b])
                rstd_of(ph, p_sb[:, 896:1152].rearrange("p (h d) -> p h d", h=1), p_sb.b, 1, 256, sqj, ssq, rstd)
                S.op("dve", lambda e: e.tensor_scalar(out=kvan[:], in0=p_sb[:, 896:1152], scalar1=rstd[:, 0:1], scalar2=None, op0=ALU.mult),
                     R=[p_sb.b, rstd.b], W=[kvan.b])
                pv4 = psb_(4)
                for kc in range(3):
                    S.op("pe", lambda e, kc=kc: e.transpose(pv4[:, kc * 128:(kc + 1) * 128], qan[:, kc * 128:(kc + 1) * 128], ident_b[:]),
                         R=[qan.b, ident_b.b], W=[ps_[4].b])
                for kc in range(2):
                    S.op("pe", lambda e, kc=kc: e.transpose(pv4[:, (3 + kc) * 128:(4 + kc) * 128], kvan[:, kc * 128:(kc + 1) * 128], ident_b[:]),
                         R=[kvan.b, ident_b.b], W=[ps_[4].b])
                S.op("act", lambda e: e.activation(out=lT[:].rearrange("p a b -> p (a b)"), in_=pv4[:, 0:640], func=AF.Copy), R=[ps_[4].b], W=[lT.b])
                for cb, (c0, c1) in enumerate(((0, 512), (512, 768))):
                    for kc in range(3):
                        S.op("pe", lambda e, cb=cb, c0=c0, c1=c1, kc=kc: e.matmul(ps_[5 + cb][:, 0:c1 - c0], lhsT=lT[:, kc, :], rhs=wuq_sb[:, kc, c0:c1],
                                                                              start=(kc == 0), stop=(kc == 2)),
                             R=[lT.b, wuq_sb.b], W=[ps_[5 + cb].b], inc=(kc == 2))
                    S.op("act", lambda e, cb=cb, c0=c0, c1=c1: e.activation(out=qm[:].rearrange("p a b -> p (a b)")[:, c0:c1], in_=ps_[5 + cb][:, 0:c1 - c0], func=AF.Copy),
                         R=[ps_[5 + cb].b], W=[qm.b])
                for cb in range(2):
                    for kc in range(2):
                        S.op("pe", lambda e, cb=cb, kc=kc: e.matmul(ps_[1 + cb][:, :], lhsT=lT[:, 3 + kc, :], rhs=wukv_sb[:, kc, cb * 512:(cb + 1) * 512],
                                                                start=(kc == 0), stop=(kc == 1)),
                             R=[lT.b, wukv_sb.b], W=[ps_[1 + cb].b], inc=(kc == 1))
                    S.op("act", lambda e, cb=cb: e.activation(out=kv[:].rearrange("p a b -> p (a b)")[:, cb * 512:(cb + 1) * 512], in_=ps_[1 + cb][:, :], func=AF.Copy),
                         R=[ps_[1 + cb].b], W=[kv.b])
                rstd_of(ph, qm[:], qm.b, 8, 96, sqj, ssq, rstd)
                S.op("dve", lambda e: e.tensor_tensor(out=qn[:], in0=qm[:], in1=rstd[:, 0:8].unsqueeze(2).to_broadcast([128, 8, 96]), op=ALU.mult),
                     R=[qm.b, rstd.b], W=[qn.b])
                S.op("dve", lambda e: e.tensor_tensor(out=qn[:], in0=qn[:], in1=g_mq[:].unsqueeze(1).to_broadcast([128, 8, 96]), op=ALU.mult),
                     R=[qn.b, g_mq.b], W=[qn.b])
                S.op("dve", lambda e: e.tensor_copy(qo[:, :, 0:64], qn[:, :, 0:64]), R=[qn.b], W=[qo.b])
                rope(qn[:, :, 64:96].rearrange("p h (a b q) -> p h a b q", a=2, b=2), qn.b, cm, cm.b, 8, 8, t1, t2,
                     qo[:, :, 64:96].rearrange("p h (a b q) -> p h a b q", a=2, b=2), qo.b)
                S.op("dve", lambda e: e.tensor_copy(kf[:, :, 0:64], kv[:, :, 0:64]), R=[kv.b], W=[kf.b])
                S.op("dve", lambda e: e.tensor_copy(kf[:, :, 64:96], p_sb[:, 1152:1184].unsqueeze(1).to_broadcast([128, 8, 32])), R=[p_sb.b], W=[kf.b])
                rstd_of(ph, kf[:], kf.b, 8, 96, sqj, ssq, rstd)
                S.op("dve", lambda e: e.tensor_tensor(out=kn[:], in0=kf[:], in1=rstd[:, 0:8].unsqueeze(2).to_broadcast([128, 8, 96]), op=ALU.mult),
                     R=[kf.b, rstd.b], W=[kn.b])
                S.op("dve", lambda e: e.tensor_tensor(out=kn[:], in0=kn[:], in1=g_mk[:].unsqueeze(1).to_broadcast([128, 8, 96]), op=ALU.mult),
                     R=[kn.b, g_mk.b], W=[kn.b])
                S.op("dve", lambda e: e.tensor_copy(ko[:, :, 0:64], kn[:, :, 0:64]), R=[kn.b], W=[ko.b])
                rope(kn[:, :, 64:96].rearrange("p h (a b q) -> p h a b q", a=2, b=2), kn.b, cm, cm.b, 8, 8, t1, t2,
                     ko[:, :, 64:96].rearrange("p h (a b q) -> p h a b q", a=2, b=2), ko.b)
                vmb = Vm_sb[i % 2]
                S.op("act", lambda e, vmb=vmb: e.activation(out=vmb[:, :, 0:64], in_=kv[:, :, 64:128], func=AF.Copy), R=[kv.b], W=[vmb.b])
                S.dma(Vm_s[tsl, :], vmb[:].rearrange("p a b -> p (a b)"), R=[vmb.b])
                sq3 = p_sb[:, 384:896].rearrange("p (h d) -> p h d", h=8)
                rstd_of(ph, sq3, p_sb.b, 8, 64, sqj, ssq, rstd)
                S.op("dve", lambda e: e.tensor_tensor(out=sqn[:], in0=sq3, in1=rstd[:, 0:8].unsqueeze(2).to_broadcast([128, 8, 64]), op=ALU.mult),
                     R=[p_sb.b, rstd.b], W=[sqn.b])
                S.op("dve", lambda e: e.tensor_tensor(out=sqn[:], in0=sqn[:], in1=g_sq[:].unsqueeze(1).to_broadcast([128, 8, 64]), op=ALU.mult),
                     R=[sqn.b, g_sq.b], W=[sqn.b])
                rope(sqn[:].rearrange("p h (a b q) -> p h a b q", a=2, b=2), sqn.b, cs_, cs_.b, 8, 16, t1, t2,
                     sqo[:].rearrange("p h (a b q) -> p h a b q", a=2, b=2), sqo.b)
                sk3 = p_sb[:, 1184:1312].rearrange("p (h d) -> p h d", h=2)
                rstd_of(ph, sk3, p_sb.b, 2, 64, sqj, ssq, rstd)
                S.op("dve", lambda e: e.tensor_tensor(out=skn[:], in0=sk3, in1=rstd[:, 0:2].unsqueeze(2).to_broadcast([128, 2, 64]), op=ALU.mult),
                     R=[p_sb.b, rstd.b], W=[skn.b])
                S.op("dve", lambda e: e.tensor_tensor(out=skn[:], in0=skn[:], in1=g_sk[:].unsqueeze(1).to_broadcast([128, 2, 64]), op=ALU.mult),
                     R=[skn.b, g_sk.b], W=[skn.b])
                rope(skn[:].rearrange("p h (a b q) -> p h a b q", a=2, b=2), skn.b, cs_, cs_.b, 2, 16, t1, t2,
                     sko[:].rearrange("p h (a b q) -> p h a b q", a=2, b=2), sko.b)
                vsb = Vs_sb[i % 2]
                S.op("act", lambda e, vsb=vsb: e.activation(out=vsb[:, :, 0:64], in_=p_sb[:, 1312:1440].rearrange("p (h d) -> p h d", h=2), func=AF.Copy),
                     R=[p_sb.b], W=[vsb.b])
                S.dma(Vs_s[tsl, :], vsb[:].rearrange("p a b -> p (a b)"), R=[vsb.b])
                for (src, dstT, nh, dh, bank, scr) in ((qo, qT_sb, 8, 96, 7, qTm_s), (ko, kT_sb, 8, 96, 0, kTm_s),
                                                       (sqo, sqT_sb, 8, 64, 4, qTs_s), (sko, skT_sb, 2, 64, 3, kTs_s)):
                    pvv = psb_(bank)
                    for h in range(nh):
                        S.op("pe", lambda e, src=src, h=h, dh=dh, pvv=pvv: e.transpose(pvv[0:dh, h * 128:(h + 1) * 128], src[:, h, :], ident_b[:]),
                             R=[src.b, ident_b.b], W=[ps_[bank].b])
                    S.op("act", lambda e, dstT=dstT, nh=nh, dh=dh, pvv=pvv: e.activation(out=dstT[0:dh, 0:nh, :].rearrange("p a b -> p (a b)"),
                                                                                       in_=pvv[0:dh, 0:nh * 128], func=AF.Copy),
                         R=[ps_[bank].b], W=[dstT.b])
                    S.dma(scr[:, :, tsl].rearrange("h d t -> d h t"), dstT[0:dh, 0:nh, :], R=[dstT.b])
                if i + 2 < NT:
                    loads(i + 2)

            loads(0)
            loads(1)
            for pr in range(NT // 2):
                bg_issue(2)
                recs = []
                for par in range(2):
                    S.rec = []
                    tile_body(2 * pr + par, par)
                    recs.append(S.rec)
                    S.rec = None
                for n_ in range(max(len(recs[0]), len(recs[1]))):
                    for par in range(2):
                        if n_ < len(recs[par]):
                            recs[par][n_]()
            S.barrier()
        chk(l, "B")

        with contextlib.ExitStack() as ph:
            kT_all = [sb("kTa%d" % h, [128, T], BF16, ph) for h in range(8)]
            Vall = sb("Vall", [128, NT, 520], BF16, ph)
            kTs_all = sb("kTs_all", [64, 2, T], BF16, ph)
            Vs_all = sb("Vs_all", [128, NT, 130], BF16, ph)
            qTg = [sb("qTg%d" % i, [128, 8, 512], BF16, ph) for i in range(2)]
            sqTg = [sb("sqTg%d" % i, [64, 8, 512], BF16, ph) for i in range(2)]
            PT = [sb("PT%d" % i, [128, 512], BF16, ph) for i in range(4)]
            obuf = [sb("obuf0", [128, 4, D], BF16, ph)]
            rden = sb("rden", [128, 4], F32, ph)
            for h in range(8):
                S.dma(kT_all[h][0:96, :], kTm_s[h], W=[kT_all[h].b])
            S.dma(Vall[:], Vm_s.rearrange("(k p) e -> p k e", p=128), W=[Vall.b])
            S.dma(kTs_all[:], kTs_s.rearrange("g d t -> d g t"), W=[kTs_all.b])
            S.dma(Vs_all[:], Vs_s.rearrange("(k p) e -> p k e", p=128), W=[Vs_all.b])
            groups = ([] if last else [(0, 256)]) + [(256 + 512 * g, 512) for g in range(8)]
            ctr = {"s": 0, "p": 0, "o": 0}

            def qloads(gi):
                t0, nq = groups[gi]
                S.dma(qTg[gi % 2][0:96, :, 0:nq], qTm_s[:, :, t0:t0 + nq].rearrange("h d t -> d h t"), W=[qTg[gi % 2].b])
                S.dma(sqTg[gi % 2][:, :, 0:nq], qTs_s[:, :, t0:t0 + nq].rearrange("h d t -> d h t"), W=[sqTg[gi % 2].b])

            qloads(0)
            for gi, (t0, nq) in enumerate(groups):
                bg_issue(4)
                if gi + 1 < len(groups):
                    qloads(gi + 1)
                isctx = t0 < 256
                nblk = nq // 128
                qg = qTg[gi % 2]
                sg = sqTg[gi % 2]
                ob = obuf[0]
                kts = [0, 1] if isctx else list(range(NT))
                its = []
                for h in range(8):
                    for ki, kt in enumerate(kts):
                        its.append(("m", h, ki, kt, len(kts), None, None))
                for blk in range(nblk):
                    ti = t0 // 128 + blk
                    if isctx:
                        kl = [0, 1]
                    else:
                        kl = [0, 1] + ([ti - 1] if ti - 1 >= 2 else []) + [ti] + ([ti + 1] if ti + 1 < NT else [])
                    for g2 in range(2):
                        for ki, kt in enumerate(kl):
                            its.append(("s", g2, ki, kt, len(kl), blk, ti))

                def emitS(n):
                    kind, a, ki, kt, nk, blk, ti = its[n]
                    pS = ps[n % 4]
                    if kind == "m":
                        S.op("pe", lambda e: e.matmul(pS[:, 0:nq], lhsT=kT_all[a][0:96, kt * 128:(kt + 1) * 128], rhs=qg[0:96, a, 0:nq], start=True, stop=True),
                             R=[kT_all[a].b, qg.b], W=[pS.b])
                    else:
                        S.op("pe", lambda e: e.matmul(pS[:, :].rearrange("p (a b) -> p a b", a=4), lhsT=kTs_all[:, a, kt * 128:(kt + 1) * 128],
                                                      rhs=sg[:, 4 * a:4 * a + 4, blk * 128:(blk + 1) * 128], start=True, stop=True),
                             R=[kTs_all.b, sg.b], W=[pS.b])

                po_of = {}

                def emitRest(n):
                    kind, a, ki, kt, nk, blk, ti = its[n]
                    pS = ps[n % 4]
                    pt = PT[n % 4]
                    if ki == 0:
                        po_of["cur"] = ps[4 + ctr["o"] % 2]
                        ctr["o"] += 1
                    po = po_of["cur"]
                    if kind == "m":
                        S.op("act", lambda e: e.activation(out=pt[:, 0:nq], in_=pS[:, 0:nq], func=AF.Exp, scale=96.0 ** -0.5), R=[pS.b], W=[pt.b])
                        for qs in range(nblk):
                            S.op("pe", lambda e, qs=qs: e.matmul(po[:, qs * 65:(qs + 1) * 65], lhsT=pt[:, qs * 128:(qs + 1) * 128], rhs=Vall[:, kt, a * 65:(a + 1) * 65],
                                                                start=(ki == 0 and qs == 0), stop=(ki == nk - 1), skip_group_check=True),
                                 R=[pt.b, Vall.b], W=[po.b], inc=(qs == nblk - 1))
                        if ki == nk - 1:
                            po3 = po[:, 0:nblk * 65].rearrange("p (a b) -> p a b", b=65)
                            S.op("dve", lambda e: e.reciprocal(out=rden[:, 0:nblk], in_=po3[:, :, 64]), R=[po.b], W=[rden.b])
                            S.op("dve", lambda e: e.tensor_tensor(out=ob[:, 0:nblk, a * 64:(a + 1) * 64], in0=po3[:, :, 0:64],
                                                                  in1=rden[:, 0:nblk].unsqueeze(2).to_broadcast([128, nblk, 64]), op=ALU.mult),
                                 R=[po.b, rden.b], W=[ob.b])
                    else:
                        S.op("act", lambda e: e.activation(out=pt[:, :], in_=pS[:, :], func=AF.Exp, scale=0.125), R=[pS.b], W=[pt.b])
                        if (not isctx) and kt >= 2 and kt != ti:
                            mi = 0 if kt == ti - 1 else 1
                            S.op("pool", lambda e: e.tensor_tensor(out=pt[:, :].rearrange("p (a b) -> p a b", a=4), in0=pt[:, :].rearrange("p (a b) -> p a b", a=4),
                                                                   in1=tri_b[:, mi, :].unsqueeze(1).to_broadcast([128, 4, 128]), op=ALU.mult),
                                 R=[pt.b, tri_b.b], W=[pt.b])
                        for r in range(4):
                            S.op("pe", lambda e, r=r: e.matmul(po[:, r * 65:(r + 1) * 65], lhsT=pt[:, r * 128:(r + 1) * 128], rhs=Vs_all[:, kt, a * 65:(a + 1) * 65],
                                                              start=(ki == 0 and r == 0), stop=(ki == nk - 1), skip_group_check=True),
                                 R=[pt.b, Vs_all.b], W=[po.b], inc=(r == 3))
                        if ki == nk - 1:
                            po3 = po[:, 0:260].rearrange("p (a b) -> p a b", b=65)
                            S.op("dve", lambda e: e.tensor_tensor(out=rden[:, 0:4], in0=po3[:, :, 64], in1=esink[:, 4 * a:4 * a + 4], op=ALU.add),
                                 R=[po.b, esink.b], W=[rden.b])
                            S.op("dve", lambda e: e.reciprocal(out=rden[:, 0:4], in_=rden[:, 0:4]), R=[rden.b], W=[rden.b])
                            S.op("dve", lambda e: e.tensor_tensor(out=ob[:, blk, 512 + a * 256:512 + (a + 1) * 256].rearrange("p (a b) -> p a b", a=4), in0=po3[:, :, 0:64],
                                                                  in1=rden[:, 0:4].unsqueeze(2).to_broadcast([128, 4, 64]), op=ALU.mult),
                                 R=[po.b, rden.b], W=[ob.b])

                LA = 3
                for n in range(min(LA, len(its))):
                    emitS(n)
                for n in range(len(its)):
                    if n + LA < len(its):
                        emitS(n + LA)
                    emitRest(n)
                S.dma(om_s[t0:t0 + nq, :].rearrange("(b p) c -> p b c", p=128), ob[:, 0:nblk, :], R=[ob.b])
            S.barrier()
        chk(l, "C")

        with contextlib.ExitStack() as ph:
            stg = [sb("stgd%d" % i, [128, 2048], F32, ph) for i in range(2)]
            w_out_sb = sb("w_out_sb", [128, 8, D], BF16, ph)
            for kc in range(8):
                load_w_bf16(ph, (w_out_sb[:, kc, :], w_out_sb.b), w_out[l, kc * 128:(kc + 1) * 128, :], D, stg)
            gb = [sb("gbd%d" % j, [128, D], F32, ph) for j in range(2)]
            gate_bcast(gb[0], 0, 0)
            gate_bcast(gb[1], 0, 1)
            htd = [sb("htd%d" % i, [128, D], F32, ph) for i in range(2)]
            omb = [sb("omb%d" % i, [128, D], BF16, ph) for i in range(2)]
            oT = sb("oT", [128, 8, 128], BF16, ph)
            h1 = [sb("h1d%d" % i, [128, D], F32, ph) for i in range(2)]
            tiles = list(range(2 if last else 0, NT))

            def loadsD(i):
                if l == 0:
                    src = ctx_d[i * 128:(i + 1) * 128, :] if i < 2 else x_d[(i - 2) * 128:(i - 1) * 128, :]
                else:
                    src = h_s[i * 128:(i + 1) * 128, :]
                S.dma(htd[i % 2][:], src, W=[htd[i % 2].b])
                S.dma(omb[i % 2][:], om_s[i * 128:(i + 1) * 128, :], W=[omb[i % 2].b])

            loadsD(tiles[0])
            for n_, i in enumerate(tiles):
                if n_ + 1 < len(tiles):
                    loadsD(tiles[n_ + 1])
                j = 1 if i < 2 else 0
                pv = psb(0)
                for kc in range(8):
                    S.op("pe", lambda e, kc=kc, i=i: e.transpose(pv[:, kc * 128:(kc + 1) * 128], omb[i % 2][:, kc * 128:(kc + 1) * 128], ident_b[:]),
                         R=[omb[i % 2].b, ident_b.b], W=[ps[0].b])
                S.op("act", lambda e: e.activation(out=oT[:].rearrange("p a b -> p (a b)"), in_=pv[:, :], func=AF.Copy), R=[ps[0].b], W=[oT.b])
                hh = h1[i % 2]
                for hf in range(2):
                    for kc in range(8):
                        S.op("pe", lambda e, hf=hf, kc=kc: e.matmul(ps[1 + hf][:, :], lhsT=oT[:, kc, :], rhs=w_out_sb[:, kc, hf * 512:(hf + 1) * 512],
                                                                start=(kc == 0), stop=(kc == 7)),
                             R=[oT.b, w_out_sb.b], W=[ps[1 + hf].b], inc=(kc == 7))
                    S.op("dve", lambda e, hf=hf, j=j, hh=hh: e.tensor_tensor(out=hh[:, hf * 512:(hf + 1) * 512], in0=ps[1 + hf][:, :],
                                                                          in1=gb[j][:, hf * 512:(hf + 1) * 512], op=ALU.mult),
                         R=[ps[1 + hf].b, gb[j].b], W=[hh.b])
                S.op("pool", lambda e, hh=hh, i=i: e.tensor_tensor(out=hh[:], in0=hh[:], in1=htd[i % 2][:], op=ALU.add), R=[hh.b, htd[i % 2].b], W=[hh.b])
                S.dma(h_s[i * 128:(i + 1) * 128, :], hh[:], R=[hh.b])
            bg_issue(len(bg_list))
            S.barrier(bg=True)
        chk(l, "D")

        with contextlib.ExitStack() as ph:
            s_sb = sb("s_sb", [128, 2048], F32, ph)
            cand = sb("cand", [128, 2048], F32, ph)
            stg = [s_sb, cand]
            wq_sb = sb("wq_sb", [128, 8, 2048], BF16, ph)
            keysT_sb = sb("keysT_sb", [128, 2048], BF16, ph)
            for dc in range(8):
                load_w_bf16(ph, (wq_sb[:, dc, :], wq_sb.b), wq_d[l, dc * 128:(dc + 1) * 128, :], 2048, stg)
            load_w_bf16(ph, (keysT_sb[:, :], keysT_sb.b), keysT_d[l], 2048, stg)
            gbe = sb("gbe", [128, D], F32, ph)
            gate_bcast(gbe, 1, 0 if last else 1)
            WtS = sb("WtS", [128, 256, 128], BF16, ph)
            Ab = [sb("Ab%d" % i, [128, 8, 128], BF16, ph) for i in range(2)]
            Bb = [sb("Bb%d" % i, [128, 8, 128], BF16, ph) for i in range(2)]
            utc = [sb("utc%d" % i, [128, 8, 128], BF16, ph) for i in range(6)]
            vc = [sb("vc%d" % i, [128, D], BF16, ph) for i in range(4)]
            h1e = [[sb("h1e%d_%d" % (p_, i), [128, D], F32, ph) for i in range(2)] for p_ in range(2)]
            xn = sb("xne", [128, D], BF16, ph)
            bT = [sb("bT%d" % p_, [128, 8, 256], BF16, ph) for p_ in range(2)]
            qTe = sb("qTe", [128, 16, 256], BF16, ph)
            sv = sb("sv", [128, 16, 16], F32, ph)
            si_u = sb("si_u", [128, 16, 16], U32, ph)
            si_f = sb("si_f", [128, 16, 16], F32, ph)
            cv = sb("cv", [128, 8, 16], F32, ph)
            ci_u = sb("ci_u", [128, 8, 16], U32, ph)
            k_u = sb("k_u", [128, 2, 128], U32, ph)
            k_f = sb("k_f", [128, 2, 128], F32, ph)
            IG = sb("IG", [128, 3, 128], F32, ph)
            IGT = [sb("IGT%d" % p_, [128, 3, 256], BF16, ph) for p_ in range(2)]
            gsum = sb("gsum", [128, 8], F32, ph)
            ssq = sb("ssqe", [128, 16], F32, ph)
            rstd = sb("rstde", [128, 16], F32, ph)
            ga = [sb("ga%d" % i, [128, 256], BF16, ph) for i in range(2)]
            GT = [sb("GT%d" % i, [128, 256], BF16, ph) for i in range(3)]
            iota16 = iota_f[:, 0:16]
            S.barrier()
            s_b = [Buf() for _ in range(16)]
            c_b = [Buf() for _ in range(16)]
            sv_b = [Buf() for _ in range(16)]
            siu_b = [Buf() for _ in range(16)]
            cv_b = [Buf() for _ in range(8)]
            ciu_b = [Buf() for _ in range(8)]
            s3 = s_sb[:].rearrange("p (a b) -> p a b", a=16)
            c3s = cand[:].rearrange("p (a b) -> p a b", a=16)
            cg3 = cand[:].rearrange("p (h a) -> p h a", h=8)
            sg3 = s_sb[:].rearrange("p (h a) -> p h a", h=8)

            def front(g, par):
                j = 1 if g == 0 else 0
                bTp = bT[par]
                for tt in range(2):
                    i = g * 2 + tt
                    S.dma(h1e[par][tt][:], h_s[i * 128:(i + 1) * 128, :], W=[h1e[par][tt].b])
                    modulate_T(h1e[par][tt], xn, xn, ssq, rstd, bTp[:, :, tt * 128:(tt + 1) * 128], bTp.b, 2, j, 7)
                    yield
                for hp in range(16):
                    pq = ps[7]
                    for dc in range(8):
                        S.op("pe", lambda e, dc=dc: e.matmul(pq[:, 0:256], lhsT=wq_sb[:, dc, hp * 128:(hp + 1) * 128], rhs=bTp[:, dc, :],
                                                            start=(dc == 0), stop=(dc == 7)),
                             R=[wq_sb.b, bTp.b], W=[pq.b], inc=(dc == 7))
                    if hp % 2 == 0:
                        S.op("act", lambda e: e.activation(out=qTe[:, hp, :], in_=pq[:, 0:256], func=AF.Copy), R=[pq.b], W=[qTe.b])
                    else:
                        S.op("dve", lambda e: e.tensor_copy(qTe[:, hp, :], pq[:, 0:256]), R=[pq.b], W=[qTe.b])
                    yield
                for tt in range(2):
                    for qd in range(4):
                        pb = ps[7]
                        for k4 in range(4):
                            hp = qd * 4 + k4
                            S.op("pe", lambda e, hp=hp, k4=k4: e.matmul(pb[:, k4 * 128:(k4 + 1) * 128], lhsT=qTe[:, hp, tt * 128:(tt + 1) * 128],
                                                                      rhs=keysT_sb[:, hp * 128:(hp + 1) * 128], start=True, stop=True),
                                 R=[qTe.b, keysT_sb.b], W=[pb.b], inc=(k4 == 3))
                        S.op("act", lambda e: e.activation(out=s_sb[:, qd * 512:(qd + 1) * 512], in_=pb[:, :], func=AF.Copy), R=[pb.b], W=s_b[qd * 4:qd * 4 + 4])
                        yield
                    for hp in range(16):
                        S.op("dve", lambda e, hp=hp: e.max(out=sv[:, hp, 0:8], in_=s3[:, hp, :]), R=[s_b[hp]], W=[sv_b[hp]])
                        if hp % 4 == 3:
                            yield
                    for hp in range(16):
                        S.op("dve", lambda e, hp=hp: e.max_index(out=si_u[:, hp, 0:8], in_max=sv[:, hp, 0:8], in_values=s3[:, hp, :]), R=[s_b[hp], sv_b[hp]], W=[siu_b[hp]])
                        if hp % 4 == 3:
                            yield
                    for hp in range(16):
                        S.op("dve", lambda e, hp=hp: e.match_replace(out=c3s[:, hp, :], in_to_replace=sv[:, hp, 0:8], in_values=s3[:, hp, :], imm_value=NEG),
                             R=[s_b[hp], sv_b[hp]], W=[c_b[hp]])
                        if hp % 4 == 3:
                            yield
                    for hp in range(16):
                        S.op("dve", lambda e, hp=hp: e.max(out=sv[:, hp, 8:16], in_=c3s[:, hp, :]), R=[c_b[hp]], W=[sv_b[hp]])
                        if hp % 4 == 3:
                            yield
                    for hp in range(16):
                        S.op("dve", lambda e, hp=hp: e.max_index(out=si_u[:, hp, 8:16], in_max=sv[:, hp, 8:16], in_values=c3s[:, hp, :]), R=[c_b[hp], sv_b[hp]], W=[siu_b[hp]])
                        if hp % 4 == 3:
                            yield
                    S.op("dve", lambda e: e.tensor_copy(si_f[:], si_u[:]), R=siu_b, W=[si_f.b])
                    sv4 = sv[:].rearrange("p (h a) k -> p h a k", a=2)
                    si4 = si_f[:].rearrange("p (h a) k -> p h a k", a=2)
                    c4 = cand[:].rearrange("p (h a b) -> p h a b", h=8, a=16)
                    S.op("dve", lambda e: e.tensor_tensor(out=c4, in0=sv4[:, :, 0, :].unsqueeze(3).to_broadcast([128, 8, 16, 16]),
                                                          in1=sv4[:, :, 1, :].unsqueeze(2).to_broadcast([128, 8, 16, 16]), op=ALU.add),
                         R=sv_b, W=c_b)
                    yield
                    hb = lambda lst, h: [lst[2 * h], lst[2 * h + 1]]
                    for h in range(8):
                        S.op("dve", lambda e, h=h: e.max(out=cv[:, h, 0:8], in_=cg3[:, h, :]), R=hb(c_b, h), W=[cv_b[h]])
                        if h % 4 == 3:
                            yield
                    for h in range(8):
                        S.op("dve", lambda e, h=h: e.max_index(out=ci_u[:, h, 0:8], in_max=cv[:, h, 0:8], in_values=cg3[:, h, :]), R=hb(c_b, h) + [cv_b[h]], W=[ciu_b[h]])
                        if h % 4 == 3:
                            yield
                    for h in range(8):
                        S.op("dve", lambda e, h=h: e.match_replace(out=sg3[:, h, :], in_to_replace=cv[:, h, 0:8], in_values=cg3[:, h, :], imm_value=NEG),
                             R=hb(c_b, h) + [cv_b[h]], W=hb(s_b, h))
                        if h % 4 == 3:
                            yield
                    for h in range(8):
                        S.op("dve", lambda e, h=h: e.max(out=cv[:, h, 8:16], in_=sg3[:, h, :]), R=hb(s_b, h), W=[cv_b[h]])
                        if h % 4 == 3:
                            yield
                    for h in range(8):
                        S.op("dve", lambda e, h=h: e.max_index(out=ci_u[:, h, 8:16], in_max=cv[:, h, 8:16], in_values=sg3[:, h, :]), R=hb(s_b, h) + [cv_b[h]], W=[ciu_b[h]])
                        if h % 4 == 3:
                            yield
                    ciu2 = ci_u[:].rearrange("p a b -> p (a b)")
                    S.op("dve", lambda e: e.tensor_scalar(out=k_u[:, 0, :], in0=ciu2, scalar1=4, scalar2=None, op0=ALU.logical_shift_right), R=ciu_b, W=[k_u.b])
                    S.op("dve", lambda e: e.tensor_scalar(out=k_u[:, 1, :], in0=ciu2, scalar1=15, scalar2=None, op0=ALU.bitwise_and), R=ciu_b, W=[k_u.b])
                    S.op("dve", lambda e: e.tensor_copy(k_f[:], k_u[:]), R=[k_u.b], W=[k_f.b])
                    yield
                    for a in range(2):
                        kk = k_f[:, a, :].rearrange("p (h k) -> p h k", h=8)
                        e4 = s_sb[:].rearrange("p (h a b) -> p h a b", h=8, a=16)
                        S.op("dve", lambda e: e.tensor_tensor(out=e4, in0=kk.unsqueeze(3).to_broadcast([128, 8, 16, 16]),
                                                              in1=iota16.unsqueeze(1).unsqueeze(1).to_broadcast([128, 8, 16, 16]), op=ALU.is_equal),
                             R=[k_f.b, iota_f.b], W=s_b)
                        yield
                        S.op("dve", lambda e: e.tensor_tensor(out=e4, in0=e4, in1=si4[:, :, a, :].unsqueeze(2).to_broadcast([128, 8, 16, 16]), op=ALU.mult),
                             R=s_b + [si_f.b], W=s_b)
                        yield
                        S.op("dve", lambda e: e.tensor_reduce(out=IG[:, a, :], in_=s_sb[:].rearrange("p (a b) -> p a b", b=16), axis=AX.X, op=ALU.add),
                             R=s_b, W=[IG.b])
                        yield
                    g3 = IG[:, 2, :].rearrange("p (h k) -> p h k", h=8)
                    S.op("dve", lambda e: e.tensor_tensor(out=g3, in0=cv[:], in1=cv[:, :, 0:1].to_broadcast([128, 8, 16]), op=ALU.subtract), R=cv_b, W=[IG.b])
                    S.op("act", lambda e: e.activation(out=IG[:, 2, :], in_=IG[:, 2, :], func=AF.Exp), R=[IG.b], W=[IG.b])
                    S.op("dve", lambda e: e.tensor_reduce(out=gsum[:], in_=g3, axis=AX.X, op=ALU.add), R=[IG.b], W=[gsum.b])
                    S.op("dve", lambda e: e.reciprocal(out=gsum[:], in_=gsum[:]), R=[gsum.b], W=[gsum.b])
                    S.op("dve", lambda e: e.tensor_tensor(out=g3, in0=g3, in1=gsum[:].unsqueeze(2).to_broadcast([128, 8, 16]), op=ALU.mult), R=[IG.b, gsum.b], W=[IG.b])
                    yield
                    pbt = ps[7]
                    for a in range(3):
                        S.op("pe", lambda e, a=a: e.transpose(pbt[:, a * 128:(a + 1) * 128], IG[:, a, :], ident_f[:]), R=[IG.b, ident_f.b], W=[pbt.b])
                    S.op("act", lambda e: e.activation(out=IGT[par][:, :, tt * 128:(tt + 1) * 128], in_=pbt[:, 0:384].rearrange("p (a b) -> p a b", a=3), func=AF.Copy),
                         R=[pbt.b], W=[IGT[par].b])
                    yield

            def build_wt(par):
                IGTp = IGT[par]
                for tb in range(32):
                    A_ = Ab[tb % 2]
                    B_ = Bb[tb % 2]
                    ab_ = AB_b[tb % 2]
                    for t8 in range(8):
                        t = tb * 8 + t8
                        S.op("dve", lambda e, t8=t8, t=t: e.tensor_scalar(out=A_[:, t8, :], in0=iota_b[:], scalar1=IGTp[:, 0, t:t + 1], scalar2=IGTp[:, 2, t:t + 1],
                                                                        op0=ALU.is_equal, op1=ALU.mult),
                             R=[IGTp.b, iota_b.b], W=[ab_[0][t8]])
                        S.op("dve", lambda e, t8=t8, t=t: e.tensor_scalar(out=B_[:, t8, :], in0=iota_b[:], scalar1=IGTp[:, 1, t:t + 1], scalar2=None, op0=ALU.is_equal),
                             R=[IGTp.b, iota_b.b], W=[ab_[1][t8]])
                    for q4 in range(2):
                        pw = ps[6 + (tb * 2 + q4) % 2]
                        for t4 in range(4):
                            t = q4 * 4 + t4
                            S.op("pe", lambda e, t=t, t4=t4: e.matmul(pw[:, t4 * 128:(t4 + 1) * 128], lhsT=B_[:, t, :], rhs=A_[:, t, :], start=True, stop=True),
                                 R=[ab_[0][t], ab_[1][t]], W=[pw.b], inc=(t4 == 3))
                        tk0 = tb * 8 + q4 * 4
                        S.op("act", lambda e: e.activation(out=WtS[:, tk0:tk0 + 4, :].rearrange("p a b -> p (a b)"), in_=pw[:, :], func=AF.Copy),
                             R=[pw.b], W=[WtS.b])

            AB_b = [[[Buf() for _ in range(8)] for _ in range(2)] for _ in range(2)]
            ngrp = T // 256
            glist = list(range(1 if last else 0, ngrp))
            for _ in front(glist[0], 0):
                pass
            for gi_, g in enumerate(glist):
                par = gi_ % 2
                j = 1 if g == 0 else 0
                bTp = bT[par]
                build_wt(par)
                nxt = front(glist[gi_ + 1], 1 - par) if gi_ + 1 < len(glist) else iter(())

                def uload(c):
                    S.dma(utc[c % 6][:].rearrange("p a b -> p (a b)"), utb_s[l, c], W=[utc[c % 6].b])

                def vload(c):
                    S.dma(vc[c % 4][:], vb_s[l, c * 128:(c + 1) * 128, :], W=[vc[c % 4].b])

                def emitA(c):
                    u_ = utc[c % 6]
                    pa = ps[4 + c % 3]
                    for dc in range(8):
                        S.op("pe", lambda e, dc=dc: e.matmul(pa[:, 0:256], lhsT=u_[:, dc, :], rhs=bTp[:, dc, :], start=(dc == 0), stop=(dc == 7)),
                             R=[u_.b, bTp.b], W=[pa.b], inc=(dc == 7))

                for c in range(5):
                    uload(c)
                for c in range(3):
                    vload(c)
                def emitMid(c):
                    pa = ps[4 + c % 3]
                    S.op("act", lambda e: e.activation(out=ga[c % 2][:], in_=pa[:, 0:256], func=AF.Gelu), R=[pa.b], W=[ga[c % 2].b])
                    S.op("pool", lambda e: e.tensor_tensor(out=GT[c % 3][:], in0=ga[c % 2][:], in1=WtS[:, :, c], op=ALU.mult), R=[ga[c % 2].b, WtS.b], W=[GT[c % 3].b])

                emitA(0)
                emitA(1)
                emitMid(0)
                for c in range(128):
                    if c + 5 < 128:
                        uload(c + 5)
                    if c + 3 < 128:
                        vload(c + 3)
                    if c + 2 < 128:
                        emitA(c + 2)
                    if c + 1 < 128:
                        emitMid(c + 1)
                    v_ = vc[c % 4]
                    for tt in range(2):
                        for hf in range(2):
                            S.op("pe", lambda e, tt=tt, hf=hf: e.matmul(ps[tt * 2 + hf][:, :], lhsT=GT[c % 3][:, tt * 128:(tt + 1) * 128], rhs=v_[:, hf * 512:(hf + 1) * 512],
                                                                      start=(c == 0), stop=(c == 127), skip_group_check=True),
                                 R=[GT[c % 3].b, v_.b], W=[ps[tt * 2 + hf].b], inc=(tt == 1 and hf == 1))
                    if c >= 2:
                        next(nxt, None)
                        if c % 2 == 0:
                            next(nxt, None)
                for _ in nxt:
                    pass
                for tt in range(2):
                    i = g * 2 + tt
                    y_ = h1e[par][tt]
                    for hf in range(2):
                        pb_ = ps[tt * 2 + hf]
                        S.op("dve", lambda e, hf=hf, pb_=pb_: e.tensor_tensor(out=pb_[:, :], in0=pb_[:, :], in1=gbe[:, hf * 512:(hf + 1) * 512], op=ALU.mult),
                             R=[pb_.b, gbe.b], W=[pb_.b])
                        S.op("dve", lambda e, hf=hf, pb_=pb_: e.tensor_tensor(out=y_[:, hf * 512:(hf + 1) * 512], in0=pb_[:, :], in1=y_[:, hf * 512:(hf + 1) * 512], op=ALU.add),
                             R=[pb_.b, y_.b], W=[y_.b])
                    if last:
                        S.dma(y_d[(i - 2) * 128:(i - 1) * 128, :], y_[:], R=[y_.b])
                    else:
                        S.dma(h_s[i * 128:(i + 1) * 128, :], y_[:], R=[y_.b])
                if g == 0:
                    gate_bcast(gbe, 1, 0)
            S.barrier()
        chk(l, "E")

    S.barrier()
    es.close()
    return nc


def _consts():
    ident = np.eye(128, dtype=np.float32)
    iota = np.tile(np.arange(128, dtype=np.float32)[None, :], (128, 1))
    jj = np.arange(128)[:, None]
    ii = np.arange(128)[None, :]
    tri = np.stack([(ii <= jj), (jj <= ii)]).astype(np.float32)
    sel = np.zeros((2, 256), np.float32)
    sel[0, 0:128] = 1.0
    sel[1, 128:256] = 1.0

    def tables(rot):
        qd = rot // 4
        inv = (np.float32(10000.0) ** (-np.arange(qd, dtype=np.float32) / np.float32(qd))).astype(np.float32)
        t = np.arange(SEQ)
        rows = (t // 64).astype(np.float32)
        cols = (t % 64).astype(np.float32)
        ar = rows[:, None] * inv
        ac = cols[:, None] * inv
        ang = np.concatenate([ar, ar, ac, ac], -1).astype(np.float32)
        cos = np.cos(ang).astype(np.float32)
        sin = np.sin(ang).astype(np.float32)
        sgn = np.concatenate([-np.ones(qd), np.ones(qd), -np.ones(qd), np.ones(qd)]).astype(np.float32)
        cs = np.zeros((T, 2, rot), np.float32)
        cs[:CTX, 0, :] = 1.0
        cs[CTX:, 0, :] = cos
        cs[CTX:, 1, :] = sin * sgn
        return cs.reshape(T, 2 * rot)

    return dict(ident=ident, iota=iota, tri=tri, sel=sel, cs_mla=tables(32), cs_swa=tables(64))


def _in_maps(inp):
    f = lambda a: np.ascontiguousarray(np.asarray(a, dtype=np.float32))
    shared = dict(_consts())
    for k in ("ada_w", "ada_b", "norm1_g", "norm2_g", "w_in", "mla_wuq", "mla_wukv", "mla_qn_g", "mla_kn_g",
              "swa_qn_g", "swa_kn_g", "swa_sink", "w_out", "peer_wq", "peer_v"):
        shared[k] = f(inp[k])
    shared["qa_g_t"] = f(np.asarray(inp["mla_qa_g"]).reshape(L, 3, 128).transpose(0, 2, 1))
    shared["kva_g_t"] = f(np.asarray(inp["mla_kva_g"]).reshape(L, 2, 128).transpose(0, 2, 1))
    shared["keysT"] = f(np.asarray(inp["peer_keys"]).transpose(0, 4, 1, 2, 3).reshape(L, 128, 2048))
    u = np.asarray(inp["peer_u"], dtype=np.float32).reshape(L, 128, 128, 8, 128)
    shared["peer_uT"] = np.ascontiguousarray(u.transpose(0, 1, 4, 3, 2)).reshape(L, 128, 128, 1024)
    x = np.asarray(inp["x"], dtype=np.float32)
    c = np.asarray(inp["c"], dtype=np.float32)
    ctx = np.asarray(inp["ctx"], dtype=np.float32)
    cc = np.asarray(inp["c_ctx"], dtype=np.float32)
    maps = []
    for b in range(8):
        m = dict(shared)
        m["x"] = np.ascontiguousarray(x[b])
        m["ctx"] = np.ascontiguousarray(ctx[b])
        m["cvec"] = np.ascontiguousarray(np.stack([c[b], cc]))
        maps.append(m)
    return maps


def kernel(**inputs):
    nc = build()
    res = run_bass_kernel_spmd(nc, _in_maps(inputs), core_ids=list(range(8)))
    return np.stack([np.asarray(r["y"], dtype=np.float32) for r in res.results], axis=0)
```

```python
import contextlib
import numpy as np
import concourse.bass as bass
import concourse.mybir as mybir
from concourse.bass_utils import run_bass_kernel_spmd

F32, BF16, U32 = mybir.dt.float32, mybir.dt.bfloat16, mybir.dt.uint32
AF = mybir.ActivationFunctionType
ALU = mybir.AluOpType
AX = mybir.AxisListType

L = 2
D = 1024
SEQ = 4096
CTX = 256
T = SEQ + CTX
NT = T // 128
EPS = 1e-6
DIN = 1440
NEG = -1.0e30


class Buf:
    __slots__ = ("w", "r")

    def __init__(self):
        self.w = None
        self.r = {}


class Sched:
    def __init__(self, nc, es):
        self.nc = nc
        self.eng = {"pe": nc.tensor, "act": nc.scalar, "dve": nc.vector, "pool": nc.gpsimd, "sp": nc.sync}
        self.sem = {}
        self.cnt = {}
        for e in ("pe", "act", "dve", "pool"):
            self.sem[e] = es.enter_context(nc.semaphore("s_" + e))
            self.cnt[e] = 0
        self.R = 32
        self.dsem = [es.enter_context(nc.semaphore("d%d" % i)) for i in range(self.R)]
        self.dlast = [None] * self.R
        self.dn = 0
        self.seen = {e: {} for e in self.eng}
        self.pe_pending = False
        self.rec = None
        self.bgsems = [es.enter_context(nc.semaphore("bg%d" % i)) for i in range(8)]
        self.bgn = 0

    def _wait(self, e, tok):
        key, sem, val = tok
        if e == "pe" and key == "pe":
            return
        if self.seen[e].get(key, 0) >= val:
            return
        self.eng[e].wait_ge(sem, val)
        self.seen[e][key] = val

    def _deps(self, e, reads, writes):
        for b in reads:
            if b.w is not None:
                self._wait(e, b.w)
        for b in writes:
            if b.w is not None:
                self._wait(e, b.w)
            for t in list(b.r.values()):
                self._wait(e, t)

    def _commit(self, tok, reads, writes):
        for b in reads:
            b.r[tok[0]] = tok
        for b in writes:
            b.w = tok
            b.r = {}

    def op(self, *a, **k):
        if self.rec is not None:
            self.rec.append(lambda: self._op(*a, **k))
        else:
            self._op(*a, **k)

    def dma(self, *a, **k):
        if self.rec is not None:
            self.rec.append(lambda: self._dma(*a, **k))
        else:
            self._dma(*a, **k)

    def _op(self, e, fn, R=(), W=(), inc=True):
        self._deps(e, R, W)
        inst = fn(self.eng[e])
        if inc:
            self.cnt[e] += 1
            inst.then_inc(self.sem[e], 1)
            tok = (e, self.sem[e], self.cnt[e])
        else:
            assert e == "pe"
            tok = (e, self.sem[e], self.cnt[e] + 1)
        self._commit(tok, R, W)

    def dma_untracked(self, out, in_, q):
        k = self.bgn % len(self.bgsems)
        if self.bgn >= len(self.bgsems):
            self.eng[q].wait_ge(self.bgsems[k], 16 * (self.bgn // len(self.bgsems)))
        inst = self.eng[q].dma_start(out=out, in_=in_)
        self.bgn += 1
        inst.then_inc(self.bgsems[k], 16)

    def _dma(self, out, in_, R=(), W=(), q="sp"):
        i = self.dn % self.R
        if self.dlast[i] is not None:
            self._wait(q, self.dlast[i])
        self._deps(q, R, W)
        inst = self.eng[q].dma_start(out=out, in_=in_)
        val = 16 * (self.dn // self.R + 1)
        inst.then_inc(self.dsem[i], 16)
        tok = ("d%d" % i, self.dsem[i], val)
        self.dlast[i] = tok
        self.dn += 1
        self._commit(tok, R, W)

    def barrier(self, engines=("pe", "act", "dve", "pool", "sp"), bg=False):
        toks = []
        if bg and self.bgn > 0:
            nb_ = len(self.bgsems)
            for k in range(nb_):
                cnt_ = len([x for x in range(self.bgn) if x % nb_ == k])
                if cnt_:
                    toks.append(("bg%d" % k, self.bgsems[k], 16 * cnt_))
        for e in ("pe", "act", "dve", "pool"):
            if self.cnt[e] > 0:
                toks.append((e, self.sem[e], self.cnt[e]))
        for t in self.dlast:
            if t is not None:
                toks.append(t)
        for e in engines:
            for t in toks:
                if not (e == t[0]):
                    self._wait(e, t)
                elif e != "pe":
                    self._wait(e, t)


class TT:
    def __init__(self, t):
        self.t = t
        self.b = Buf()

    def __getitem__(self, k):
        return self.t[k]


class _Stop(Exception):
    pass


_LAST = {}


def build_dbg(debug, stop):
    try:
        return build(debug, stop)
    except _Stop:
        _LAST["es"].close()
        return _LAST["nc"]


def build(debug=None, stop=None):
    nc = bass.Bass("TRN2", target_bir_lowering=False)
    es = contextlib.ExitStack()
    _LAST["nc"] = nc
    _LAST["es"] = es

    def din(name, shape, dt=F32):
        return nc.dram_tensor(name, list(shape), dt, kind="ExternalInput").ap()

    dbg_names = set(debug or [])

    def dscr(name, shape, dt):
        kind = "ExternalOutput" if name in dbg_names else "Internal"
        return nc.dram_tensor(name, list(shape), dt, kind=kind).ap()

    x_d = din("x", [SEQ, D])
    ctx_d = din("ctx", [CTX, D])
    cvec_d = din("cvec", [2, D])
    ada_w = din("ada_w", [L, D, 6 * D])
    ada_b = din("ada_b", [L, 6 * D])
    n1g = din("norm1_g", [L, D])
    n2g = din("norm2_g", [L, D])
    w_in = din("w_in", [L, D, DIN])
    qa_g = din("qa_g_t", [L, 128, 3])
    wuq = din("mla_wuq", [L, 384, 768])
    kva_g = din("kva_g_t", [L, 128, 2])
    wukv = din("mla_wukv", [L, 256, 1024])
    mqn_g = din("mla_qn_g", [L, 96])
    mkn_g = din("mla_kn_g", [L, 96])
    sqn_g = din("swa_qn_g", [L, 64])
    skn_g = din("swa_kn_g", [L, 64])
    sink_d = din("swa_sink", [L, 8])
    w_out = din("w_out", [L, D, D])
    wq_d = din("peer_wq", [L, D, 2048])
    keysT_d = din("keysT", [L, 128, 2048])
    ut_d = din("peer_uT", [L, 128, 128, 1024])
    v_d = din("peer_v", [L, 16384, D])
    ident_d = din("ident", [128, 128])
    iota_d = din("iota", [128, 128])
    tri_d = din("tri", [2, 128, 128])
    sel_d = din("sel", [2, 256])
    csm_d = din("cs_mla", [T, 64])
    css_d = din("cs_swa", [T, 128])
    y_d = nc.dram_tensor("y", [SEQ, D], F32, kind="ExternalOutput").ap()

    h_s = dscr("h_s", [T, D], F32)
    qTm_s = dscr("qTm_s", [8, 96, T], BF16)
    kTm_s = dscr("kTm_s", [8, 96, T], BF16)
    Vm_s = dscr("Vm_s", [T, 520], BF16)
    qTs_s = dscr("qTs_s", [8, 64, T], BF16)
    kTs_s = dscr("kTs_s", [2, 64, T], BF16)
    Vs_s = dscr("Vs_s", [T, 130], BF16)
    om_s = dscr("om_s", [T, D], BF16)
    utb_s = dscr("utb_s", [L, 128, 128, 1024], BF16)
    vb_s = dscr("vb_s", [L, 16384, D], BF16)

    S = Sched(nc, es)

    uid = [0]

    def sb(name, shape, dt, stack=None):
        uid[0] += 1
        return TT((stack or es).enter_context(nc.sbuf_tensor("%s_%d" % (name, uid[0]), list(shape), dt)))

    ps = [TT(es.enter_context(nc.psum_tensor("ps%d" % i, [128, 512], F32))) for i in range(8)]

    def psb(i):
        return ps[i][:].bitcast(BF16)

    ident_f = sb("ident_f", [128, 128], F32)
    ident_b = sb("ident_b", [128, 128], BF16)
    iota_f = sb("iota_f", [128, 128], F32)
    iota_b = sb("iota_b", [128, 128], BF16)
    tri_f = sb("tri_f", [128, 2, 128], F32)
    tri_b = sb("tri_b", [128, 2, 128], BF16)
    sel_f = sb("sel_f", [2, 256], F32)
    epsc = sb("epsc", [128, 1], F32)
    S.dma(ident_f[:], ident_d[:, :], W=[ident_f.b])
    S.dma(iota_f[:], iota_d[:, :], W=[iota_f.b])
    S.dma(tri_f[:], tri_d.rearrange("a p q -> p a q"), W=[tri_f.b])
    S.dma(sel_f[:], sel_d[:, :], W=[sel_f.b])
    S.op("dve", lambda e: e.tensor_copy(ident_b[:], ident_f[:]), R=[ident_f.b], W=[ident_b.b])
    S.op("dve", lambda e: e.tensor_copy(iota_b[:], iota_f[:]), R=[iota_f.b], W=[iota_b.b])
    S.op("dve", lambda e: e.tensor_copy(tri_b[:], tri_f[:]), R=[tri_f.b], W=[tri_b.b])
    S.op("dve", lambda e: e.memset(epsc[:], EPS), W=[epsc.b])

    modT = sb("modT", [128, 4, 8, 2], F32)
    grow = sb("grow", [2, 2 * D], F32)
    sT = sb("sT", [128, 8, 2], F32)
    esink = sb("esink", [128, 8], F32)
    tmp_es = contextlib.ExitStack()
    s2row = sb("s2row", [2, D], F32, tmp_es)

    S.dma(s2row[:], cvec_d[:, :], W=[s2row.b])
    S.op("act", lambda e: e.activation(out=s2row[:], in_=s2row[:], func=AF.Silu), R=[s2row.b], W=[s2row.b])
    for dc in range(8):
        S.op("pe", lambda e, dc=dc: e.transpose(ps[0][:, dc * 2:dc * 2 + 2], s2row[0:2, dc * 128:(dc + 1) * 128], ident_f[0:2, 0:2]),
             R=[s2row.b, ident_f.b], W=[ps[0].b])
    S.op("dve", lambda e: e.tensor_copy(sT[:].rearrange("p a b -> p (a b)"), ps[0][:, 0:16]), R=[ps[0].b], W=[sT.b])
    S.barrier()
    tmp_es.close()

    bg_list = []
    for l in range(L):
        for c in range(0, 128, 8):
            bg_list.append((utb_s[l, c:c + 8].rearrange("c p n -> (c p) n"), ut_d[l, c:c + 8].rearrange("c p n -> (c p) n")))
            bg_list.append((vb_s[l, c * 128:(c + 8) * 128, :], v_d[l, c * 128:(c + 8) * 128, :]))

    def bg_issue(n):
        for _ in range(n):
            if bg_list:
                o_, i_ = bg_list.pop(0)
                S.dma_untracked(o_, i_, "pool")

    def rstd_of(ph, x3, xb, H, Dh, tmp, ssq, rstd):
        tv = tmp[:, 0:H * Dh].rearrange("p (h d) -> p h d", h=H)
        S.op("dve", lambda e: e.tensor_tensor(out=tv, in0=x3, in1=x3, op=ALU.mult), R=[xb], W=[tmp.b])
        S.op("dve", lambda e: e.tensor_reduce(out=ssq[:, 0:H], in_=tv, axis=AX.X, op=ALU.add), R=[tmp.b], W=[ssq.b])
        S.op("act", lambda e: e.activation(out=rstd[:, 0:H], in_=ssq[:, 0:H], func=AF.Sqrt, scale=1.0 / Dh, bias=epsc[:, 0:1]),
             R=[ssq.b, epsc.b], W=[rstd.b])
        S.op("dve", lambda e: e.reciprocal(out=rstd[:, 0:H], in_=rstd[:, 0:H]), R=[rstd.b], W=[rstd.b])

    def rope(x5, xb, cst, cstb, H, q, t1, t2, out5, outb):
        cos4 = cst[:, 0, :].rearrange("p (a b q) -> p a b q", a=2, b=2)
        sin4 = cst[:, 1, :].rearrange("p (a b q) -> p a b q", a=2, b=2)
        n = H * 4 * q
        t1v = t1[:, 0:n].rearrange("p (h a b q) -> p h a b q", h=H, a=2, b=2)
        t2v = t2[:, 0:n].rearrange("p (h a b q) -> p h a b q", h=H, a=2, b=2)
        for b_ in range(2):
            cb = cos4[:, :, b_, :].unsqueeze(1).to_broadcast([128, H, 2, q])
            sbn = sin4[:, :, b_, :].unsqueeze(1).to_broadcast([128, H, 2, q])
            S.op("dve", lambda e, b_=b_, cb=cb: e.tensor_tensor(out=t1v[:, :, :, b_, :], in0=x5[:, :, :, b_, :], in1=cb, op=ALU.mult),
                 R=[xb, cstb], W=[t1.b])
            S.op("dve", lambda e, b_=b_, sbn=sbn: e.tensor_tensor(out=t2v[:, :, :, b_, :], in0=x5[:, :, :, 1 - b_, :], in1=sbn, op=ALU.mult),
                 R=[xb, cstb], W=[t2.b])
        for b_ in range(2):
            S.op("dve", lambda e, b_=b_: e.tensor_tensor(out=out5[:, :, :, b_, :], in0=t1v[:, :, :, b_, :], in1=t2v[:, :, :, b_, :], op=ALU.add),
                 R=[t1.b, t2.b], W=[outb])

    def load_w_bf16(ph, dst2d, src2d, ncols, stg, scale_ap=None, scale_b=None, k=[0]):
        for c0 in range(0, ncols, 2048):
            c1 = min(ncols, c0 + 2048)
            st = stg[k[0] % 2]
            k[0] += 1
            S.dma(st[:, 0:c1 - c0], src2d[:, c0:c1], W=[st.b])
            if scale_ap is None:
                S.op("act", lambda e, st=st, c0=c0, c1=c1: e.activation(out=dst2d[0][:, c0:c1], in_=st[:, 0:c1 - c0], func=AF.Copy),
                     R=[st.b], W=[dst2d[1]])
            else:
                S.op("dve", lambda e, st=st, c0=c0, c1=c1: e.tensor_scalar(out=dst2d[0][:, c0:c1], in0=st[:, 0:c1 - c0], scalar1=scale_ap,
                                                                         scalar2=None, op0=ALU.mult),
                     R=[st.b, scale_b], W=[dst2d[1]])

    def chk(l, phn):
        if stop is not None and stop == (l, phn):
            raise _Stop()

    for l in (range(L) if stop is None else range(stop[0] + 1)):
        last = l == L - 1
        with contextlib.ExitStack() as ph:
            aw = [sb("aw%d" % i, [128, 3072], F32, ph) for i in range(4)]
            modrow = sb("modrow", [2, 6 * D], F32, ph)
            abr = sb("abr", [2, 6 * D], F32, ph)
            ng = sb("ng", [2, 2, D], F32, ph)
            vrow = sb("vrow", [2, 4, D], F32, ph)
            S.dma(abr[:], ada_b[l:l + 1, :].to_broadcast([2, 6 * D]), W=[abr.b])
            S.dma(ng[:, 0, :], n1g[l:l + 1, :].to_broadcast([2, D]), W=[ng.b])
            S.dma(ng[:, 1, :], n2g[l:l + 1, :].to_broadcast([2, D]), W=[ng.b])
            S.dma(esink[:], sink_d[l:l + 1, :].to_broadcast([128, 8]), W=[esink.b])
            S.op("act", lambda e: e.activation(out=esink[:], in_=esink[:], func=AF.Exp), R=[esink.b], W=[esink.b])
            k = 0
            for half in range(2):
                for dc in range(8):
                    a = aw[k % 4]
                    k += 1
                    S.dma(a[:], ada_w[l, dc * 128:(dc + 1) * 128, half * 3072:(half + 1) * 3072], W=[a.b])
                    for cb in range(6):
                        S.op("pe", lambda e, a=a, cb=cb, dc=dc: e.matmul(ps[cb][0:2, :], lhsT=sT[:, dc, :], rhs=a[:, cb * 512:(cb + 1) * 512],
                                                                       start=(dc == 0), stop=(dc == 7)),
                             R=[a.b, sT.b], W=[ps[cb].b])
                for cb in range(6):
                    c0 = half * 3072 + cb * 512
                    S.op("dve", lambda e, cb=cb, c0=c0: e.tensor_tensor(out=modrow[:, c0:c0 + 512], in0=ps[cb][0:2, :], in1=abr[:, c0:c0 + 512], op=ALU.add),
                         R=[ps[cb].b, abr.b], W=[modrow.b])
            for j, (sci, shi) in enumerate(((1, 0), (4, 3))):
                S.op("dve", lambda e, j=j, sci=sci: e.scalar_tensor_tensor(out=vrow[:, 2 * j, :], in0=modrow[:, sci * D:(sci + 1) * D], scalar=1.0,
                                                                          in1=ng[:, j, :], op0=ALU.add, op1=ALU.mult),
                     R=[modrow.b, ng.b], W=[vrow.b])
                S.op("dve", lambda e, j=j, shi=shi: e.tensor_copy(vrow[:, 2 * j + 1, :], modrow[:, shi * D:(shi + 1) * D]),
                     R=[modrow.b], W=[vrow.b])
            S.op("dve", lambda e: e.tensor_copy(grow[:, 0:D], modrow[:, 2 * D:3 * D]), R=[modrow.b], W=[grow.b])
            S.op("dve", lambda e: e.tensor_copy(grow[:, D:2 * D], modrow[:, 5 * D:6 * D]), R=[modrow.b], W=[grow.b])
            for v in range(4):
                for dc in range(8):
                    o = (v * 8 + dc) * 2
                    S.op("pe", lambda e, v=v, dc=dc, o=o: e.transpose(ps[6][:, o:o + 2], vrow[0:2, v, dc * 128:(dc + 1) * 128], ident_f[0:2, 0:2]),
                         R=[vrow.b, ident_f.b], W=[ps[6].b])
            S.op("dve", lambda e: e.tensor_copy(modT[:].rearrange("p a b c -> p (a b c)"), ps[6][:, 0:64]), R=[ps[6].b], W=[modT.b])
            S.barrier()
        chk(l, "A")

        def gate_bcast(dst, gi, j):
            for hf in range(2):
                S.op("pe", lambda e, hf=hf: e.matmul(ps[6 + hf][:, :], lhsT=sel_f[0:2, j * 128:(j + 1) * 128],
                                                    rhs=grow[0:2, gi * D + hf * 512: gi * D + (hf + 1) * 512], start=True, stop=True),
                     R=[sel_f.b, grow.b], W=[ps[6 + hf].b])
                S.op("act", lambda e, hf=hf: e.activation(out=dst[:, hf * 512:(hf + 1) * 512], in_=ps[6 + hf][:, :], func=AF.Copy),
                     R=[ps[6 + hf].b], W=[dst.b])

        def modulate_T(ht, xn, sqj, ssq, rstd, aT_ap, aT_b, vA, j, psbank):
            rstd_of(None, ht[:].rearrange("p (h d) -> p h d", h=1), ht.b, 1, D, sqj, ssq, rstd)
            S.op("dve", lambda e: e.tensor_scalar(out=xn[:], in0=ht[:], scalar1=rstd[:, 0:1], scalar2=None, op0=ALU.mult),
                 R=[ht.b, rstd.b], W=[xn.b])
            pv = psb(psbank)
            for dc in range(8):
                S.op("pe", lambda e, dc=dc: e.transpose(pv[:, dc * 128:(dc + 1) * 128], xn[:, dc * 128:(dc + 1) * 128], ident_b[:]),
                     R=[xn.b, ident_b.b], W=[ps[psbank].b])
            for dc in range(8):
                S.op("dve", lambda e, dc=dc: e.tensor_scalar(out=aT_ap[:, dc, :], in0=pv[:, dc * 128:(dc + 1) * 128],
                                                            scalar1=modT[:, vA, dc, j:j + 1], scalar2=modT[:, vA + 1, dc, j:j + 1],
                                                            op0=ALU.mult, op1=ALU.add),
                     R=[ps[psbank].b, modT.b], W=[aT_b])

        with contextlib.ExitStack() as ph:
            stg = [sb("stg%d" % i, [128, 2048], F32, ph) for i in range(2)]
            w_in_sb = sb("w_in_sb", [128, 8, DIN], BF16, ph)
            wuq_sb = sb("wuq_sb", [128, 3, 768], BF16, ph)
            wukv_sb = sb("wukv_sb", [128, 2, 1024], BF16, ph)
            gq = sb("gq", [128, 3], F32, ph)
            gkv = sb("gkv", [128, 2], F32, ph)
            g_mq = sb("g_mq", [128, 96], F32, ph)
            g_mk = sb("g_mk", [128, 96], F32, ph)
            g_sq = sb("g_sq", [128, 64], F32, ph)
            g_sk = sb("g_sk", [128, 64], F32, ph)
            S.dma(gq[:], qa_g[l], W=[gq.b])
            S.dma(gkv[:], kva_g[l], W=[gkv.b])
            S.dma(g_mq[:], mqn_g[l:l + 1, :].to_broadcast([128, 96]), W=[g_mq.b])
            S.dma(g_mk[:], mkn_g[l:l + 1, :].to_broadcast([128, 96]), W=[g_mk.b])
            S.dma(g_sq[:], sqn_g[l:l + 1, :].to_broadcast([128, 64]), W=[g_sq.b])
            S.dma(g_sk[:], skn_g[l:l + 1, :].to_broadcast([128, 64]), W=[g_sk.b])
            for dc in range(8):
                load_w_bf16(ph, (w_in_sb[:, dc, :], w_in_sb.b), w_in[l, dc * 128:(dc + 1) * 128, :], DIN, stg)
            for kc in range(3):
                load_w_bf16(ph, (wuq_sb[:, kc, :], wuq_sb.b), wuq[l, kc * 128:(kc + 1) * 128, :], 768, stg, gq[:, kc:kc + 1], gq.b)
            for kc in range(2):
                load_w_bf16(ph, (wukv_sb[:, kc, :], wukv_sb.b), wukv[l, kc * 128:(kc + 1) * 128, :], 1024, stg, gkv[:, kc:kc + 1], gkv.b)

            ht = [sb("ht%d" % i, [128, D], F32, ph) for i in range(2)]
            cstm = [sb("cstm%d" % i, [128, 2, 32], F32, ph) for i in range(2)]
            csts = [sb("csts%d" % i, [128, 2, 64], F32, ph) for i in range(2)]
            def mkset(par):
                d_ = {}
                d_["sqj"] = sb("sqj%d" % par, [128, D], F32, ph)
                d_["t1"] = sb("t1%d" % par, [128, D], F32, ph)
                d_["t2"] = sb("t2%d" % par, [128, D], F32, ph)
                d_["xn"] = sb("xn%d" % par, [128, D], BF16, ph)
                d_["aT"] = sb("aT%d" % par, [128, 8, 128], BF16, ph)
                d_["p_sb"] = sb("p_sb%d" % par, [128, DIN], F32, ph)
                d_["qan"] = sb("qan%d" % par, [128, 384], BF16, ph)
                d_["kvan"] = sb("kvan%d" % par, [128, 256], BF16, ph)
                d_["lT"] = sb("lT%d" % par, [128, 5, 128], BF16, ph)
                d_["qm"] = sb("qm%d" % par, [128, 8, 96], F32, ph)
                d_["kv"] = sb("kv%d" % par, [128, 8, 128], F32, ph)
                d_["kf"] = sb("kf%d" % par, [128, 8, 96], F32, ph)
                d_["qn"] = sb("qn%d" % par, [128, 8, 96], F32, ph)
                d_["kn"] = sb("kn%d" % par, [128, 8, 96], F32, ph)
                d_["sqn"] = sb("sqn%d" % par, [128, 8, 64], F32, ph)
                d_["skn"] = sb("skn%d" % par, [128, 2, 64], F32, ph)
                d_["qo"] = sb("qo%d" % par, [128, 8, 96], BF16, ph)
                d_["ko"] = sb("ko%d" % par, [128, 8, 96], BF16, ph)
                d_["sqo"] = sb("sqo%d" % par, [128, 8, 64], BF16, ph)
                d_["sko"] = sb("sko%d" % par, [128, 2, 64], BF16, ph)
                d_["qT_sb"] = sb("qT_sb%d" % par, [128, 8, 128], BF16, ph)
                d_["kT_sb"] = sb("kT_sb%d" % par, [128, 8, 128], BF16, ph)
                d_["sqT_sb"] = sb("sqT_sb%d" % par, [128, 8, 128], BF16, ph)
                d_["skT_sb"] = sb("skT_sb%d" % par, [128, 2, 128], BF16, ph)
                return d_

            BS = [mkset(0), mkset(1)]
            Vm_sb = [sb("Vm_sb%d" % i, [128, 8, 65], BF16, ph) for i in range(2)]
            Vs_sb = [sb("Vs_sb%d" % i, [128, 2, 65], BF16, ph) for i in range(2)]
            for par_ in range(2):
                BS[par_]["ssq"] = sb("ssq%d" % par_, [128, 16], F32, ph)
                BS[par_]["rstd"] = sb("rstd%d" % par_, [128, 16], F32, ph)
            for i in range(2):
                S.op("pool", lambda e, i=i: e.memset(Vm_sb[i][:], 1.0), W=[Vm_sb[i].b])
                S.op("pool", lambda e, i=i: e.memset(Vs_sb[i][:], 1.0), W=[Vs_sb[i].b])

            def loads(i):
                hb = ht[i % 2]
                if l == 0:
                    src = ctx_d[i * 128:(i + 1) * 128, :] if i < 2 else x_d[(i - 2) * 128:(i - 1) * 128, :]
                else:
                    src = h_s[i * 128:(i + 1) * 128, :]
                S.dma(hb[:], src, W=[hb.b])
                S.dma(cstm[i % 2][:].rearrange("p a b -> p (a b)"), csm_d[i * 128:(i + 1) * 128, :], W=[cstm[i % 2].b])
                S.dma(csts[i % 2][:].rearrange("p a b -> p (a b)"), css_d[i * 128:(i + 1) * 128, :], W=[csts[i % 2].b])

            def tile_body(i, par):
                d_ = BS[par]
                sqj, t1, t2, xn, aT, p_sb, qan, kvan, lT, qm, kv, kf, qn, kn, sqn, skn, qo, ko, sqo, sko, qT_sb, kT_sb, sqT_sb, skT_sb, ssq, rstd = d_["sqj"], d_["t1"], d_["t2"], d_["xn"], d_["aT"], d_["p_sb"], d_["qan"], d_["kvan"], d_["lT"], d_["qm"], d_["kv"], d_["kf"], d_["qn"], d_["kn"], d_["sqn"], d_["skn"], d_["qo"], d_["ko"], d_["sqo"], d_["sko"], d_["qT_sb"], d_["kT_sb"], d_["sqT_sb"], d_["skT_sb"], d_["ssq"], d_["rstd"]
                ps_ = ps[4 * par:] + ps[:4 * par]
                psb_ = lambda k_: psb((k_ + 4 * par) % 8)
                j = 1 if i < 2 else 0
                hb = ht[i % 2]
                cm = cstm[i % 2]
                cs_ = csts[i % 2]
                tsl = slice(i * 128, (i + 1) * 128)
                modulate_T(hb, xn, sqj, ssq, rstd, aT[:], aT.b, 0, j, (4 * par) % 8)
                for cb, (c0, c1) in enumerate(((0, 512), (512, 1024), (1024, DIN))):
                    for dc in range(8):
                        S.op("pe", lambda e, cb=cb, c0=c0, c1=c1, dc=dc: e.matmul(ps_[1 + cb][:, 0:c1 - c0], lhsT=aT[:, dc, :], rhs=w_in_sb[:, dc, c0:c1],
                                                                              start=(dc == 0), stop=(dc == 7)),
                             R=[aT.b, w_in_sb.b], W=[ps_[1 + cb].b], inc=(dc == 7))
                    S.op("act", lambda e, cb=cb, c0=c0, c1=c1: e.activation(out=p_sb[:, c0:c1], in_=ps_[1 + cb][:, 0:c1 - c0], func=AF.Copy),
                         R=[ps_[1 + cb].b], W=[p_sb.b])
                rstd_of(ph, p_sb[:, 0:384].rearrange("p (h d) -> p h d", h=1), p_sb.b, 1, 384, sqj, ssq, rstd)
                S.op("dve", lambda e: e.tensor_scalar(out=qan[:], in0=p_sb[:, 0:384], scalar1=rstd[:, 0:1], scalar2=None, op0=ALU.mult),
                     R=[p_sb.b, rstd.b], W=[qan.b])
                rstd_of(ph, p_sb[:, 896:1152].rearrange("p (h d) -> p h d", h=1), p_sb.b, 1, 256, sqj, ssq, rstd)
                S.op("dve", lambda e: e.tensor_scalar(out=kvan[:], in0=p_sb[:, 896:1152], scalar1=rstd[:, 0:1], scalar2=None, op0=ALU.mult),
                     R=[p_sb.b, rstd.b], W=[kvan.b])
                pv4 = psb_(4)
                for kc in range(3):
                    S.op("pe", lambda e, kc=kc: e.transpose(pv4[:, kc * 128:(kc + 1) * 128], qan[:, kc * 128:(kc + 1) * 128], ident_b[:]),
                         R=[qan.b, ident_b.b], W=[ps_[4].b])
                for kc in range(2):
                    S.op("pe", lambda e, kc=kc: e.transpose(pv4[:, (3 + kc) * 128:(4 + kc) * 128], kvan[:, kc * 128:(kc + 1) * 128], ident_b[:]),
                         R=[kvan.b, ident_b.b], W=[ps_[4].b])
                S.op("act", lambda e: e.activation(out=lT[:].rearrange("p a b -> p (a b)"), in_=pv4[:, 0:640], func=AF.Copy), R=[ps_[4].b], W=[lT.b])
                for cb, (c0, c1) in enumerate(((0, 512), (512, 768))):
                    for kc in range(3):
                        S.op("pe", lambda e, cb=cb, c0=c0, c1=c1, kc=kc: e.matmul(ps_[5 + cb][:, 0:c1 - c0], lhsT=lT[:, kc, :], rhs=wuq_sb[:, kc, c0:c1],
                                                                              start=(kc == 0), stop=(kc == 2)),
                             R=[lT.b, wuq_sb.b], W=[ps_[5 + cb].b], inc=(kc == 2))
                    S.op("act", lambda e, cb=cb, c0=c0, c1=c1: e.activation(out=qm[:].rearrange("p a b -> p (a b)")[:, c0:c1], in_=ps_[5 + cb][:, 0:c1 - c0], func=AF.Copy),
                         R=[ps_[5 + cb].b], W=[qm.b])
                for cb in range(2):
                    for kc in range(2):
                        S.op("pe", lambda e, cb=cb, kc=kc: e.matmul(ps_[1 + cb][:, :], lhsT=lT[:, 3 + kc, :], rhs=wukv_sb[:, kc, cb * 512:(cb + 1) * 512],
                                                                start=(kc == 0), stop=(kc == 1)),
                             R=[lT.b, wukv_sb.b], W=[ps_[1 + cb].b], inc=(kc == 1))
                    S.op("act", lambda e, cb=cb: e.activation(out=kv[:].rearrange("p a b -> p (a b)")[:, cb * 512:(cb + 1) * 512], in_=ps_[1 + cb][:, :], func=AF.Copy),
                         R=[ps_[1 + cb].b], W=[kv.b])
                rstd_of(ph, qm[:], qm.b, 8, 96, sqj, ssq, rstd)
                S.op("dve", lambda e: e.tensor_tensor(out=qn[:], in0=qm[:], in1=rstd[:, 0:8].unsqueeze(2).to_broadcast([128, 8, 96]), op=ALU.mult),
                     R=[qm.b, rstd.b], W=[qn.b])
                S.op("dve", lambda e: e.tensor_tensor(out=qn[:], in0=qn[:], in1=g_mq[:].unsqueeze(1).to_broadcast([128, 8, 96]), op=ALU.mult),
                     R=[qn.b, g_mq.b], W=[qn.b])
                S.op("dve", lambda e: e.tensor_copy(qo[:, :, 0:64], qn[:, :, 0:64]), R=[qn.b], W=[qo.b])
                rope(qn[:, :, 64:96].rearrange("p h (a b q) -> p h a b q", a=2, b=2), qn.b, cm, cm.b, 8, 8, t1, t2,
                     qo[:, :, 64:96].rearrange("p h (a b q) -> p h a b q", a=2, b=2), qo.b)
                S.op("dve", lambda e: e.tensor_copy(kf[:, :, 0:64], kv[:, :, 0:64]), R=[kv.b], W=[kf.b])
                S.op("dve", lambda e: e.tensor_copy(kf[:, :, 64:96], p_sb[:, 1152:1184].unsqueeze(1).to_broadcast([128, 8, 32])), R=[p_sb.b], W=[kf.b])
                rstd_of(ph, kf[:], kf.b, 8, 96, sqj, ssq, rstd)
                S.op("dve", lambda e: e.tensor_tensor(out=kn[:], in0=kf[:], in1=rstd[:, 0:8].unsqueeze(2).to_broadcast([128, 8, 96]), op=ALU.mult),
                     R=[kf.b, rstd.b], W=[kn.b])
                S.op("dve", lambda e: e.tensor_tensor(out=kn[:], in0=kn[:], in1=g_mk[:].unsqueeze(1).to_broadcast([128, 8, 96]), op=ALU.mult),
                     R=[kn.b, g_mk.b], W=[kn.b])
                S.op("dve", lambda e: e.tensor_copy(ko[:, :, 0:64], kn[:, :, 0:64]), R=[kn.b], W=[ko.b])
                rope(kn[:, :, 64:96].rearrange("p h (a b q) -> p h a b q", a=2, b=2), kn.b, cm, cm.b, 8, 8, t1, t2,
                     ko[:, :, 64:96].rearrange("p h (a b q) -> p h a b q", a=2, b=2), ko.b)
                vmb = Vm_sb[i % 2]
                S.op("act", lambda e, vmb=vmb: e.activation(out=vmb[:, :, 0:64], in_=kv[:, :, 64:128], func=AF.Copy), R=[kv.b], W=[vmb.b])
                S.dma(Vm_s[tsl, :], vmb[:].rearrange("p a b -> p (a b)"), R=[vmb.b])
                sq3 = p_sb[:, 384:896].rearrange("p (h d) -> p h d", h=8)
                rstd_of(ph, sq3, p_sb.b, 8, 64, sqj, ssq, rstd)
                S.op("dve", lambda e: e.tensor_tensor(out=sqn[:], in0=sq3, in1=rstd[:, 0:8].unsqueeze(2).to_broadcast([128, 8, 64]), op=ALU.mult),
                     R=[p_sb.b, rstd.b], W=[sqn.b])
                S.op("dve", lambda e: e.tensor_tensor(out=sqn[:], in0=sqn[:], in1=g_sq[:].unsqueeze(1).to_broadcast([128, 8, 64]), op=ALU.mult),
                     R=[sqn.b, g_sq.b], W=[sqn.b])
                rope(sqn[:].rearrange("p h (a b q) -> p h a b q", a=2, b=2), sqn.b, cs_, cs_.b, 8, 16, t1, t2,
                     sqo[:].rearrange("p h (a b q) -> p h a b q", a=2, b=2), sqo.b)
                sk3 = p_sb[:, 1184:1312].rearrange("p (h d) -> p h d", h=2)
                rstd_of(ph, sk3, p_sb.b, 2, 64, sqj, ssq, rstd)
                S.op("dve", lambda e: e.tensor_tensor(out=skn[:], in0=sk3, in1=rstd[:, 0:2].unsqueeze(2).to_broadcast([128, 2, 64]), op=ALU.mult),
                     R=[p_sb.b, rstd.b], W=[skn.b])
                S.op("dve", lambda e: e.tensor_tensor(out=skn[:], in0=skn[:], in1=g_sk[:].unsqueeze(1).to_broadcast([128, 2, 64]), op=ALU.mult),
                     R=[skn.b, g_sk.b], W=[skn.b])
                rope(skn[:].rearrange("p h (a b q) -> p h a b q", a=2, b=2), skn.b, cs_, cs_.b, 2, 16, t1, t2,
                     sko[:].rearrange("p h (a b q) -> p h a b q", a=2, b=2), sko.b)
                vsb = Vs_sb[i % 2]
                S.op("act", lambda e, vsb=vsb: e.activation(out=vsb[:, :, 0:64], in_=p_sb[:, 1312:1440].rearrange("p (h d) -> p h d", h=2), func=AF.Copy),
                     R=[p_sb.b], W=[vsb.b])
                S.dma(Vs_s[tsl, :], vsb[:].rearrange("p a b -> p (a b)"), R=[vsb.b])
                for (src, dstT, nh, dh, bank, scr) in ((qo, qT_sb, 8, 96, 7, qTm_s), (ko, kT_sb, 8, 96, 0, kTm_s),
                                                       (sqo, sqT_sb, 8, 64, 4, qTs_s), (sko, skT_sb, 2, 64, 3, kTs_s)):
                    pvv = psb_(bank)
                    for h in range(nh):
                        S.op("pe", lambda e, src=src, h=h, dh=dh, pvv=pvv: e.transpose(pvv[0:dh, h * 128:(h + 1) * 128], src[:, h, :], ident_b[:]),
                             R=[src.b, ident_b.b], W=[ps_[bank].b])
                    S.op("act", lambda e, dstT=dstT, nh=nh, dh=dh, pvv=pvv: e.activation(out=dstT[0:dh, 0:nh, :].rearrange("p a b -> p (a b)"),
                                                                                       in_=pvv[0:dh, 0:nh * 128], func=AF.Copy),
                         R=[ps_[bank].b], W=[dstT.b])
                    S.dma(scr[:, :, tsl].rearrange("h d t -> d h t"), dstT[0:dh, 0:nh, :], R=[dstT.b])
                if i + 2 < NT:
                    loads(i + 2)

            loads(0)
            loads(1)
            for pr in range(NT // 2):
                bg_issue(2)
                recs = []
                for par in range(2):
                    S.rec = []
                    tile_body(2 * pr + par, par)
                    recs.append(S.rec)
                    S.rec = None
                for n_ in range(max(len(recs[0]), len(recs[1]))):
                    for par in range(2):
                        if n_ < len(recs[par]):
                            recs[par][n_]()
            S.barrier()
        chk(l, "B")

        with contextlib.ExitStack() as ph:
            kT_all = [sb("kTa%d" % h, [128, T], BF16, ph) for h in range(8)]
            Vall = sb("Vall", [128, NT, 520], BF16, ph)
            kTs_all = sb("kTs_all", [64, 2, T], BF16, ph)
            Vs_all = sb("Vs_all", [128, NT, 130], BF16, ph)
            qTg = [sb("qTg%d" % i, [128, 8, 512], BF16, ph) for i in range(2)]
            sqTg = [sb("sqTg%d" % i, [64, 8, 512], BF16, ph) for i in range(2)]
            PT = [sb("PT%d" % i, [128, 512], BF16, ph) for i in range(4)]
            obuf = [sb("obuf0", [128, 4, D], BF16, ph)]
            rden = sb("rden", [128, 4], F32, ph)
            for h in range(8):
                S.dma(kT_all[h][0:96, :], kTm_s[h], W=[kT_all[h].b])
            S.dma(Vall[:], Vm_s.rearrange("(k p) e -> p k e", p=128), W=[Vall.b])
            S.dma(kTs_all[:], kTs_s.rearrange("g d t -> d g t"), W=[kTs_all.b])
            S.dma(Vs_all[:], Vs_s.rearrange("(k p) e -> p k e", p=128), W=[Vs_all.b])
            groups = ([] if last else [(0, 256)]) + [(256 + 512 * g, 512) for g in range(8)]
            ctr = {"s": 0, "p": 0, "o": 0}

            def qloads(gi):
                t0, nq = groups[gi]
                S.dma(qTg[gi % 2][0:96, :, 0:nq], qTm_s[:, :, t0:t0 + nq].rearrange("h d t -> d h t"), W=[qTg[gi % 2].b])
                S.dma(sqTg[gi % 2][:, :, 0:nq], qTs_s[:, :, t0:t0 + nq].rearrange("h d t -> d h t"), W=[sqTg[gi % 2].b])

            qloads(0)
            for gi, (t0, nq) in enumerate(groups):
                bg_issue(4)
                if gi + 1 < len(groups):
                    qloads(gi + 1)
                isctx = t0 < 256
                nblk = nq // 128
                qg = qTg[gi % 2]
                sg = sqTg[gi % 2]
                ob = obuf[0]
                kts = [0, 1] if isctx else list(range(NT))
                its = []
                for h in range(8):
                    for ki, kt in enumerate(kts):
                        its.append(("m", h, ki, kt, len(kts), None, None))
                for blk in range(nblk):
                    ti = t0 // 128 + blk
                    if isctx:
                        kl = [0, 1]
                    else:
                        kl = [0, 1] + ([ti - 1] if ti - 1 >= 2 else []) + [ti] + ([ti + 1] if ti + 1 < NT else [])
                    for g2 in range(2):
                        for ki, kt in enumerate(kl):
                            its.append(("s", g2, ki, kt, len(kl), blk, ti))

                def emitS(n):
                    kind, a, ki, kt, nk, blk, ti = its[n]
                    pS = ps[n % 4]
                    if kind == "m":
                        S.op("pe", lambda e: e.matmul(pS[:, 0:nq], lhsT=kT_all[a][0:96, kt * 128:(kt + 1) * 128], rhs=qg[0:96, a, 0:nq], start=True, stop=True),
                             R=[kT_all[a].b, qg.b], W=[pS.b])
                    else:
                        S.op("pe", lambda e: e.matmul(pS[:, :].rearrange("p (a b) -> p a b", a=4), lhsT=kTs_all[:, a, kt * 128:(kt + 1) * 128],
                                                      rhs=sg[:, 4 * a:4 * a + 4, blk * 128:(blk + 1) * 128], start=True, stop=True),
                             R=[kTs_all.b, sg.b], W=[pS.b])

                po_of = {}

                def emitRest(n):
                    kind, a, ki, kt, nk, blk, ti = its[n]
                    pS = ps[n % 4]
                    pt = PT[n % 4]
                    if ki == 0:
                        po_of["cur"] = ps[4 + ctr["o"] % 2]
                        ctr["o"] += 1
                    po = po_of["cur"]
                    if kind == "m":
                        S.op("act", lambda e: e.activation(out=pt[:, 0:nq], in_=pS[:, 0:nq], func=AF.Exp, scale=96.0 ** -0.5), R=[pS.b], W=[pt.b])
                        for qs in range(nblk):
                            S.op("pe", lambda e, qs=qs: e.matmul(po[:, qs * 65:(qs + 1) * 65], lhsT=pt[:, qs * 128:(qs + 1) * 128], rhs=Vall[:, kt, a * 65:(a + 1) * 65],
                                                                start=(ki == 0 and qs == 0), stop=(ki == nk - 1), skip_group_check=True),
                                 R=[pt.b, Vall.b], W=[po.b], inc=(qs == nblk - 1))
                        if ki == nk - 1:
                            po3 = po[:, 0:nblk * 65].rearrange("p (a b) -> p a b", b=65)
                            S.op("dve", lambda e: e.reciprocal(out=rden[:, 0:nblk], in_=po3[:, :, 64]), R=[po.b], W=[rden.b])
                            S.op("dve", lambda e: e.tensor_tensor(out=ob[:, 0:nblk, a * 64:(a + 1) * 64], in0=po3[:, :, 0:64],
                                                                  in1=rden[:, 0:nblk].unsqueeze(2).to_broadcast([128, nblk, 64]), op=ALU.mult),
                                 R=[po.b, rden.b], W=[ob.b])
                    else:
                        S.op("act", lambda e: e.activation(out=pt[:, :], in_=pS[:, :], func=AF.Exp, scale=0.125), R=[pS.b], W=[pt.b])
                        if (not isctx) and kt >= 2 and kt != ti:
                            mi = 0 if kt == ti - 1 else 1
                            S.op("pool", lambda e: e.tensor_tensor(out=pt[:, :].rearrange("p (a b) -> p a b", a=4), in0=pt[:, :].rearrange("p (a b) -> p a b", a=4),
                                                                   in1=tri_b[:, mi, :].unsqueeze(1).to_broadcast([128, 4, 128]), op=ALU.mult),
                                 R=[pt.b, tri_b.b], W=[pt.b])
                        for r in range(4):
                            S.op("pe", lambda e, r=r: e.matmul(po[:, r * 65:(r + 1) * 65], lhsT=pt[:, r * 128:(r + 1) * 128], rhs=Vs_all[:, kt, a * 65:(a + 1) * 65],
                                                              start=(ki == 0 and r == 0), stop=(ki == nk - 1), skip_group_check=True),
                                 R=[pt.b, Vs_all.b], W=[po.b], inc=(r == 3))
                        if ki == nk - 1:
                            po3 = po[:, 0:260].rearrange("p (a b) -> p a b", b=65)
                            S.op("dve", lambda e: e.tensor_tensor(out=rden[:, 0:4], in0=po3[:, :, 64], in1=esink[:, 4 * a:4 * a + 4], op=ALU.add),
                                 R=[po.b, esink.b], W=[rden.b])
                            S.op("dve", lambda e: e.reciprocal(out=rden[:, 0:4], in_=rden[:, 0:4]), R=[rden.b], W=[rden.b])
                            S.op("dve", lambda e: e.tensor_tensor(out=ob[:, blk, 512 + a * 256:512 + (a + 1) * 256].rearrange("p (a b) -> p a b", a=4), in0=po3[:, :, 0:64],
                                                                  in1=rden[:, 0:4].unsqueeze(2).to_broadcast([128, 4, 64]), op=ALU.mult),
                                 R=[po.b, rden.b], W=[ob.b])

                LA = 3
                for n in range(min(LA, len(its))):
                    emitS(n)
                for n in range(len(its)):
                    if n + LA < len(its):
                        emitS(n + LA)
                    emitRest(n)
                S.dma(om_s[t0:t0 + nq, :].rearrange("(b p) c -> p b c", p=128), ob[:, 0:nblk, :], R=[ob.b])
            S.barrier()
        chk(l, "C")

        with contextlib.ExitStack() as ph:
            stg = [sb("stgd%d" % i, [128, 2048], F32, ph) for i in range(2)]
            w_out_sb = sb("w_out_sb", [128, 8, D], BF16, ph)
            for kc in range(8):
                load_w_bf16(ph, (w_out_sb[:, kc, :], w_out_sb.b), w_out[l, kc * 128:(kc + 1) * 128, :], D, stg)
            gb = [sb("gbd%d" % j, [128, D], F32, ph) for j in range(2)]
            gate_bcast(gb[0], 0, 0)
            gate_bcast(gb[1], 0, 1)
            htd = [sb("htd%d" % i, [128, D], F32, ph) for i in range(2)]
            omb = [sb("omb%d" % i, [128, D], BF16, ph) for i in range(2)]
            oT = sb("oT", [128, 8, 128], BF16, ph)
            h1 = [sb("h1d%d" % i, [128, D], F32, ph) for i in range(2)]
            tiles = list(range(2 if last else 0, NT))

            def loadsD(i):
                if l == 0:
                    src = ctx_d[i * 128:(i + 1) * 128, :] if i < 2 else x_d[(i - 2) * 128:(i - 1) * 128, :]
                else:
                    src = h_s[i * 128:(i + 1) * 128, :]
                S.dma(htd[i % 2][:], src, W=[htd[i % 2].b])
                S.dma(omb[i % 2][:], om_s[i * 128:(i + 1) * 128, :], W=[omb[i % 2].b])

            loadsD(tiles[0])
            for n_, i in enumerate(tiles):
                if n_ + 1 < len(tiles):
                    loadsD(tiles[n_ + 1])
                j = 1 if i < 2 else 0
                pv = psb(0)
                for kc in range(8):
                    S.op("pe", lambda e, kc=kc, i=i: e.transpose(pv[:, kc * 128:(kc + 1) * 128], omb[i % 2][:, kc * 128:(kc + 1) * 128], ident_b[:]),
                         R=[omb[i % 2].b, ident_b.b], W=[ps[0].b])
                S.op("act", lambda e: e.activation(out=oT[:].rearrange("p a b -> p (a b)"), in_=pv[:, :], func=AF.Copy), R=[ps[0].b], W=[oT.b])
                hh = h1[i % 2]
                for hf in range(2):
                    for kc in range(8):
                        S.op("pe", lambda e, hf=hf, kc=kc: e.matmul(ps[1 + hf][:, :], lhsT=oT[:, kc, :], rhs=w_out_sb[:, kc, hf * 512:(hf + 1) * 512],
                                                                start=(kc == 0), stop=(kc == 7)),
                             R=[oT.b, w_out_sb.b], W=[ps[1 + hf].b], inc=(kc == 7))
                    S.op("dve", lambda e, hf=hf, j=j, hh=hh: e.tensor_tensor(out=hh[:, hf * 512:(hf + 1) * 512], in0=ps[1 + hf][:, :],
                                                                          in1=gb[j][:, hf * 512:(hf + 1) * 512], op=ALU.mult),
                         R=[ps[1 + hf].b, gb[j].b], W=[hh.b])
                S.op("dve", lambda e, hh=hh, i=i: e.tensor_tensor(out=hh[:], in0=hh[:], in1=htd[i % 2][:], op=ALU.add), R=[hh.b, htd[i % 2].b], W=[hh.b])
                S.dma(h_s[i * 128:(i + 1) * 128, :], hh[:], R=[hh.b])
            bg_issue(len(bg_list))
            S.barrier(bg=True)
        chk(l, "D")

        with contextlib.ExitStack() as ph:
            s_sb = sb("s_sb", [128, 2048], F32, ph)
            cand = sb("cand", [128, 2048], F32, ph)
            stg = [s_sb, cand]
            wq_sb = sb("wq_sb", [128, 8, 2048], BF16, ph)
            keysT_sb = sb("keysT_sb", [128, 2048], BF16, ph)
            for dc in range(8):
                load_w_bf16(ph, (wq_sb[:, dc, :], wq_sb.b), wq_d[l, dc * 128:(dc + 1) * 128, :], 2048, stg)
            load_w_bf16(ph, (keysT_sb[:, :], keysT_sb.b), keysT_d[l], 2048, stg)
            gbe = sb("gbe", [128, D], F32, ph)
            gate_bcast(gbe, 1, 0 if last else 1)
            WtS = sb("WtS", [128, 256, 128], BF16, ph)
            Ab = [sb("Ab%d" % i, [128, 8, 128], BF16, ph) for i in range(2)]
            Bb = [sb("Bb%d" % i, [128, 8, 128], BF16, ph) for i in range(2)]
            utc = [sb("utc%d" % i, [128, 8, 128], BF16, ph) for i in range(6)]
            vc = [sb("vc%d" % i, [128, D], BF16, ph) for i in range(4)]
            h1e = [[sb("h1e%d_%d" % (p_, i), [128, D], F32, ph) for i in range(2)] for p_ in range(2)]
            xn = sb("xne", [128, D], BF16, ph)
            bT = [sb("bT%d" % p_, [128, 8, 256], BF16, ph) for p_ in range(2)]
            qTe = sb("qTe", [128, 16, 256], BF16, ph)
            sv = sb("sv", [128, 16, 16], F32, ph)
            si_u = sb("si_u", [128, 16, 16], U32, ph)
            si_f = sb("si_f", [128, 16, 16], F32, ph)
            cv = sb("cv", [128, 8, 16], F32, ph)
            ci_u = sb("ci_u", [128, 8, 16], U32, ph)
            k_u = sb("k_u", [128, 2, 128], U32, ph)
            k_f = sb("k_f", [128, 2, 128], F32, ph)
            IG = sb("IG", [128, 3, 128], F32, ph)
            IGT = [sb("IGT%d" % p_, [128, 3, 256], BF16, ph) for p_ in range(2)]
            gsum = sb("gsum", [128, 8], F32, ph)
            ssq = sb("ssqe", [128, 16], F32, ph)
            rstd = sb("rstde", [128, 16], F32, ph)
            ga = [sb("ga%d" % i, [128, 256], BF16, ph) for i in range(2)]
            GT = [sb("GT%d" % i, [128, 256], BF16, ph) for i in range(3)]
            iota16 = iota_f[:, 0:16]
            S.barrier()
            s_b = [Buf() for _ in range(16)]
            c_b = [Buf() for _ in range(16)]
            sv_b = [Buf() for _ in range(16)]
            siu_b = [Buf() for _ in range(16)]
            cv_b = [Buf() for _ in range(8)]
            ciu_b = [Buf() for _ in range(8)]
            s3 = s_sb[:].rearrange("p (a b) -> p a b", a=16)
            c3s = cand[:].rearrange("p (a b) -> p a b", a=16)
            cg3 = cand[:].rearrange("p (h a) -> p h a", h=8)
            sg3 = s_sb[:].rearrange("p (h a) -> p h a", h=8)

            def front(g, par):
                j = 1 if g == 0 else 0
                bTp = bT[par]
                for tt in range(2):
                    i = g * 2 + tt
                    S.dma(h1e[par][tt][:], h_s[i * 128:(i + 1) * 128, :], W=[h1e[par][tt].b])
                    modulate_T(h1e[par][tt], xn, xn, ssq, rstd, bTp[:, :, tt * 128:(tt + 1) * 128], bTp.b, 2, j, 7)
                    yield
                for hp in range(16):
                    pq = ps[7]
                    for dc in range(8):
                        S.op("pe", lambda e, dc=dc: e.matmul(pq[:, 0:256], lhsT=wq_sb[:, dc, hp * 128:(hp + 1) * 128], rhs=bTp[:, dc, :],
                                                            start=(dc == 0), stop=(dc == 7)),
                             R=[wq_sb.b, bTp.b], W=[pq.b], inc=(dc == 7))
                    if hp % 2 == 0:
                        S.op("act", lambda e: e.activation(out=qTe[:, hp, :], in_=pq[:, 0:256], func=AF.Copy), R=[pq.b], W=[qTe.b])
                    else:
                        S.op("dve", lambda e: e.tensor_copy(qTe[:, hp, :], pq[:, 0:256]), R=[pq.b], W=[qTe.b])
                    yield
                for tt in range(2):
                    for qd in range(4):
                        pb = ps[7]
                        for k4 in range(4):
                            hp = qd * 4 + k4
                            S.op("pe", lambda e, hp=hp, k4=k4: e.matmul(pb[:, k4 * 128:(k4 + 1) * 128], lhsT=qTe[:, hp, tt * 128:(tt + 1) * 128],
                                                                      rhs=keysT_sb[:, hp * 128:(hp + 1) * 128], start=True, stop=True),
                                 R=[qTe.b, keysT_sb.b], W=[pb.b], inc=(k4 == 3))
                        S.op("act", lambda e: e.activation(out=s_sb[:, qd * 512:(qd + 1) * 512], in_=pb[:, :], func=AF.Copy), R=[pb.b], W=s_b[qd * 4:qd * 4 + 4])
                        yield
                    for hp in range(16):
                        S.op("dve", lambda e, hp=hp: e.max(out=sv[:, hp, 0:8], in_=s3[:, hp, :]), R=[s_b[hp]], W=[sv_b[hp]])
                        if hp % 4 == 3:
                            yield
                    for hp in range(16):
                        S.op("dve", lambda e, hp=hp: e.max_index(out=si_u[:, hp, 0:8], in_max=sv[:, hp, 0:8], in_values=s3[:, hp, :]), R=[s_b[hp], sv_b[hp]], W=[siu_b[hp]])
                        if hp % 4 == 3:
                            yield
                    for hp in range(16):
                        S.op("dve", lambda e, hp=hp: e.match_replace(out=c3s[:, hp, :], in_to_replace=sv[:, hp, 0:8], in_values=s3[:, hp, :], imm_value=NEG),
                             R=[s_b[hp], sv_b[hp]], W=[c_b[hp]])
                        if hp % 4 == 3:
                            yield
                    for hp in range(16):
                        S.op("dve", lambda e, hp=hp: e.max(out=sv[:, hp, 8:16], in_=c3s[:, hp, :]), R=[c_b[hp]], W=[sv_b[hp]])
                        if hp % 4 == 3:
                            yield
                    for hp in range(16):
                        S.op("dve", lambda e, hp=hp: e.max_index(out=si_u[:, hp, 8:16], in_max=sv[:, hp, 8:16], in_values=c3s[:, hp, :]), R=[c_b[hp], sv_b[hp]], W=[siu_b[hp]])
                        if hp % 4 == 3:
                            yield
                    S.op("dve", lambda e: e.tensor_copy(si_f[:], si_u[:]), R=siu_b, W=[si_f.b])
                    sv4 = sv[:].rearrange("p (h a) k -> p h a k", a=2)
                    si4 = si_f[:].rearrange("p (h a) k -> p h a k", a=2)
                    c4 = cand[:].rearrange("p (h a b) -> p h a b", h=8, a=16)
                    S.op("dve", lambda e: e.tensor_tensor(out=c4, in0=sv4[:, :, 0, :].unsqueeze(3).to_broadcast([128, 8, 16, 16]),
                                                          in1=sv4[:, :, 1, :].unsqueeze(2).to_broadcast([128, 8, 16, 16]), op=ALU.add),
                         R=sv_b, W=c_b)
                    yield
                    hb = lambda lst, h: [lst[2 * h], lst[2 * h + 1]]
                    for h in range(8):
                        S.op("dve", lambda e, h=h: e.max(out=cv[:, h, 0:8], in_=cg3[:, h, :]), R=hb(c_b, h), W=[cv_b[h]])
                        if h % 4 == 3:
                            yield
                    for h in range(8):
                        S.op("dve", lambda e, h=h: e.max_index(out=ci_u[:, h, 0:8], in_max=cv[:, h, 0:8], in_values=cg3[:, h, :]), R=hb(c_b, h) + [cv_b[h]], W=[ciu_b[h]])
                        if h % 4 == 3:
                            yield
                    for h in range(8):
                        S.op("dve", lambda e, h=h: e.match_replace(out=sg3[:, h, :], in_to_replace=cv[:, h, 0:8], in_values=cg3[:, h, :], imm_value=NEG),
                             R=hb(c_b, h) + [cv_b[h]], W=hb(s_b, h))
                        if h % 4 == 3:
                            yield
                    for h in range(8):
                        S.op("dve", lambda e, h=h: e.max(out=cv[:, h, 8:16], in_=sg3[:, h, :]), R=hb(s_b, h), W=[cv_b[h]])
                        if h % 4 == 3:
                            yield
                    for h in range(8):
                        S.op("dve", lambda e, h=h: e.max_index(out=ci_u[:, h, 8:16], in_max=cv[:, h, 8:16], in_values=sg3[:, h, :]), R=hb(s_b, h) + [cv_b[h]], W=[ciu_b[h]])
                        if h % 4 == 3:
                            yield
                    ciu2 = ci_u[:].rearrange("p a b -> p (a b)")
                    S.op("dve", lambda e: e.tensor_scalar(out=k_u[:, 0, :], in0=ciu2, scalar1=4, scalar2=None, op0=ALU.logical_shift_right), R=ciu_b, W=[k_u.b])
                    S.op("dve", lambda e: e.tensor_scalar(out=k_u[:, 1, :], in0=ciu2, scalar1=15, scalar2=None, op0=ALU.bitwise_and), R=ciu_b, W=[k_u.b])
                    S.op("dve", lambda e: e.tensor_copy(k_f[:], k_u[:]), R=[k_u.b], W=[k_f.b])
                    yield
                    for a in range(2):
                        kk = k_f[:, a, :].rearrange("p (h k) -> p h k", h=8)
                        e4 = s_sb[:].rearrange("p (h a b) -> p h a b", h=8, a=16)
                        S.op("dve", lambda e: e.tensor_tensor(out=e4, in0=kk.unsqueeze(3).to_broadcast([128, 8, 16, 16]),
                                                              in1=iota16.unsqueeze(1).unsqueeze(1).to_broadcast([128, 8, 16, 16]), op=ALU.is_equal),
                             R=[k_f.b, iota_f.b], W=s_b)
                        yield
                        S.op("dve", lambda e: e.tensor_tensor(out=e4, in0=e4, in1=si4[:, :, a, :].unsqueeze(2).to_broadcast([128, 8, 16, 16]), op=ALU.mult),
                             R=s_b + [si_f.b], W=s_b)
                        yield
                        S.op("dve", lambda e: e.tensor_reduce(out=IG[:, a, :], in_=s_sb[:].rearrange("p (a b) -> p a b", b=16), axis=AX.X, op=ALU.add),
                             R=s_b, W=[IG.b])
                        yield
                    g3 = IG[:, 2, :].rearrange("p (h k) -> p h k", h=8)
                    S.op("dve", lambda e: e.tensor_tensor(out=g3, in0=cv[:], in1=cv[:, :, 0:1].to_broadcast([128, 8, 16]), op=ALU.subtract), R=cv_b, W=[IG.b])
                    S.op("act", lambda e: e.activation(out=IG[:, 2, :], in_=IG[:, 2, :], func=AF.Exp), R=[IG.b], W=[IG.b])
                    S.op("dve", lambda e: e.tensor_reduce(out=gsum[:], in_=g3, axis=AX.X, op=ALU.add), R=[IG.b], W=[gsum.b])
                    S.op("dve", lambda e: e.reciprocal(out=gsum[:], in_=gsum[:]), R=[gsum.b], W=[gsum.b])
                    S.op("dve", lambda e: e.tensor_tensor(out=g3, in0=g3, in1=gsum[:].unsqueeze(2).to_broadcast([128, 8, 16]), op=ALU.mult), R=[IG.b, gsum.b], W=[IG.b])
                    yield
                    pbt = ps[7]
                    for a in range(3):
                        S.op("pe", lambda e, a=a: e.transpose(pbt[:, a * 128:(a + 1) * 128], IG[:, a, :], ident_f[:]), R=[IG.b, ident_f.b], W=[pbt.b])
                    S.op("act", lambda e: e.activation(out=IGT[par][:, :, tt * 128:(tt + 1) * 128], in_=pbt[:, 0:384].rearrange("p (a b) -> p a b", a=3), func=AF.Copy),
                         R=[pbt.b], W=[IGT[par].b])
                    yield

            def build_wt(par):
                IGTp = IGT[par]
                for tb in range(32):
                    A_ = Ab[tb % 2]
                    B_ = Bb[tb % 2]
                    ab_ = AB_b[tb % 2]
                    for t8 in range(8):
                        t = tb * 8 + t8
                        S.op("dve", lambda e, t8=t8, t=t: e.tensor_scalar(out=A_[:, t8, :], in0=iota_b[:], scalar1=IGTp[:, 0, t:t + 1], scalar2=IGTp[:, 2, t:t + 1],
                                                                        op0=ALU.is_equal, op1=ALU.mult),
                             R=[IGTp.b, iota_b.b], W=[ab_[0][t8]])
                        S.op("dve", lambda e, t8=t8, t=t: e.tensor_scalar(out=B_[:, t8, :], in0=iota_b[:], scalar1=IGTp[:, 1, t:t + 1], scalar2=None, op0=ALU.is_equal),
                             R=[IGTp.b, iota_b.b], W=[ab_[1][t8]])
                    for q4 in range(2):
                        pw = ps[6 + (tb * 2 + q4) % 2]
                        for t4 in range(4):
                            t = q4 * 4 + t4
                            S.op("pe", lambda e, t=t, t4=t4: e.matmul(pw[:, t4 * 128:(t4 + 1) * 128], lhsT=B_[:, t, :], rhs=A_[:, t, :], start=True, stop=True),
                                 R=[ab_[0][t], ab_[1][t]], W=[pw.b], inc=(t4 == 3))
                        tk0 = tb * 8 + q4 * 4
                        S.op("act", lambda e: e.activation(out=WtS[:, tk0:tk0 + 4, :].rearrange("p a b -> p (a b)"), in_=pw[:, :], func=AF.Copy),
                             R=[pw.b], W=[WtS.b])

            AB_b = [[[Buf() for _ in range(8)] for _ in range(2)] for _ in range(2)]
            ngrp = T // 256
            glist = list(range(1 if last else 0, ngrp))
            for _ in front(glist[0], 0):
                pass
            for gi_, g in enumerate(glist):
                par = gi_ % 2
                j = 1 if g == 0 else 0
                bTp = bT[par]
                nxt = front(glist[gi_ + 1], 1 - par) if gi_ + 1 < len(glist) else iter(())

                def uload(c):
                    S.dma(utc[c % 6][:].rearrange("p a b -> p (a b)"), utb_s[l, c], W=[utc[c % 6].b])

                def vload(c):
                    S.dma(vc[c % 4][:], vb_s[l, c * 128:(c + 1) * 128, :], W=[vc[c % 4].b])

                def emitA(c):
                    u_ = utc[c % 6]
                    pa = ps[4 + c % 3]
                    for dc in range(8):
                        S.op("pe", lambda e, dc=dc: e.matmul(pa[:, 0:256], lhsT=u_[:, dc, :], rhs=bTp[:, dc, :], start=(dc == 0), stop=(dc == 7)),
                             R=[u_.b, bTp.b], W=[pa.b], inc=(dc == 7))

                for c in range(5):
                    uload(c)
                for c in range(3):
                    vload(c)
                def emitMid(c):
                    pa = ps[4 + c % 3]
                    S.op("act", lambda e: e.activation(out=ga[c % 2][:], in_=pa[:, 0:256], func=AF.Gelu), R=[pa.b], W=[ga[c % 2].b])
                    S.op("pool", lambda e: e.tensor_tensor(out=GT[c % 3][:], in0=ga[c % 2][:], in1=WtS[:, :, c], op=ALU.mult), R=[ga[c % 2].b, WtS.b], W=[GT[c % 3].b])

                emitA(0)
                emitA(1)
                build_wt(par)
                emitMid(0)
                for c in range(128):
                    if c + 5 < 128:
                        uload(c + 5)
                    if c + 3 < 128:
                        vload(c + 3)
                    if c + 2 < 128:
                        emitA(c + 2)
                    if c + 1 < 128:
                        emitMid(c + 1)
                    v_ = vc[c % 4]
                    for tt in range(2):
                        for hf in range(2):
                            S.op("pe", lambda e, tt=tt, hf=hf: e.matmul(ps[tt * 2 + hf][:, :], lhsT=GT[c % 3][:, tt * 128:(tt + 1) * 128], rhs=v_[:, hf * 512:(hf + 1) * 512],
                                                                      start=(c == 0), stop=(c == 127), skip_group_check=True),
                                 R=[GT[c % 3].b, v_.b], W=[ps[tt * 2 + hf].b], inc=(tt == 1 and hf == 1))
                    if c >= 2:
                        next(nxt, None)
                        if c % 2 == 0:
                            next(nxt, None)
                for _ in nxt:
                    pass
                for tt in range(2):
                    i = g * 2 + tt
                    y_ = h1e[par][tt]
                    for hf in range(2):
                        pb_ = ps[tt * 2 + hf]
                        S.op("dve", lambda e, hf=hf, pb_=pb_: e.tensor_tensor(out=pb_[:, :], in0=pb_[:, :], in1=gbe[:, hf * 512:(hf + 1) * 512], op=ALU.mult),
                             R=[pb_.b, gbe.b], W=[pb_.b])
                        S.op("dve", lambda e, hf=hf, pb_=pb_: e.tensor_tensor(out=y_[:, hf * 512:(hf + 1) * 512], in0=pb_[:, :], in1=y_[:, hf * 512:(hf + 1) * 512], op=ALU.add),
                             R=[pb_.b, y_.b], W=[y_.b])
                    if last:
                        S.dma(y_d[(i - 2) * 128:(i - 1) * 128, :], y_[:], R=[y_.b])
                    else:
                        S.dma(h_s[i * 128:(i + 1) * 128, :], y_[:], R=[y_.b])
                if g == 0:
                    gate_bcast(gbe, 1, 0)
            S.barrier()
        chk(l, "E")

    S.barrier()
    es.close()
    return nc


def _consts():
    ident = np.eye(128, dtype=np.float32)
    iota = np.tile(np.arange(128, dtype=np.float32)[None, :], (128, 1))
    jj = np.arange(128)[:, None]
    ii = np.arange(128)[None, :]
    tri = np.stack([(ii <= jj), (jj <= ii)]).astype(np.float32)
    sel = np.zeros((2, 256), np.float32)
    sel[0, 0:128] = 1.0
    sel[1, 128:256] = 1.0

    def tables(rot):
        qd = rot // 4
        inv = (np.float32(10000.0) ** (-np.arange(qd, dtype=np.float32) / np.float32(qd))).astype(np.float32)
        t = np.arange(SEQ)
        rows = (t // 64).astype(np.float32)
        cols = (t % 64).astype(np.float32)
        ar = rows[:, None] * inv
        ac = cols[:, None] * inv
        ang = np.concatenate([ar, ar, ac, ac], -1).astype(np.float32)
        cos = np.cos(ang).astype(np.float32)
        sin = np.sin(ang).astype(np.float32)
        sgn = np.concatenate([-np.ones(qd), np.ones(qd), -np.ones(qd), np.ones(qd)]).astype(np.float32)
        cs = np.zeros((T, 2, rot), np.float32)
        cs[:CTX, 0, :] = 1.0
        cs[CTX:, 0, :] = cos
        cs[CTX:, 1, :] = sin * sgn
        return cs.reshape(T, 2 * rot)

    return dict(ident=ident, iota=iota, tri=tri, sel=sel, cs_mla=tables(32), cs_swa=tables(64))


def _in_maps(inp):
    f = lambda a: np.ascontiguousarray(np.asarray(a, dtype=np.float32))
    shared = dict(_consts())
    for k in ("ada_w", "ada_b", "norm1_g", "norm2_g", "w_in", "mla_wuq", "mla_wukv", "mla_qn_g", "mla_kn_g",
              "swa_qn_g", "swa_kn_g", "swa_sink", "w_out", "peer_wq", "peer_v"):
        shared[k] = f(inp[k])
    shared["qa_g_t"] = f(np.asarray(inp["mla_qa_g"]).reshape(L, 3, 128).transpose(0, 2, 1))
    shared["kva_g_t"] = f(np.asarray(inp["mla_kva_g"]).reshape(L, 2, 128).transpose(0, 2, 1))
    shared["keysT"] = f(np.asarray(inp["peer_keys"]).transpose(0, 4, 1, 2, 3).reshape(L, 128, 2048))
    u = np.asarray(inp["peer_u"], dtype=np.float32).reshape(L, 128, 128, 8, 128)
    shared["peer_uT"] = np.ascontiguousarray(u.transpose(0, 1, 4, 3, 2)).reshape(L, 128, 128, 1024)
    x = np.asarray(inp["x"], dtype=np.float32)
    c = np.asarray(inp["c"], dtype=np.float32)
    ctx = np.asarray(inp["ctx"], dtype=np.float32)
    cc = np.asarray(inp["c_ctx"], dtype=np.float32)
    maps = []
    for b in range(8):
        m = dict(shared)
        m["x"] = np.ascontiguousarray(x[b])
        m["ctx"] = np.ascontiguousarray(ctx[b])
        m["cvec"] = np.ascontiguousarray(np.stack([c[b], cc]))
        maps.append(m)
    return maps


def kernel(**inputs):
    nc = build()
    res = run_bass_kernel_spmd(nc, _in_maps(inputs), core_ids=list(range(8)))
    return np.stack([np.asarray(r["y"], dtype=np.float32) for r in res.results], axis=0)
```

```python
import contextlib
import numpy as np
import concourse.bass as bass
import concourse.mybir as mybir
from concourse.bass_utils import run_bass_kernel_spmd

F32, BF16, U32 = mybir.dt.float32, mybir.dt.bfloat16, mybir.dt.uint32
AF = mybir.ActivationFunctionType
ALU = mybir.AluOpType
AX = mybir.AxisListType

L = 2
D = 1024
SEQ = 4096
CTX = 256
T = SEQ + CTX
NT = T // 128
EPS = 1e-6
DIN = 1440
NEG = -1.0e30


class Buf:
    __slots__ = ("w", "r")

    def __init__(self):
        self.w = None
        self.r = {}


class Sched:
    def __init__(self, nc, es):
        self.nc = nc
        self.eng = {"pe": nc.tensor, "act": nc.scalar, "dve": nc.vector, "pool": nc.gpsimd, "sp": nc.sync}
        self.sem = {}
        self.cnt = {}
        for e in ("pe", "act", "dve", "pool"):
            self.sem[e] = es.enter_context(nc.semaphore("s_" + e))
            self.cnt[e] = 0
        self.R = 32
        self.dsem = [es.enter_context(nc.semaphore("d%d" % i)) for i in range(self.R)]
        self.dlast = [None] * self.R
        self.dn = 0
        self.seen = {e: {} for e in self.eng}
        self.pe_pending = False
        self.rec = None
        self.bgsems = [es.enter_context(nc.semaphore("bg%d" % i)) for i in range(8)]
        self.bgn = 0

    def _wait(self, e, tok):
        key, sem, val = tok
        if e == "pe" and key == "pe":
            return
        if self.seen[e].get(key, 0) >= val:
            return
        self.eng[e].wait_ge(sem, val)
        self.seen[e][key] = val

    def _deps(self, e, reads, writes):
        for b in reads:
            if b.w is not None:
                self._wait(e, b.w)
        for b in writes:
            if b.w is not None:
                self._wait(e, b.w)
            for t in list(b.r.values()):
                self._wait(e, t)

    def _commit(self, tok, reads, writes):
        for b in reads:
            b.r[tok[0]] = tok
        for b in writes:
            b.w = tok
            b.r = {}

    def op(self, *a, **k):
        if self.rec is not None:
            self.rec.append(lambda: self._op(*a, **k))
        else:
            self._op(*a, **k)

    def dma(self, *a, **k):
        if self.rec is not None:
            self.rec.append(lambda: self._dma(*a, **k))
        else:
            self._dma(*a, **k)

    def _op(self, e, fn, R=(), W=(), inc=True):
        self._deps(e, R, W)
        inst = fn(self.eng[e])
        if inc:
            self.cnt[e] += 1
            inst.then_inc(self.sem[e], 1)
            tok = (e, self.sem[e], self.cnt[e])
        else:
            assert e == "pe"
            tok = (e, self.sem[e], self.cnt[e] + 1)
        self._commit(tok, R, W)

    def dma_untracked(self, out, in_, q):
        k = self.bgn % len(self.bgsems)
        if self.bgn >= len(self.bgsems):
            self.eng[q].wait_ge(self.bgsems[k], 16 * (self.bgn // len(self.bgsems)))
        inst = self.eng[q].dma_start(out=out, in_=in_)
        self.bgn += 1
        inst.then_inc(self.bgsems[k], 16)

    def _dma(self, out, in_, R=(), W=(), q="sp"):
        i = self.dn % self.R
        if self.dlast[i] is not None:
            self._wait(q, self.dlast[i])
        self._deps(q, R, W)
        inst = self.eng[q].dma_start(out=out, in_=in_)
        val = 16 * (self.dn // self.R + 1)
        inst.then_inc(self.dsem[i], 16)
        tok = ("d%d" % i, self.dsem[i], val)
        self.dlast[i] = tok
        self.dn += 1
        self._commit(tok, R, W)

    def barrier(self, engines=("pe", "act", "dve", "pool", "sp"), bg=False):
        toks = []
        if bg and self.bgn > 0:
            nb_ = len(self.bgsems)
            for k in range(nb_):
                cnt_ = len([x for x in range(self.bgn) if x % nb_ == k])
                if cnt_:
                    toks.append(("bg%d" % k, self.bgsems[k], 16 * cnt_))
        for e in ("pe", "act", "dve", "pool"):
            if self.cnt[e] > 0:
                toks.append((e, self.sem[e], self.cnt[e]))
        for t in self.dlast:
            if t is not None:
                toks.append(t)
        for e in engines:
            for t in toks:
                if not (e == t[0]):
                    self._wait(e, t)
                elif e != "pe":
                    self._wait(e, t)


class TT:
    def __init__(self, t):
        self.t = t
        self.b = Buf()

    def __getitem__(self, k):
        return self.t[k]


class _Stop(Exception):
    pass


_LAST = {}


def build_dbg(debug, stop):
    try:
        return build(debug, stop)
    except _Stop:
        _LAST["es"].close()
        return _LAST["nc"]


def build(debug=None, stop=None):
    nc = bass.Bass("TRN2", target_bir_lowering=False)
    es = contextlib.ExitStack()
    _LAST["nc"] = nc
    _LAST["es"] = es

    def din(name, shape, dt=F32):
        return nc.dram_tensor(name, list(shape), dt, kind="ExternalInput").ap()

    dbg_names = set(debug or [])

    def dscr(name, shape, dt):
        kind = "ExternalOutput" if name in dbg_names else "Internal"
        return nc.dram_tensor(name, list(shape), dt, kind=kind).ap()

    x_d = din("x", [SEQ, D])
    ctx_d = din("ctx", [CTX, D])
    cvec_d = din("cvec", [2, D])
    ada_w = din("ada_w", [L, D, 6 * D])
    ada_b = din("ada_b", [L, 6 * D])
    n1g = din("norm1_g", [L, D])
    n2g = din("norm2_g", [L, D])
    w_in = din("w_in", [L, D, DIN])
    qa_g = din("qa_g_t", [L, 128, 3])
    wuq = din("mla_wuq", [L, 384, 768])
    kva_g = din("kva_g_t", [L, 128, 2])
    wukv = din("mla_wukv", [L, 256, 1024])
    mqn_g = din("mla_qn_g", [L, 96])
    mkn_g = din("mla_kn_g", [L, 96])
    sqn_g = din("swa_qn_g", [L, 64])
    skn_g = din("swa_kn_g", [L, 64])
    sink_d = din("swa_sink", [L, 8])
    w_out = din("w_out", [L, D, D])
    wq_d = din("peer_wq", [L, D, 2048])
    keysT_d = din("keysT", [L, 128, 2048])
    ut_d = din("peer_uT", [L, 128, 128, 1024])
    v_d = din("peer_v", [L, 16384, D])
    ident_d = din("ident", [128, 128])
    iota_d = din("iota", [128, 128])
    tri_d = din("tri", [2, 128, 128])
    sel_d = din("sel", [2, 256])
    csm_d = din("cs_mla", [T, 64])
    css_d = din("cs_swa", [T, 128])
    y_d = nc.dram_tensor("y", [SEQ, D], F32, kind="ExternalOutput").ap()

    h_s = dscr("h_s", [T, D], F32)
    qTm_s = dscr("qTm_s", [8, 96, T], BF16)
    kTm_s = dscr("kTm_s", [8, 96, T], BF16)
    Vm_s = dscr("Vm_s", [T, 520], BF16)
    qTs_s = dscr("qTs_s", [8, 64, T], BF16)
    kTs_s = dscr("kTs_s", [2, 64, T], BF16)
    Vs_s = dscr("Vs_s", [T, 130], BF16)
    om_s = dscr("om_s", [T, D], BF16)
    utb_s = dscr("utb_s", [L, 128, 128, 1024], BF16)
    vb_s = dscr("vb_s", [L, 16384, D], BF16)

    S = Sched(nc, es)

    uid = [0]

    def sb(name, shape, dt, stack=None):
        uid[0] += 1
        return TT((stack or es).enter_context(nc.sbuf_tensor("%s_%d" % (name, uid[0]), list(shape), dt)))

    ps = [TT(es.enter_context(nc.psum_tensor("ps%d" % i, [128, 512], F32))) for i in range(8)]

    def psb(i):
        return ps[i][:].bitcast(BF16)

    ident_f = sb("ident_f", [128, 128], F32)
    ident_b = sb("ident_b", [128, 128], BF16)
    iota_f = sb("iota_f", [128, 128], F32)
    iota_b = sb("iota_b", [128, 128], BF16)
    tri_f = sb("tri_f", [128, 2, 128], F32)
    tri_b = sb("tri_b", [128, 2, 128], BF16)
    sel_f = sb("sel_f", [2, 256], F32)
    epsc = sb("epsc", [128, 1], F32)
    S.dma(ident_f[:], ident_d[:, :], W=[ident_f.b])
    S.dma(iota_f[:], iota_d[:, :], W=[iota_f.b])
    S.dma(tri_f[:], tri_d.rearrange("a p q -> p a q"), W=[tri_f.b])
    S.dma(sel_f[:], sel_d[:, :], W=[sel_f.b])
    S.op("dve", lambda e: e.tensor_copy(ident_b[:], ident_f[:]), R=[ident_f.b], W=[ident_b.b])
    S.op("dve", lambda e: e.tensor_copy(iota_b[:], iota_f[:]), R=[iota_f.b], W=[iota_b.b])
    S.op("dve", lambda e: e.tensor_copy(tri_b[:], tri_f[:]), R=[tri_f.b], W=[tri_b.b])
    S.op("dve", lambda e: e.memset(epsc[:], EPS), W=[epsc.b])

    modT = sb("modT", [128, 4, 8, 2], F32)
    grow = sb("grow", [2, 2 * D], F32)
    sT = sb("sT", [128, 8, 2], F32)
    esink = sb("esink", [128, 8], F32)
    tmp_es = contextlib.ExitStack()
    s2row = sb("s2row", [2, D], F32, tmp_es)

    S.dma(s2row[:], cvec_d[:, :], W=[s2row.b])
    S.op("act", lambda e: e.activation(out=s2row[:], in_=s2row[:], func=AF.Silu), R=[s2row.b], W=[s2row.b])
    for dc in range(8):
        S.op("pe", lambda e, dc=dc: e.transpose(ps[0][:, dc * 2:dc * 2 + 2], s2row[0:2, dc * 128:(dc + 1) * 128], ident_f[0:2, 0:2]),
             R=[s2row.b, ident_f.b], W=[ps[0].b])
    S.op("dve", lambda e: e.tensor_copy(sT[:].rearrange("p a b -> p (a b)"), ps[0][:, 0:16]), R=[ps[0].b], W=[sT.b])
    S.barrier()
    tmp_es.close()

    bg_list = []
    for l in range(L):
        for c in range(0, 128, 8):
            bg_list.append((utb_s[l, c:c + 8].rearrange("c p n -> (c p) n"), ut_d[l, c:c + 8].rearrange("c p n -> (c p) n")))
            bg_list.append((vb_s[l, c * 128:(c + 8) * 128, :], v_d[l, c * 128:(c + 8) * 128, :]))

    def bg_issue(n):
        for _ in range(n):
            if bg_list:
                o_, i_ = bg_list.pop(0)
                S.dma_untracked(o_, i_, "pool")

    def rstd_of(ph, x3, xb, H, Dh, tmp, ssq, rstd):
        tv = tmp[:, 0:H * Dh].rearrange("p (h d) -> p h d", h=H)
        S.op("dve", lambda e: e.tensor_tensor(out=tv, in0=x3, in1=x3, op=ALU.mult), R=[xb], W=[tmp.b])
        S.op("dve", lambda e: e.tensor_reduce(out=ssq[:, 0:H], in_=tv, axis=AX.X, op=ALU.add), R=[tmp.b], W=[ssq.b])
        S.op("act", lambda e: e.activation(out=rstd[:, 0:H], in_=ssq[:, 0:H], func=AF.Sqrt, scale=1.0 / Dh, bias=epsc[:, 0:1]),
             R=[ssq.b, epsc.b], W=[rstd.b])
        S.op("dve", lambda e: e.reciprocal(out=rstd[:, 0:H], in_=rstd[:, 0:H]), R=[rstd.b], W=[rstd.b])

    def rope(x5, xb, cst, cstb, H, q, t1, t2, out5, outb):
        cos4 = cst[:, 0, :].rearrange("p (a b q) -> p a b q", a=2, b=2)
        sin4 = cst[:, 1, :].rearrange("p (a b q) -> p a b q", a=2, b=2)
        n = H * 4 * q
        t1v = t1[:, 0:n].rearrange("p (h a b q) -> p h a b q", h=H, a=2, b=2)
        t2v = t2[:, 0:n].rearrange("p (h a b q) -> p h a b q", h=H, a=2, b=2)
        for b_ in range(2):
            cb = cos4[:, :, b_, :].unsqueeze(1).to_broadcast([128, H, 2, q])
            sbn = sin4[:, :, b_, :].unsqueeze(1).to_broadcast([128, H, 2, q])
            S.op("dve", lambda e, b_=b_, cb=cb: e.tensor_tensor(out=t1v[:, :, :, b_, :], in0=x5[:, :, :, b_, :], in1=cb, op=ALU.mult),
                 R=[xb, cstb], W=[t1.b])
            S.op("dve", lambda e, b_=b_, sbn=sbn: e.tensor_tensor(out=t2v[:, :, :, b_, :], in0=x5[:, :, :, 1 - b_, :], in1=sbn, op=ALU.mult),
                 R=[xb, cstb], W=[t2.b])
        for b_ in range(2):
            S.op("dve", lambda e, b_=b_: e.tensor_tensor(out=out5[:, :, :, b_, :], in0=t1v[:, :, :, b_, :], in1=t2v[:, :, :, b_, :], op=ALU.add),
                 R=[t1.b, t2.b], W=[outb])

    def load_w_bf16(ph, dst2d, src2d, ncols, stg, scale_ap=None, scale_b=None, k=[0]):
        for c0 in range(0, ncols, 2048):
            c1 = min(ncols, c0 + 2048)
            st = stg[k[0] % 2]
            k[0] += 1
            S.dma(st[:, 0:c1 - c0], src2d[:, c0:c1], W=[st.b])
            if scale_ap is None:
                S.op("act", lambda e, st=st, c0=c0, c1=c1: e.activation(out=dst2d[0][:, c0:c1], in_=st[:, 0:c1 - c0], func=AF.Copy),
                     R=[st.b], W=[dst2d[1]])
            else:
                S.op("dve", lambda e, st=st, c0=c0, c1=c1: e.tensor_scalar(out=dst2d[0][:, c0:c1], in0=st[:, 0:c1 - c0], scalar1=scale_ap,
                                                                         scalar2=None, op0=ALU.mult),
                     R=[st.b, scale_b], W=[dst2d[1]])

    def chk(l, phn):
        if stop is not None and stop == (l, phn):
            raise _Stop()

    for l in (range(L) if stop is None else range(stop[0] + 1)):
        last = l == L - 1
        with contextlib.ExitStack() as ph:
            aw = [sb("aw%d" % i, [128, 3072], F32, ph) for i in range(4)]
            modrow = sb("modrow", [2, 6 * D], F32, ph)
            abr = sb("abr", [2, 6 * D], F32, ph)
            ng = sb("ng", [2, 2, D], F32, ph)
            vrow = sb("vrow", [2, 4, D], F32, ph)
            S.dma(abr[:], ada_b[l:l + 1, :].to_broadcast([2, 6 * D]), W=[abr.b])
            S.dma(ng[:, 0, :], n1g[l:l + 1, :].to_broadcast([2, D]), W=[ng.b])
            S.dma(ng[:, 1, :], n2g[l:l + 1, :].to_broadcast([2, D]), W=[ng.b])
            S.dma(esink[:], sink_d[l:l + 1, :].to_broadcast([128, 8]), W=[esink.b])
            S.op("act", lambda e: e.activation(out=esink[:], in_=esink[:], func=AF.Exp), R=[esink.b], W=[esink.b])
            k = 0
            for half in range(2):
                for dc in range(8):
                    a = aw[k % 4]
                    k += 1
                    S.dma(a[:], ada_w[l, dc * 128:(dc + 1) * 128, half * 3072:(half + 1) * 3072], W=[a.b])
                    for cb in range(6):
                        S.op("pe", lambda e, a=a, cb=cb, dc=dc: e.matmul(ps[cb][0:2, :], lhsT=sT[:, dc, :], rhs=a[:, cb * 512:(cb + 1) * 512],
                                                                       start=(dc == 0), stop=(dc == 7)),
                             R=[a.b, sT.b], W=[ps[cb].b])
                for cb in range(6):
                    c0 = half * 3072 + cb * 512
                    S.op("dve", lambda e, cb=cb, c0=c0: e.tensor_tensor(out=modrow[:, c0:c0 + 512], in0=ps[cb][0:2, :], in1=abr[:, c0:c0 + 512], op=ALU.add),
                         R=[ps[cb].b, abr.b], W=[modrow.b])
            for j, (sci, shi) in enumerate(((1, 0), (4, 3))):
                S.op("dve", lambda e, j=j, sci=sci: e.scalar_tensor_tensor(out=vrow[:, 2 * j, :], in0=modrow[:, sci * D:(sci + 1) * D], scalar=1.0,
                                                                          in1=ng[:, j, :], op0=ALU.add, op1=ALU.mult),
                     R=[modrow.b, ng.b], W=[vrow.b])
                S.op("dve", lambda e, j=j, shi=shi: e.tensor_copy(vrow[:, 2 * j + 1, :], modrow[:, shi * D:(shi + 1) * D]),
                     R=[modrow.b], W=[vrow.b])
            S.op("dve", lambda e: e.tensor_copy(grow[:, 0:D], modrow[:, 2 * D:3 * D]), R=[modrow.b], W=[grow.b])
            S.op("dve", lambda e: e.tensor_copy(grow[:, D:2 * D], modrow[:, 5 * D:6 * D]), R=[modrow.b], W=[grow.b])
            for v in range(4):
                for dc in range(8):
                    o = (v * 8 + dc) * 2
                    S.op("pe", lambda e, v=v, dc=dc, o=o: e.transpose(ps[6][:, o:o + 2], vrow[0:2, v, dc * 128:(dc + 1) * 128], ident_f[0:2, 0:2]),
                         R=[vrow.b, ident_f.b], W=[ps[6].b])
            S.op("dve", lambda e: e.tensor_copy(modT[:].rearrange("p a b c -> p (a b c)"), ps[6][:, 0:64]), R=[ps[6].b], W=[modT.b])
            S.barrier()
        chk(l, "A")

        def gate_bcast(dst, gi, j):
            for hf in range(2):
                S.op("pe", lambda e, hf=hf: e.matmul(ps[6 + hf][:, :], lhsT=sel_f[0:2, j * 128:(j + 1) * 128],
                                                    rhs=grow[0:2, gi * D + hf * 512: gi * D + (hf + 1) * 512], start=True, stop=True),
                     R=[sel_f.b, grow.b], W=[ps[6 + hf].b])
                S.op("act", lambda e, hf=hf: e.activation(out=dst[:, hf * 512:(hf + 1) * 512], in_=ps[6 + hf][:, :], func=AF.Copy),
                     R=[ps[6 + hf].b], W=[dst.b])

        def modulate_T(ht, xn, sqj, ssq, rstd, aT_ap, aT_b, vA, j, psbank):
            rstd_of(None, ht[:].rearrange("p (h d) -> p h d", h=1), ht.b, 1, D, sqj, ssq, rstd)
            S.op("dve", lambda e: e.tensor_scalar(out=xn[:], in0=ht[:], scalar1=rstd[:, 0:1], scalar2=None, op0=ALU.mult),
                 R=[ht.b, rstd.b], W=[xn.b])
            pv = psb(psbank)
            for dc in range(8):
                S.op("pe", lambda e, dc=dc: e.transpose(pv[:, dc * 128:(dc + 1) * 128], xn[:, dc * 128:(dc + 1) * 128], ident_b[:]),
                     R=[xn.b, ident_b.b], W=[ps[psbank].b])
            for dc in range(8):
                S.op("dve", lambda e, dc=dc: e.tensor_scalar(out=aT_ap[:, dc, :], in0=pv[:, dc * 128:(dc + 1) * 128],
                                                            scalar1=modT[:, vA, dc, j:j + 1], scalar2=modT[:, vA + 1, dc, j:j + 1],
                                                            op0=ALU.mult, op1=ALU.add),
                     R=[ps[psbank].b, modT.b], W=[aT_b])

        with contextlib.ExitStack() as ph:
            stg = [sb("stg%d" % i, [128, 2048], F32, ph) for i in range(2)]
            w_in_sb = sb("w_in_sb", [128, 8, DIN], BF16, ph)
            wuq_sb = sb("wuq_sb", [128, 3, 768], BF16, ph)
            wukv_sb = sb("wukv_sb", [128, 2, 1024], BF16, ph)
            gq = sb("gq", [128, 3], F32, ph)
            gkv = sb("gkv", [128, 2], F32, ph)
            g_mq = sb("g_mq", [128, 96], F32, ph)
            g_mk = sb("g_mk", [128, 96], F32, ph)
            g_sq = sb("g_sq", [128, 64], F32, ph)
            g_sk = sb("g_sk", [128, 64], F32, ph)
            S.dma(gq[:], qa_g[l], W=[gq.b])
            S.dma(gkv[:], kva_g[l], W=[gkv.b])
            S.dma(g_mq[:], mqn_g[l:l + 1, :].to_broadcast([128, 96]), W=[g_mq.b])
            S.dma(g_mk[:], mkn_g[l:l + 1, :].to_broadcast([128, 96]), W=[g_mk.b])
            S.dma(g_sq[:], sqn_g[l:l + 1, :].to_broadcast([128, 64]), W=[g_sq.b])
            S.dma(g_sk[:], skn_g[l:l + 1, :].to_broadcast([128, 64]), W=[g_sk.b])
            for dc in range(8):
                load_w_bf16(ph, (w_in_sb[:, dc, :], w_in_sb.b), w_in[l, dc * 128:(dc + 1) * 128, :], DIN, stg)
            for kc in range(3):
                load_w_bf16(ph, (wuq_sb[:, kc, :], wuq_sb.b), wuq[l, kc * 128:(kc + 1) * 128, :], 768, stg, gq[:, kc:kc + 1], gq.b)
            for kc in range(2):
                load_w_bf16(ph, (wukv_sb[:, kc, :], wukv_sb.b), wukv[l, kc * 128:(kc + 1) * 128, :], 1024, stg, gkv[:, kc:kc + 1], gkv.b)

            ht = [sb("ht%d" % i, [128, D], F32, ph) for i in range(2)]
            cstm = [sb("cstm%d" % i, [128, 2, 32], F32, ph) for i in range(2)]
            csts = [sb("csts%d" % i, [128, 2, 64], F32, ph) for i in range(2)]
            def mkset(par):
                d_ = {}
                d_["sqj"] = sb("sqj%d" % par, [128, D], F32, ph)
                d_["t1"] = sb("t1%d" % par, [128, D], F32, ph)
                d_["t2"] = sb("t2%d" % par, [128, D], F32, ph)
                d_["xn"] = sb("xn%d" % par, [128, D], BF16, ph)
                d_["aT"] = sb("aT%d" % par, [128, 8, 128], BF16, ph)
                d_["p_sb"] = sb("p_sb%d" % par, [128, DIN], F32, ph)
                d_["qan"] = sb("qan%d" % par, [128, 384], BF16, ph)
                d_["kvan"] = sb("kvan%d" % par, [128, 256], BF16, ph)
                d_["lT"] = sb("lT%d" % par, [128, 5, 128], BF16, ph)
                d_["qm"] = sb("qm%d" % par, [128, 8, 96], F32, ph)
                d_["kv"] = sb("kv%d" % par, [128, 8, 128], F32, ph)
                d_["kf"] = sb("kf%d" % par, [128, 8, 96], F32, ph)
                d_["qn"] = sb("qn%d" % par, [128, 8, 96], F32, ph)
                d_["kn"] = sb("kn%d" % par, [128, 8, 96], F32, ph)
                d_["sqn"] = sb("sqn%d" % par, [128, 8, 64], F32, ph)
                d_["skn"] = sb("skn%d" % par, [128, 2, 64], F32, ph)
                d_["qo"] = sb("qo%d" % par, [128, 8, 96], BF16, ph)
                d_["ko"] = sb("ko%d" % par, [128, 8, 96], BF16, ph)
                d_["sqo"] = sb("sqo%d" % par, [128, 8, 64], BF16, ph)
                d_["sko"] = sb("sko%d" % par, [128, 2, 64], BF16, ph)
                d_["qT_sb"] = sb("qT_sb%d" % par, [128, 8, 128], BF16, ph)
                d_["kT_sb"] = sb("kT_sb%d" % par, [128, 8, 128], BF16, ph)
                d_["sqT_sb"] = sb("sqT_sb%d" % par, [128, 8, 128], BF16, ph)
                d_["skT_sb"] = sb("skT_sb%d" % par, [128, 2, 128], BF16, ph)
                return d_

            BS = [mkset(0), mkset(1)]
            Vm_sb = [sb("Vm_sb%d" % i, [128, 8, 65], BF16, ph) for i in range(2)]
            Vs_sb = [sb("Vs_sb%d" % i, [128, 2, 65], BF16, ph) for i in range(2)]
            for par_ in range(2):
                BS[par_]["ssq"] = sb("ssq%d" % par_, [128, 16], F32, ph)
                BS[par_]["rstd"] = sb("rstd%d" % par_, [128, 16], F32, ph)
            for i in range(2):
                S.op("pool", lambda e, i=i: e.memset(Vm_sb[i][:], 1.0), W=[Vm_sb[i].b])
                S.op("pool", lambda e, i=i: e.memset(Vs_sb[i][:], 1.0), W=[Vs_sb[i].b])

            def loads(i):
                hb = ht[i % 2]
                if l == 0:
                    src = ctx_d[i * 128:(i + 1) * 128, :] if i < 2 else x_d[(i - 2) * 128:(i - 1) * 128, :]
                else:
                    src = h_s[i * 128:(i + 1) * 128, :]
                S.dma(hb[:], src, W=[hb.b])
                S.dma(cstm[i % 2][:].rearrange("p a b -> p (a b)"), csm_d[i * 128:(i + 1) * 128, :], W=[cstm[i % 2].b])
                S.dma(csts[i % 2][:].rearrange("p a b -> p (a b)"), css_d[i * 128:(i + 1) * 128, :], W=[csts[i % 2].b])

            def tile_body(i, par):
                d_ = BS[par]
                sqj, t1, t2, xn, aT, p_sb, qan, kvan, lT, qm, kv, kf, qn, kn, sqn, skn, qo, ko, sqo, sko, qT_sb, kT_sb, sqT_sb, skT_sb, ssq, rstd = d_["sqj"], d_["t1"], d_["t2"], d_["xn"], d_["aT"], d_["p_sb"], d_["qan"], d_["kvan"], d_["lT"], d_["qm"], d_["kv"], d_["kf"], d_["qn"], d_["kn"], d_["sqn"], d_["skn"], d_["qo"], d_["ko"], d_["sqo"], d_["sko"], d_["qT_sb"], d_["kT_sb"], d_["sqT_sb"], d_["skT_sb"], d_["ssq"], d_["rstd"]
                ps_ = ps[4 * par:] + ps[:4 * par]
                psb_ = lambda k_: psb((k_ + 4 * par) % 8)
                j = 1 if i < 2 else 0
                hb = ht[i % 2]
                cm = cstm[i % 2]
                cs_ = csts[i % 2]
                tsl = slice(i * 128, (i + 1) * 128)
                modulate_T(hb, xn, sqj, ssq, rstd, aT[:], aT.b, 0, j, (4 * par) % 8)
                for cb, (c0, c1) in enumerate(((0, 512), (512, 1024), (1024, DIN))):
                    for dc in range(8):
                        S.op("pe", lambda e, cb=cb, c0=c0, c1=c1, dc=dc: e.matmul(ps_[1 + cb][:, 0:c1 - c0], lhsT=aT[:, dc, :], rhs=w_in_sb[:, dc, c0:c1],
                                                                              start=(dc == 0), stop=(dc == 7)),
                             R=[aT.b, w_in_sb.b], W=[ps_[1 + cb].b], inc=(dc == 7))
                    S.op("act", lambda e, cb=cb, c0=c0, c1=c1: e.activation(out=p_sb[:, c0:c1], in_=ps_[1 + cb][:, 0:c1 - c0], func=AF.Copy),
                         R=[ps_[1 + cb].b], W=[p_sb.b])
                rstd_of(ph, p_sb[:, 0:384].rearrange("p (h d) -> p h d", h=1), p_sb.b, 1, 384, sqj, ssq, rstd)
                S.op("dve", lambda e: e.tensor_scalar(out=qan[:], in0=p_sb[:, 0:384], scalar1=rstd[:, 0:1], scalar2=None, op0=ALU.mult),
                     R=[p_sb.b, rstd.b], W=[qan.b])
                rstd_of(ph, p_sb[:, 896:1152].rearrange("p (h d) -> p h d", h=1), p_sb.b, 1, 256, sqj, ssq, rstd)
                S.op("dve", lambda e: e.tensor_scalar(out=kvan[:], in0=p_sb[:, 896:1152], scalar1=rstd[:, 0:1], scalar2=None, op0=ALU.mult),
                     R=[p_sb.b, rstd.b], W=[kvan.b])
                pv4 = psb_(4)
                for kc in range(3):
                    S.op("pe", lambda e, kc=kc: e.transpose(pv4[:, kc * 128:(kc + 1) * 128], qan[:, kc * 128:(kc + 1) * 128], ident_b[:]),
                         R=[qan.b, ident_b.b], W=[ps_[4].b])
                for kc in range(2):
                    S.op("pe", lambda e, kc=kc: e.transpose(pv4[:, (3 + kc) * 128:(4 + kc) * 128], kvan[:, kc * 128:(kc + 1) * 128], ident_b[:]),
                         R=[kvan.b, ident_b.b], W=[ps_[4].b])
                S.op("act", lambda e: e.activation(out=lT[:].rearrange("p a b -> p (a b)"), in_=pv4[:, 0:640], func=AF.Copy), R=[ps_[4].b], W=[lT.b])
                for cb, (c0, c1) in enumerate(((0, 512), (512, 768))):
                    for kc in range(3):
                        S.op("pe", lambda e, cb=cb, c0=c0, c1=c1, kc=kc: e.matmul(ps_[5 + cb][:, 0:c1 - c0], lhsT=lT[:, kc, :], rhs=wuq_sb[:, kc, c0:c1],
                                                                              start=(kc == 0), stop=(kc == 2)),
                             R=[lT.b, wuq_sb.b], W=[ps_[5 + cb].b], inc=(kc == 2))
                    S.op("act", lambda e, cb=cb, c0=c0, c1=c1: e.activation(out=qm[:].rearrange("p a b -> p (a b)")[:, c0:c1], in_=ps_[5 + cb][:, 0:c1 - c0], func=AF.Copy),
                         R=[ps_[5 + cb].b], W=[qm.b])
                for cb in range(2):
                    for kc in range(2):
                        S.op("pe", lambda e, cb=cb, kc=kc: e.matmul(ps_[1 + cb][:, :], lhsT=lT[:, 3 + kc, :], rhs=wukv_sb[:, kc, cb * 512:(cb + 1) * 512],
                                                                start=(kc == 0), stop=(kc == 1)),
                             R=[lT.b, wukv_sb.b], W=[ps_[1 + cb].b], inc=(kc == 1))
                    S.op("act", lambda e, cb=cb: e.activation(out=kv[:].rearrange("p a b -> p (a b)")[:, cb * 512:(cb + 1) * 512], in_=ps_[1 + cb][:, :], func=AF.Copy),
                         R=[ps_[1 + cb].b], W=[kv.b])
                rstd_of(ph, qm[:], qm.b, 8, 96, sqj, ssq, rstd)
                S.op("dve", lambda e: e.tensor_tensor(out=qn[:], in0=qm[:], in1=rstd[:, 0:8].unsqueeze(2).to_broadcast([128, 8, 96]), op=ALU.mult),
                     R=[qm.b, rstd.b], W=[qn.b])
                S.op("dve", lambda e: e.tensor_tensor(out=qn[:], in0=qn[:], in1=g_mq[:].unsqueeze(1).to_broadcast([128, 8, 96]), op=ALU.mult),
                     R=[qn.b, g_mq.b], W=[qn.b])
                S.op("dve", lambda e: e.tensor_copy(qo[:, :, 0:64], qn[:, :, 0:64]), R=[qn.b], W=[qo.b])
                rope(qn[:, :, 64:96].rearrange("p h (a b q) -> p h a b q", a=2, b=2), qn.b, cm, cm.b, 8, 8, t1, t2,
                     qo[:, :, 64:96].rearrange("p h (a b q) -> p h a b q", a=2, b=2), qo.b)
                S.op("dve", lambda e: e.tensor_copy(kf[:, :, 0:64], kv[:, :, 0:64]), R=[kv.b], W=[kf.b])
                S.op("dve", lambda e: e.tensor_copy(kf[:, :, 64:96], p_sb[:, 1152:1184].unsqueeze(1).to_broadcast([128, 8, 32])), R=[p_sb.b], W=[kf.b])
                rstd_of(ph, kf[:], kf.b, 8, 96, sqj, ssq, rstd)
                S.op("dve", lambda e: e.tensor_tensor(out=kn[:], in0=kf[:], in1=rstd[:, 0:8].unsqueeze(2).to_broadcast([128, 8, 96]), op=ALU.mult),
                     R=[kf.b, rstd.b], W=[kn.b])
                S.op("dve", lambda e: e.tensor_tensor(out=kn[:], in0=kn[:], in1=g_mk[:].unsqueeze(1).to_broadcast([128, 8, 96]), op=ALU.mult),
                     R=[kn.b, g_mk.b], W=[kn.b])
                S.op("dve", lambda e: e.tensor_copy(ko[:, :, 0:64], kn[:, :, 0:64]), R=[kn.b], W=[ko.b])
                rope(kn[:, :, 64:96].rearrange("p h (a b q) -> p h a b q", a=2, b=2), kn.b, cm, cm.b, 8, 8, t1, t2,
                     ko[:, :, 64:96].rearrange("p h (a b q) -> p h a b q", a=2, b=2), ko.b)
                vmb = Vm_sb[i % 2]
                S.op("act", lambda e, vmb=vmb: e.activation(out=vmb[:, :, 0:64], in_=kv[:, :, 64:128], func=AF.Copy), R=[kv.b], W=[vmb.b])
                S.dma(Vm_s[tsl, :], vmb[:].rearrange("p a b -> p (a b)"), R=[vmb.b])
                sq3 = p_sb[:, 384:896].rearrange("p (h d) -> p h d", h=8)
                rstd_of(ph, sq3, p_sb.b, 8, 64, sqj, ssq, rstd)
                S.op("dve", lambda e: e.tensor_tensor(out=sqn[:], in0=sq3, in1=rstd[:, 0:8].unsqueeze(2).to_broadcast([128, 8, 64]), op=ALU.mult),
                     R=[p_sb.b, rstd.b], W=[sqn.b])
                S.op("dve", lambda e: e.tensor_tensor(out=sqn[:], in0=sqn[:], in1=g_sq[:].unsqueeze(1).to_broadcast([128, 8, 64]), op=ALU.mult),
                     R=[sqn.b, g_sq.b], W=[sqn.b])
                rope(sqn[:].rearrange("p h (a b q) -> p h a b q", a=2, b=2), sqn.b, cs_, cs_.b, 8, 16, t1, t2,
                     sqo[:].rearrange("p h (a b q) -> p h a b q", a=2, b=2), sqo.b)
                sk3 = p_sb[:, 1184:1312].rearrange("p (h d) -> p h d", h=2)
                rstd_of(ph, sk3, p_sb.b, 2, 64, sqj, ssq, rstd)
                S.op("dve", lambda e: e.tensor_tensor(out=skn[:], in0=sk3, in1=rstd[:, 0:2].unsqueeze(2).to_broadcast([128, 2, 64]), op=ALU.mult),
                     R=[p_sb.b, rstd.b], W=[skn.b])
                S.op("dve", lambda e: e.tensor_tensor(out=skn[:], in0=skn[:], in1=g_sk[:].unsqueeze(1).to_broadcast([128, 2, 64]), op=ALU.mult),
                     R=[skn.b, g_sk.b], W=[skn.b])
                rope(skn[:].rearrange("p h (a b q) -> p h a b q", a=2, b=2), skn.b, cs_, cs_.b, 2, 16, t1, t2,
                     sko[:].rearrange("p h (a b q) -> p h a b q", a=2, b=2), sko.b)
                vsb = Vs_sb[i % 2]
                S.op("act", lambda e, vsb=vsb: e.activation(out=vsb[:, :, 0:64], in_=p_sb[:, 1312:1440].rearrange("p (h d) -> p h d", h=2), func=AF.Copy),
                     R=[p_sb.b], W=[vsb.b])
                S.dma(Vs_s[tsl, :], vsb[:].rearrange("p a b -> p (a b)"), R=[vsb.b])
                for (src, dstT, nh, dh, bank, scr) in ((qo, qT_sb, 8, 96, 7, qTm_s), (ko, kT_sb, 8, 96, 0, kTm_s),
                                                       (sqo, sqT_sb, 8, 64, 4, qTs_s), (sko, skT_sb, 2, 64, 3, kTs_s)):
                    pvv = psb_(bank)
                    for h in range(nh):
                        S.op("pe", lambda e, src=src, h=h, dh=dh, pvv=pvv: e.transpose(pvv[0:dh, h * 128:(h + 1) * 128], src[:, h, :], ident_b[:]),
                             R=[src.b, ident_b.b], W=[ps_[bank].b])
                    S.op("act", lambda e, dstT=dstT, nh=nh, dh=dh, pvv=pvv: e.activation(out=dstT[0:dh, 0:nh, :].rearrange("p a b -> p (a b)"),
                                                                                       in_=pvv[0:dh, 0:nh * 128], func=AF.Copy),
                         R=[ps_[bank].b], W=[dstT.b])
                    S.dma(scr[:, :, tsl].rearrange("h d t -> d h t"), dstT[0:dh, 0:nh, :], R=[dstT.b])
                if i + 2 < NT:
                    loads(i + 2)

            loads(0)
            loads(1)
            for pr in range(NT // 2):
                bg_issue(2)
                recs = []
                for par in range(2):
                    S.rec = []
                    tile_body(2 * pr + par, par)
                    recs.append(S.rec)
                    S.rec = None
                for n_ in range(max(len(recs[0]), len(recs[1]))):
                    for par in range(2):
                        if n_ < len(recs[par]):
                            recs[par][n_]()
            S.barrier()
        chk(l, "B")

        with contextlib.ExitStack() as ph:
            kT_all = [sb("kTa%d" % h, [128, T], BF16, ph) for h in range(8)]
            Vall = sb("Vall", [128, NT, 520], BF16, ph)
            kTs_all = sb("kTs_all", [64, 2, T], BF16, ph)
            Vs_all = sb("Vs_all", [128, NT, 130], BF16, ph)
            qTg = [sb("qTg%d" % i, [128, 8, 512], BF16, ph) for i in range(2)]
            sqTg = [sb("sqTg%d" % i, [64, 8, 512], BF16, ph) for i in range(2)]
            PT = [sb("PT%d" % i, [128, 512], BF16, ph) for i in range(4)]
            obuf = [sb("obuf0", [128, 4, D], BF16, ph)]
            rden = sb("rden", [128, 4], F32, ph)
            for h in range(8):
                S.dma(kT_all[h][0:96, :], kTm_s[h], W=[kT_all[h].b])
            S.dma(Vall[:], Vm_s.rearrange("(k p) e -> p k e", p=128), W=[Vall.b])
            S.dma(kTs_all[:], kTs_s.rearrange("g d t -> d g t"), W=[kTs_all.b])
            S.dma(Vs_all[:], Vs_s.rearrange("(k p) e -> p k e", p=128), W=[Vs_all.b])
            groups = ([] if last else [(0, 256)]) + [(256 + 512 * g, 512) for g in range(8)]
            ctr = {"s": 0, "p": 0, "o": 0}

            def qloads(gi):
                t0, nq = groups[gi]
                S.dma(qTg[gi % 2][0:96, :, 0:nq], qTm_s[:, :, t0:t0 + nq].rearrange("h d t -> d h t"), W=[qTg[gi % 2].b])
                S.dma(sqTg[gi % 2][:, :, 0:nq], qTs_s[:, :, t0:t0 + nq].rearrange("h d t -> d h t"), W=[sqTg[gi % 2].b])

            qloads(0)
            for gi, (t0, nq) in enumerate(groups):
                bg_issue(4)
                if gi + 1 < len(groups):
                    qloads(gi + 1)
                isctx = t0 < 256
                nblk = nq // 128
                qg = qTg[gi % 2]
                sg = sqTg[gi % 2]
                ob = obuf[0]
                kts = [0, 1] if isctx else list(range(NT))
                its = []
                for h in range(8):
                    for ki, kt in enumerate(kts):
                        its.append(("m", h, ki, kt, len(kts), None, None))
                for blk in range(nblk):
                    ti = t0 // 128 + blk
                    if isctx:
                        kl = [0, 1]
                    else:
                        kl = [0, 1] + ([ti - 1] if ti - 1 >= 2 else []) + [ti] + ([ti + 1] if ti + 1 < NT else [])
                    for g2 in range(2):
                        for ki, kt in enumerate(kl):
                            its.append(("s", g2, ki, kt, len(kl), blk, ti))

                def emitS(n):
                    kind, a, ki, kt, nk, blk, ti = its[n]
                    pS = ps[n % 4]
                    if kind == "m":
                        S.op("pe", lambda e: e.matmul(pS[:, 0:nq], lhsT=kT_all[a][0:96, kt * 128:(kt + 1) * 128], rhs=qg[0:96, a, 0:nq], start=True, stop=True),
                             R=[kT_all[a].b, qg.b], W=[pS.b])
                    else:
                        S.op("pe", lambda e: e.matmul(pS[:, :].rearrange("p (a b) -> p a b", a=4), lhsT=kTs_all[:, a, kt * 128:(kt + 1) * 128],
                                                      rhs=sg[:, 4 * a:4 * a + 4, blk * 128:(blk + 1) * 128], start=True, stop=True),
                             R=[kTs_all.b, sg.b], W=[pS.b])

                po_of = {}

                def emitRest(n):
                    kind, a, ki, kt, nk, blk, ti = its[n]
                    pS = ps[n % 4]
                    pt = PT[n % 4]
                    if ki == 0:
                        po_of["cur"] = ps[4 + ctr["o"] % 2]
                        ctr["o"] += 1
                    po = po_of["cur"]
                    if kind == "m":
                        S.op("act", lambda e: e.activation(out=pt[:, 0:nq], in_=pS[:, 0:nq], func=AF.Exp, scale=96.0 ** -0.5), R=[pS.b], W=[pt.b])
                        for qs in range(nblk):
                            S.op("pe", lambda e, qs=qs: e.matmul(po[:, qs * 65:(qs + 1) * 65], lhsT=pt[:, qs * 128:(qs + 1) * 128], rhs=Vall[:, kt, a * 65:(a + 1) * 65],
                                                                start=(ki == 0 and qs == 0), stop=(ki == nk - 1), skip_group_check=True),
                                 R=[pt.b, Vall.b], W=[po.b], inc=(qs == nblk - 1))
                        if ki == nk - 1:
                            po3 = po[:, 0:nblk * 65].rearrange("p (a b) -> p a b", b=65)
                            S.op("dve", lambda e: e.reciprocal(out=rden[:, 0:nblk], in_=po3[:, :, 64]), R=[po.b], W=[rden.b])
                            S.op("dve", lambda e: e.tensor_tensor(out=ob[:, 0:nblk, a * 64:(a + 1) * 64], in0=po3[:, :, 0:64],
                                                                  in1=rden[:, 0:nblk].unsqueeze(2).to_broadcast([128, nblk, 64]), op=ALU.mult),
                                 R=[po.b, rden.b], W=[ob.b])
                    else:
                        S.op("act", lambda e: e.activation(out=pt[:, :], in_=pS[:, :], func=AF.Exp, scale=0.125), R=[pS.b], W=[pt.b])
                        if (not isctx) and kt >= 2 and kt != ti:
                            mi = 0 if kt == ti - 1 else 1
                            S.op("pool", lambda e: e.tensor_tensor(out=pt[:, :].rearrange("p (a b) -> p a b", a=4), in0=pt[:, :].rearrange("p (a b) -> p a b", a=4),
                                                                   in1=tri_b[:, mi, :].unsqueeze(1).to_broadcast([128, 4, 128]), op=ALU.mult),
                                 R=[pt.b, tri_b.b], W=[pt.b])
                        for r in range(4):
                            S.op("pe", lambda e, r=r: e.matmul(po[:, r * 65:(r + 1) * 65], lhsT=pt[:, r * 128:(r + 1) * 128], rhs=Vs_all[:, kt, a * 65:(a + 1) * 65],
                                                              start=(ki == 0 and r == 0), stop=(ki == nk - 1), skip_group_check=True),
                                 R=[pt.b, Vs_all.b], W=[po.b], inc=(r == 3))
                        if ki == nk - 1:
                            po3 = po[:, 0:260].rearrange("p (a b) -> p a b", b=65)
                            S.op("dve", lambda e: e.tensor_tensor(out=rden[:, 0:4], in0=po3[:, :, 64], in1=esink[:, 4 * a:4 * a + 4], op=ALU.add),
                                 R=[po.b, esink.b], W=[rden.b])
                            S.op("dve", lambda e: e.reciprocal(out=rden[:, 0:4], in_=rden[:, 0:4]), R=[rden.b], W=[rden.b])
                            S.op("dve", lambda e: e.tensor_tensor(out=ob[:, blk, 512 + a * 256:512 + (a + 1) * 256].rearrange("p (a b) -> p a b", a=4), in0=po3[:, :, 0:64],
                                                                  in1=rden[:, 0:4].unsqueeze(2).to_broadcast([128, 4, 64]), op=ALU.mult),
                                 R=[po.b, rden.b], W=[ob.b])

                LA = 3
                for n in range(min(LA, len(its))):
                    emitS(n)
                for n in range(len(its)):
                    if n + LA < len(its):
                        emitS(n + LA)
                    emitRest(n)
                S.dma(om_s[t0:t0 + nq, :].rearrange("(b p) c -> p b c", p=128), ob[:, 0:nblk, :], R=[ob.b])
            S.barrier()
        chk(l, "C")

        with contextlib.ExitStack() as ph:
            stg = [sb("stgd%d" % i, [128, 2048], F32, ph) for i in range(2)]
            w_out_sb = sb("w_out_sb", [128, 8, D], BF16, ph)
            for kc in range(8):
                load_w_bf16(ph, (w_out_sb[:, kc, :], w_out_sb.b), w_out[l, kc * 128:(kc + 1) * 128, :], D, stg)
            gb = [sb("gbd%d" % j, [128, D], F32, ph) for j in range(2)]
            gate_bcast(gb[0], 0, 0)
            gate_bcast(gb[1], 0, 1)
            htd = [sb("htd%d" % i, [128, D], F32, ph) for i in range(2)]
            omb = [sb("omb%d" % i, [128, D], BF16, ph) for i in range(2)]
            oT = sb("oT", [128, 8, 128], BF16, ph)
            h1 = [sb("h1d%d" % i, [128, D], F32, ph) for i in range(2)]
            tiles = list(range(2 if last else 0, NT))

            def loadsD(i):
                if l == 0:
                    src = ctx_d[i * 128:(i + 1) * 128, :] if i < 2 else x_d[(i - 2) * 128:(i - 1) * 128, :]
                else:
                    src = h_s[i * 128:(i + 1) * 128, :]
                S.dma(htd[i % 2][:], src, W=[htd[i % 2].b])
                S.dma(omb[i % 2][:], om_s[i * 128:(i + 1) * 128, :], W=[omb[i % 2].b])

            loadsD(tiles[0])
            for n_, i in enumerate(tiles):
                if n_ + 1 < len(tiles):
                    loadsD(tiles[n_ + 1])
                j = 1 if i < 2 else 0
                pv = psb(0)
                for kc in range(8):
                    S.op("pe", lambda e, kc=kc, i=i: e.transpose(pv[:, kc * 128:(kc + 1) * 128], omb[i % 2][:, kc * 128:(kc + 1) * 128], ident_b[:]),
                         R=[omb[i % 2].b, ident_b.b], W=[ps[0].b])
                S.op("act", lambda e: e.activation(out=oT[:].rearrange("p a b -> p (a b)"), in_=pv[:, :], func=AF.Copy), R=[ps[0].b], W=[oT.b])
                hh = h1[i % 2]
                for hf in range(2):
                    for kc in range(8):
                        S.op("pe", lambda e, hf=hf, kc=kc: e.matmul(ps[1 + hf][:, :], lhsT=oT[:, kc, :], rhs=w_out_sb[:, kc, hf * 512:(hf + 1) * 512],
                                                                start=(kc == 0), stop=(kc == 7)),
                             R=[oT.b, w_out_sb.b], W=[ps[1 + hf].b], inc=(kc == 7))
                    S.op("dve", lambda e, hf=hf, j=j, hh=hh: e.tensor_tensor(out=hh[:, hf * 512:(hf + 1) * 512], in0=ps[1 + hf][:, :],
                                                                          in1=gb[j][:, hf * 512:(hf + 1) * 512], op=ALU.mult),
                         R=[ps[1 + hf].b, gb[j].b], W=[hh.b])
                S.op("pool", lambda e, hh=hh, i=i: e.tensor_tensor(out=hh[:], in0=hh[:], in1=htd[i % 2][:], op=ALU.add), R=[hh.b, htd[i % 2].b], W=[hh.b])
                S.dma(h_s[i * 128:(i + 1) * 128, :], hh[:], R=[hh.b])
            bg_issue(len(bg_list))
            S.barrier(bg=True)
        chk(l, "D")

        with contextlib.ExitStack() as ph:
            s_sb = sb("s_sb", [128, 2048], F32, ph)
            cand = sb("cand", [128, 2048], F32, ph)
            stg = [s_sb, cand]
            wq_sb = sb("wq_sb", [128, 8, 2048], BF16, ph)
            keysT_sb = sb("keysT_sb", [128, 2048], BF16, ph)
            for dc in range(8):
                load_w_bf16(ph, (wq_sb[:, dc, :], wq_sb.b), wq_d[l, dc * 128:(dc + 1) * 128, :], 2048, stg)
            load_w_bf16(ph, (keysT_sb[:, :], keysT_sb.b), keysT_d[l], 2048, stg)
            gbe = sb("gbe", [128, D], F32, ph)
            gate_bcast(gbe, 1, 0 if last else 1)
            WtS = sb("WtS", [128, 256, 128], BF16, ph)
            Ab = [sb("Ab%d" % i, [128, 8, 128], BF16, ph) for i in range(2)]
            Bb = [sb("Bb%d" % i, [128, 8, 128], BF16, ph) for i in range(2)]
            utc = [sb("utc%d" % i, [128, 8, 128], BF16, ph) for i in range(6)]
            vc = [sb("vc%d" % i, [128, D], BF16, ph) for i in range(4)]
            h1e = [[sb("h1e%d_%d" % (p_, i), [128, D], F32, ph) for i in range(2)] for p_ in range(2)]
            xn = sb("xne", [128, D], BF16, ph)
            bT = [sb("bT%d" % p_, [128, 8, 256], BF16, ph) for p_ in range(2)]
            qTe = sb("qTe", [128, 16, 256], BF16, ph)
            sv = sb("sv", [128, 16, 16], F32, ph)
            si_u = sb("si_u", [128, 16, 16], U32, ph)
            si_f = sb("si_f", [128, 16, 16], F32, ph)
            cv = sb("cv", [128, 8, 16], F32, ph)
            ci_u = sb("ci_u", [128, 8, 16], U32, ph)
            k_u = sb("k_u", [128, 2, 128], U32, ph)
            k_f = sb("k_f", [128, 2, 128], F32, ph)
            IG = sb("IG", [128, 3, 128], F32, ph)
            IGT = [sb("IGT%d" % p_, [128, 3, 256], BF16, ph) for p_ in range(2)]
            gsum = sb("gsum", [128, 8], F32, ph)
            ssq = sb("ssqe", [128, 16], F32, ph)
            rstd = sb("rstde", [128, 16], F32, ph)
            ga = [sb("ga%d" % i, [128, 256], BF16, ph) for i in range(2)]
            GT = [sb("GT%d" % i, [128, 256], BF16, ph) for i in range(3)]
            iota16 = iota_f[:, 0:16]
            S.barrier()
            s_b = [Buf() for _ in range(16)]
            c_b = [Buf() for _ in range(16)]
            sv_b = [Buf() for _ in range(16)]
            siu_b = [Buf() for _ in range(16)]
            cv_b = [Buf() for _ in range(8)]
            ciu_b = [Buf() for _ in range(8)]
            s3 = s_sb[:].rearrange("p (a b) -> p a b", a=16)
            c3s = cand[:].rearrange("p (a b) -> p a b", a=16)
            cg3 = cand[:].rearrange("p (h a) -> p h a", h=8)
            sg3 = s_sb[:].rearrange("p (h a) -> p h a", h=8)

            def front(g, par):
                j = 1 if g == 0 else 0
                bTp = bT[par]
                for tt in range(2):
                    i = g * 2 + tt
                    S.dma(h1e[par][tt][:], h_s[i * 128:(i + 1) * 128, :], W=[h1e[par][tt].b])
                    modulate_T(h1e[par][tt], xn, xn, ssq, rstd, bTp[:, :, tt * 128:(tt + 1) * 128], bTp.b, 2, j, 7)
                    yield
                for hp in range(16):
                    pq = ps[7]
                    for dc in range(8):
                        S.op("pe", lambda e, dc=dc: e.matmul(pq[:, 0:256], lhsT=wq_sb[:, dc, hp * 128:(hp + 1) * 128], rhs=bTp[:, dc, :],
                                                            start=(dc == 0), stop=(dc == 7)),
                             R=[wq_sb.b, bTp.b], W=[pq.b], inc=(dc == 7))
                    if hp % 2 == 0:
                        S.op("act", lambda e: e.activation(out=qTe[:, hp, :], in_=pq[:, 0:256], func=AF.Copy), R=[pq.b], W=[qTe.b])
                    else:
                        S.op("dve", lambda e: e.tensor_copy(qTe[:, hp, :], pq[:, 0:256]), R=[pq.b], W=[qTe.b])
                    yield
                for tt in range(2):
                    for qd in range(4):
                        pb = ps[7]
                        for k4 in range(4):
                            hp = qd * 4 + k4
                            S.op("pe", lambda e, hp=hp, k4=k4: e.matmul(pb[:, k4 * 128:(k4 + 1) * 128], lhsT=qTe[:, hp, tt * 128:(tt + 1) * 128],
                                                                      rhs=keysT_sb[:, hp * 128:(hp + 1) * 128], start=True, stop=True),
                                 R=[qTe.b, keysT_sb.b], W=[pb.b], inc=(k4 == 3))
                        S.op("act", lambda e: e.activation(out=s_sb[:, qd * 512:(qd + 1) * 512], in_=pb[:, :], func=AF.Copy), R=[pb.b], W=s_b[qd * 4:qd * 4 + 4])
                        yield
                    for hp in range(16):
                        S.op("dve", lambda e, hp=hp: e.max(out=sv[:, hp, 0:8], in_=s3[:, hp, :]), R=[s_b[hp]], W=[sv_b[hp]])
                        if hp % 4 == 3:
                            yield
                    for hp in range(16):
                        S.op("dve", lambda e, hp=hp: e.max_index(out=si_u[:, hp, 0:8], in_max=sv[:, hp, 0:8], in_values=s3[:, hp, :]), R=[s_b[hp], sv_b[hp]], W=[siu_b[hp]])
                        if hp % 4 == 3:
                            yield
                    for hp in range(16):
                        S.op("dve", lambda e, hp=hp: e.match_replace(out=c3s[:, hp, :], in_to_replace=sv[:, hp, 0:8], in_values=s3[:, hp, :], imm_value=NEG),
                             R=[s_b[hp], sv_b[hp]], W=[c_b[hp]])
                        if hp % 4 == 3:
                            yield
                    for hp in range(16):
                        S.op("dve", lambda e, hp=hp: e.max(out=sv[:, hp, 8:16], in_=c3s[:, hp, :]), R=[c_b[hp]], W=[sv_b[hp]])
                        if hp % 4 == 3:
                            yield
                    for hp in range(16):
                        S.op("dve", lambda e, hp=hp: e.max_index(out=si_u[:, hp, 8:16], in_max=sv[:, hp, 8:16], in_values=c3s[:, hp, :]), R=[c_b[hp], sv_b[hp]], W=[siu_b[hp]])
                        if hp % 4 == 3:
                            yield
                    S.op("dve", lambda e: e.tensor_copy(si_f[:], si_u[:]), R=siu_b, W=[si_f.b])
                    sv4 = sv[:].rearrange("p (h a) k -> p h a k", a=2)
                    si4 = si_f[:].rearrange("p (h a) k -> p h a k", a=2)
                    c4 = cand[:].rearrange("p (h a b) -> p h a b", h=8, a=16)
                    S.op("dve", lambda e: e.tensor_tensor(out=c4, in0=sv4[:, :, 0, :].unsqueeze(3).to_broadcast([128, 8, 16, 16]),
                                                          in1=sv4[:, :, 1, :].unsqueeze(2).to_broadcast([128, 8, 16, 16]), op=ALU.add),
                         R=sv_b, W=c_b)
                    yield
                    hb = lambda lst, h: [lst[2 * h], lst[2 * h + 1]]
                    for h in range(8):
                        S.op("dve", lambda e, h=h: e.max(out=cv[:, h, 0:8], in_=cg3[:, h, :]), R=hb(c_b, h), W=[cv_b[h]])
                        if h % 4 == 3:
                            yield
                    for h in range(8):
                        S.op("dve", lambda e, h=h: e.max_index(out=ci_u[:, h, 0:8], in_max=cv[:, h, 0:8], in_values=cg3[:, h, :]), R=hb(c_b, h) + [cv_b[h]], W=[ciu_b[h]])
                        if h % 4 == 3:
                            yield
                    for h in range(8):
                        S.op("dve", lambda e, h=h: e.match_replace(out=sg3[:, h, :], in_to_replace=cv[:, h, 0:8], in_values=cg3[:, h, :], imm_value=NEG),
                             R=hb(c_b, h) + [cv_b[h]], W=hb(s_b, h))
                        if h % 4 == 3:
                            yield
                    for h in range(8):
                        S.op("dve", lambda e, h=h: e.max(out=cv[:, h, 8:16], in_=sg3[:, h, :]), R=hb(s_b, h), W=[cv_b[h]])
                        if h % 4 == 3:
                            yield
                    for h in range(8):
                        S.op("dve", lambda e, h=h: e.max_index(out=ci_u[:, h, 8:16], in_max=cv[:, h, 8:16], in_values=sg3[:, h, :]), R=hb(s_b, h) + [cv_b[h]], W=[ciu_b[h]])
                        if h % 4 == 3:
                            yield
                    ciu2 = ci_u[:].rearrange("p a b -> p (a b)")
                    S.op("dve", lambda e: e.tensor_scalar(out=k_u[:, 0, :], in0=ciu2, scalar1=4, scalar2=None, op0=ALU.logical_shift_right), R=ciu_b, W=[k_u.b])
                    S.op("dve", lambda e: e.tensor_scalar(out=k_u[:, 1, :], in0=ciu2, scalar1=15, scalar2=None, op0=ALU.bitwise_and), R=ciu_b, W=[k_u.b])
                    S.op("dve", lambda e: e.tensor_copy(k_f[:], k_u[:]), R=[k_u.b], W=[k_f.b])
                    yield
                    for a in range(2):
                        kk = k_f[:, a, :].rearrange("p (h k) -> p h k", h=8)
                        e4 = s_sb[:].rearrange("p (h a b) -> p h a b", h=8, a=16)
                        S.op("dve", lambda e: e.tensor_tensor(out=e4, in0=kk.unsqueeze(3).to_broadcast([128, 8, 16, 16]),
                                                              in1=iota16.unsqueeze(1).unsqueeze(1).to_broadcast([128, 8, 16, 16]), op=ALU.is_equal),
                             R=[k_f.b, iota_f.b], W=s_b)
                        yield
                        S.op("dve", lambda e: e.tensor_tensor(out=e4, in0=e4, in1=si4[:, :, a, :].unsqueeze(2).to_broadcast([128, 8, 16, 16]), op=ALU.mult),
                             R=s_b + [si_f.b], W=s_b)
                        yield
                        S.op("dve", lambda e: e.tensor_reduce(out=IG[:, a, :], in_=s_sb[:].rearrange("p (a b) -> p a b", b=16), axis=AX.X, op=ALU.add),
                             R=s_b, W=[IG.b])
                        yield
                    g3 = IG[:, 2, :].rearrange("p (h k) -> p h k", h=8)
                    S.op("dve", lambda e: e.tensor_tensor(out=g3, in0=cv[:], in1=cv[:, :, 0:1].to_broadcast([128, 8, 16]), op=ALU.subtract), R=cv_b, W=[IG.b])
                    S.op("act", lambda e: e.activation(out=IG[:, 2, :], in_=IG[:, 2, :], func=AF.Exp), R=[IG.b], W=[IG.b])
                    S.op("dve", lambda e: e.tensor_reduce(out=gsum[:], in_=g3, axis=AX.X, op=ALU.add), R=[IG.b], W=[gsum.b])
                    S.op("dve", lambda e: e.reciprocal(out=gsum[:], in_=gsum[:]), R=[gsum.b], W=[gsum.b])
                    S.op("dve", lambda e: e.tensor_tensor(out=g3, in0=g3, in1=gsum[:].unsqueeze(2).to_broadcast([128, 8, 16]), op=ALU.mult), R=[IG.b, gsum.b], W=[IG.b])
                    yield
                    pbt = ps[7]
                    for a in range(3):
                        S.op("pe", lambda e, a=a: e.transpose(pbt[:, a * 128:(a + 1) * 128], IG[:, a, :], ident_f[:]), R=[IG.b, ident_f.b], W=[pbt.b])
                    S.op("act", lambda e: e.activation(out=IGT[par][:, :, tt * 128:(tt + 1) * 128], in_=pbt[:, 0:384].rearrange("p (a b) -> p a b", a=3), func=AF.Copy),
                         R=[pbt.b], W=[IGT[par].b])
                    yield

            def build_wt(par):
                IGTp = IGT[par]
                for tb in range(32):
                    A_ = Ab[tb % 2]
                    B_ = Bb[tb % 2]
                    ab_ = AB_b[tb % 2]
                    for t8 in range(8):
                        t = tb * 8 + t8
                        S.op("dve", lambda e, t8=t8, t=t: e.tensor_scalar(out=A_[:, t8, :], in0=iota_b[:], scalar1=IGTp[:, 0, t:t + 1], scalar2=IGTp[:, 2, t:t + 1],
                                                                        op0=ALU.is_equal, op1=ALU.mult),
                             R=[IGTp.b, iota_b.b], W=[ab_[0][t8]])
                        S.op("dve", lambda e, t8=t8, t=t: e.tensor_scalar(out=B_[:, t8, :], in0=iota_b[:], scalar1=IGTp[:, 1, t:t + 1], scalar2=None, op0=ALU.is_equal),
                             R=[IGTp.b, iota_b.b], W=[ab_[1][t8]])
                    for q4 in range(2):
                        pw = ps[6 + (tb * 2 + q4) % 2]
                        for t4 in range(4):
                            t = q4 * 4 + t4
                            S.op("pe", lambda e, t=t, t4=t4: e.matmul(pw[:, t4 * 128:(t4 + 1) * 128], lhsT=B_[:, t, :], rhs=A_[:, t, :], start=True, stop=True),
                                 R=[ab_[0][t], ab_[1][t]], W=[pw.b], inc=(t4 == 3))
                        tk0 = tb * 8 + q4 * 4
                        S.op("act", lambda e: e.activation(out=WtS[:, tk0:tk0 + 4, :].rearrange("p a b -> p (a b)"), in_=pw[:, :], func=AF.Copy),
                             R=[pw.b], W=[WtS.b])

            AB_b = [[[Buf() for _ in range(8)] for _ in range(2)] for _ in range(2)]
            ngrp = T // 256
            glist = list(range(1 if last else 0, ngrp))
            for _ in front(glist[0], 0):
                pass
            for gi_, g in enumerate(glist):
                par = gi_ % 2
                j = 1 if g == 0 else 0
                bTp = bT[par]
                build_wt(par)
                nxt = front(glist[gi_ + 1], 1 - par) if gi_ + 1 < len(glist) else iter(())

                def uload(c):
                    S.dma(utc[c % 6][:].rearrange("p a b -> p (a b)"), utb_s[l, c], W=[utc[c % 6].b])

                def vload(c):
                    S.dma(vc[c % 4][:], vb_s[l, c * 128:(c + 1) * 128, :], W=[vc[c % 4].b])

                def emitA(c):
                    u_ = utc[c % 6]
                    pa = ps[4 + c % 3]
                    for dc in range(8):
                        S.op("pe", lambda e, dc=dc: e.matmul(pa[:, 0:256], lhsT=u_[:, dc, :], rhs=bTp[:, dc, :], start=(dc == 0), stop=(dc == 7)),
                             R=[u_.b, bTp.b], W=[pa.b], inc=(dc == 7))

                for c in range(5):
                    uload(c)
                for c in range(3):
                    vload(c)
                def emitMid(c):
                    pa = ps[4 + c % 3]
                    S.op("act", lambda e: e.activation(out=ga[c % 2][:], in_=pa[:, 0:256], func=AF.Gelu), R=[pa.b], W=[ga[c % 2].b])
                    S.op("pool", lambda e: e.tensor_tensor(out=GT[c % 3][:], in0=ga[c % 2][:], in1=WtS[:, :, c], op=ALU.mult), R=[ga[c % 2].b, WtS.b], W=[GT[c % 3].b])

                emitA(0)
                emitA(1)
                emitMid(0)
                for c in range(128):
                    if c + 5 < 128:
                        uload(c + 5)
                    if c + 3 < 128:
                        vload(c + 3)
                    if c + 2 < 128:
                        emitA(c + 2)
                    if c + 1 < 128:
                        emitMid(c + 1)
                    v_ = vc[c % 4]
                    for tt in range(2):
                        for hf in range(2):
                            S.op("pe", lambda e, tt=tt, hf=hf: e.matmul(ps[tt * 2 + hf][:, :], lhsT=GT[c % 3][:, tt * 128:(tt + 1) * 128], rhs=v_[:, hf * 512:(hf + 1) * 512],
                                                                      start=(c == 0), stop=(c == 127), skip_group_check=True),
                                 R=[GT[c % 3].b, v_.b], W=[ps[tt * 2 + hf].b], inc=(tt == 1 and hf == 1))
                    if c >= 1:
                        next(nxt, None)
                        if c % 8 == 0:
                            next(nxt, None)
                for _ in nxt:
                    pass
                for tt in range(2):
                    i = g * 2 + tt
                    y_ = h1e[par][tt]
                    for hf in range(2):
                        pb_ = ps[tt * 2 + hf]
                        S.op("dve", lambda e, hf=hf, pb_=pb_: e.tensor_tensor(out=pb_[:, :], in0=pb_[:, :], in1=gbe[:, hf * 512:(hf + 1) * 512], op=ALU.mult),
                             R=[pb_.b, gbe.b], W=[pb_.b])
                        S.op("dve", lambda e, hf=hf, pb_=pb_: e.tensor_tensor(out=y_[:, hf * 512:(hf + 1) * 512], in0=pb_[:, :], in1=y_[:, hf * 512:(hf + 1) * 512], op=ALU.add),
                             R=[pb_.b, y_.b], W=[y_.b])
                    if last:
                        S.dma(y_d[(i - 2) * 128:(i - 1) * 128, :], y_[:], R=[y_.b])
                    else:
                        S.dma(h_s[i * 128:(i + 1) * 128, :], y_[:], R=[y_.b])
                if g == 0:
                    gate_bcast(gbe, 1, 0)
            S.barrier()
        chk(l, "E")

    S.barrier()
    es.close()
    return nc


def _consts():
    ident = np.eye(128, dtype=np.float32)
    iota = np.tile(np.arange(128, dtype=np.float32)[None, :], (128, 1))
    jj = np.arange(128)[:, None]
    ii = np.arange(128)[None, :]
    tri = np.stack([(ii <= jj), (jj <= ii)]).astype(np.float32)
    sel = np.zeros((2, 256), np.float32)
    sel[0, 0:128] = 1.0
    sel[1, 128:256] = 1.0

    def tables(rot):
        qd = rot // 4
        inv = (np.float32(10000.0) ** (-np.arange(qd, dtype=np.float32) / np.float32(qd))).astype(np.float32)
        t = np.arange(SEQ)
        rows = (t // 64).astype(np.float32)
        cols = (t % 64).astype(np.float32)
        ar = rows[:, None] * inv
        ac = cols[:, None] * inv
        ang = np.concatenate([ar, ar, ac, ac], -1).astype(np.float32)
        cos = np.cos(ang).astype(np.float32)
        sin = np.sin(ang).astype(np.float32)
        sgn = np.concatenate([-np.ones(qd), np.ones(qd), -np.ones(qd), np.ones(qd)]).astype(np.float32)
        cs = np.zeros((T, 2, rot), np.float32)
        cs[:CTX, 0, :] = 1.0
        cs[CTX:, 0, :] = cos
        cs[CTX:, 1, :] = sin * sgn
        return cs.reshape(T, 2 * rot)

    return dict(ident=ident, iota=iota, tri=tri, sel=sel, cs_mla=tables(32), cs_swa=tables(64))


def _in_maps(inp):
    f = lambda a: np.ascontiguousarray(np.asarray(a, dtype=np.float32))
    shared = dict(_consts())
    for k in ("ada_w", "ada_b", "norm1_g", "norm2_g", "w_in", "mla_wuq", "mla_wukv", "mla_qn_g", "mla_kn_g",
              "swa_qn_g", "swa_kn_g", "swa_sink", "w_out", "peer_wq", "peer_v"):
        shared[k] = f(inp[k])
    shared["qa_g_t"] = f(np.asarray(inp["mla_qa_g"]).reshape(L, 3, 128).transpose(0, 2, 1))
    shared["kva_g_t"] = f(np.asarray(inp["mla_kva_g"]).reshape(L, 2, 128).transpose(0, 2, 1))
    shared["keysT"] = f(np.asarray(inp["peer_keys"]).transpose(0, 4, 1, 2, 3).reshape(L, 128, 2048))
    u = np.asarray(inp["peer_u"], dtype=np.float32).reshape(L, 128, 128, 8, 128)
    shared["peer_uT"] = np.ascontiguousarray(u.transpose(0, 1, 4, 3, 2)).reshape(L, 128, 128, 1024)
    x = np.asarray(inp["x"], dtype=np.float32)
    c = np.asarray(inp["c"], dtype=np.float32)
    ctx = np.asarray(inp["ctx"], dtype=np.float32)
    cc = np.asarray(inp["c_ctx"], dtype=np.float32)
    maps = []
    for b in range(8):
        m = dict(shared)
        m["x"] = np.ascontiguousarray(x[b])
        m["ctx"] = np.ascontiguousarray(ctx[b])
        m["cvec"] = np.ascontiguousarray(np.stack([c[b], cc]))
        maps.append(m)
    return maps


def kernel(**inputs):
    nc = build()
    res = run_bass_kernel_spmd(nc, _in_maps(inputs), core_ids=list(range(8)))
    return np.stack([np.asarray(r["y"], dtype=np.float32) for r in res.results], axis=0)
```

```python
import contextlib
import numpy as np
import concourse.bass as bass
import concourse.mybir as mybir
from concourse.bass_utils import run_bass_kernel_spmd

F32, BF16, U32 = mybir.dt.float32, mybir.dt.bfloat16, mybir.dt.uint32
AF = mybir.ActivationFunctionType
ALU = mybir.AluOpType
AX = mybir.AxisListType

L = 2
D = 1024
SEQ = 4096
CTX = 256
T = SEQ + CTX
NT = T // 128
EPS = 1e-6
DIN = 1440
NEG = -1.0e30


class Buf:
    __slots__ = ("w", "r")

    def __init__(self):
        self.w = None
        self.r = {}


class Sched:
    def __init__(self, nc, es):
        self.nc = nc
        self.eng = {"pe": nc.tensor, "act": nc.scalar, "dve": nc.vector, "pool": nc.gpsimd, "sp": nc.sync}
        self.sem = {}
        self.cnt = {}
        for e in ("pe", "act", "dve", "pool"):
            self.sem[e] = es.enter_context(nc.semaphore("s_" + e))
            self.cnt[e] = 0
        self.R = 32
        self.dsem = [es.enter_context(nc.semaphore("d%d" % i)) for i in range(self.R)]
        self.dlast = [None] * self.R
        self.dn = 0
        self.seen = {e: {} for e in self.eng}
        self.pe_pending = False
        self.rec = None
        self.bgsems = [es.enter_context(nc.semaphore("bg%d" % i)) for i in range(8)]
        self.bgn = 0

    def _wait(self, e, tok):
        key, sem, val = tok
        if e == "pe" and key == "pe":
            return
        if self.seen[e].get(key, 0) >= val:
            return
        self.eng[e].wait_ge(sem, val)
        self.seen[e][key] = val

    def _deps(self, e, reads, writes):
        for b in reads:
            if b.w is not None:
                self._wait(e, b.w)
        for b in writes:
            if b.w is not None:
                self._wait(e, b.w)
            for t in list(b.r.values()):
                self._wait(e, t)

    def _commit(self, tok, reads, writes):
        for b in reads:
            b.r[tok[0]] = tok
        for b in writes:
            b.w = tok
            b.r = {}

    def op(self, *a, **k):
        if self.rec is not None:
            self.rec.append(lambda: self._op(*a, **k))
        else:
            self._op(*a, **k)

    def dma(self, *a, **k):
        if self.rec is not None:
            self.rec.append(lambda: self._dma(*a, **k))
        else:
            self._dma(*a, **k)

    def _op(self, e, fn, R=(), W=(), inc=True):
        self._deps(e, R, W)
        inst = fn(self.eng[e])
        if inc:
            self.cnt[e] += 1
            inst.then_inc(self.sem[e], 1)
            tok = (e, self.sem[e], self.cnt[e])
        else:
            assert e == "pe"
            tok = (e, self.sem[e], self.cnt[e] + 1)
        self._commit(tok, R, W)

    def dma_untracked(self, out, in_, q):
        k = self.bgn % len(self.bgsems)
        if self.bgn >= len(self.bgsems):
            self.eng[q].wait_ge(self.bgsems[k], 16 * (self.bgn // len(self.bgsems)))
        inst = self.eng[q].dma_start(out=out, in_=in_)
        self.bgn += 1
        inst.then_inc(self.bgsems[k], 16)

    def _dma(self, out, in_, R=(), W=(), q="sp"):
        i = self.dn % self.R
        if self.dlast[i] is not None:
            self._wait(q, self.dlast[i])
        self._deps(q, R, W)
        inst = self.eng[q].dma_start(out=out, in_=in_)
        val = 16 * (self.dn // self.R + 1)
        inst.then_inc(self.dsem[i], 16)
        tok = ("d%d" % i, self.dsem[i], val)
        self.dlast[i] = tok
        self.dn += 1
        self._commit(tok, R, W)

    def barrier(self, engines=("pe", "act", "dve", "pool", "sp"), bg=False):
        toks = []
        if bg and self.bgn > 0:
            nb_ = len(self.bgsems)
            for k in range(nb_):
                cnt_ = len([x for x in range(self.bgn) if x % nb_ == k])
                if cnt_:
                    toks.append(("bg%d" % k, self.bgsems[k], 16 * cnt_))
        for e in ("pe", "act", "dve", "pool"):
            if self.cnt[e] > 0:
                toks.append((e, self.sem[e], self.cnt[e]))
        for t in self.dlast:
            if t is not None:
                toks.append(t)
        for e in engines:
            for t in toks:
                if not (e == t[0]):
                    self._wait(e, t)
                elif e != "pe":
                    self._wait(e, t)


class TT:
    def __init__(self, t):
        self.t = t
        self.b = Buf()

    def __getitem__(self, k):
        return self.t[k]


class _Stop(Exception):
    pass


_LAST = {}


def build_dbg(debug, stop):
    try:
        return build(debug, stop)
    except _Stop:
        _LAST["es"].close()
        return _LAST["nc"]


def build(debug=None, stop=None):
    nc = bass.Bass("TRN2", target_bir_lowering=False)
    es = contextlib.ExitStack()
    _LAST["nc"] = nc
    _LAST["es"] = es

    def din(name, shape, dt=F32):
        return nc.dram_tensor(name, list(shape), dt, kind="ExternalInput").ap()

    dbg_names = set(debug or [])

    def dscr(name, shape, dt):
        kind = "ExternalOutput" if name in dbg_names else "Internal"
        return nc.dram_tensor(name, list(shape), dt, kind=kind).ap()

    x_d = din("x", [SEQ, D])
    ctx_d = din("ctx", [CTX, D])
    cvec_d = din("cvec", [2, D])
    ada_w = din("ada_w", [L, D, 6 * D])
    ada_b = din("ada_b", [L, 6 * D])
    n1g = din("norm1_g", [L, D])
    n2g = din("norm2_g", [L, D])
    w_in = din("w_in", [L, D, DIN])
    qa_g = din("qa_g_t", [L, 128, 3])
    wuq = din("mla_wuq", [L, 384, 768])
    kva_g = din("kva_g_t", [L, 128, 2])
    wukv = din("mla_wukv", [L, 256, 1024])
    mqn_g = din("mla_qn_g", [L, 96])
    mkn_g = din("mla_kn_g", [L, 96])
    sqn_g = din("swa_qn_g", [L, 64])
    skn_g = din("swa_kn_g", [L, 64])
    sink_d = din("swa_sink", [L, 8])
    w_out = din("w_out", [L, D, D])
    wq_d = din("peer_wq", [L, D, 2048])
    keysT_d = din("keysT", [L, 128, 2048])
    ut_d = din("peer_uT", [L, 128, 128, 1024])
    v_d = din("peer_v", [L, 16384, D])
    ident_d = din("ident", [128, 128])
    iota_d = din("iota", [128, 128])
    tri_d = din("tri", [2, 128, 128])
    sel_d = din("sel", [2, 256])
    csm_d = din("cs_mla", [T, 64])
    css_d = din("cs_swa", [T, 128])
    y_d = nc.dram_tensor("y", [SEQ, D], F32, kind="ExternalOutput").ap()

    h_s = dscr("h_s", [T, D], F32)
    qTm_s = dscr("qTm_s", [8, 96, T], BF16)
    kTm_s = dscr("kTm_s", [8, 96, T], BF16)
    Vm_s = dscr("Vm_s", [T, 520], BF16)
    qTs_s = dscr("qTs_s", [8, 64, T], BF16)
    kTs_s = dscr("kTs_s", [2, 64, T], BF16)
    Vs_s = dscr("Vs_s", [T, 130], BF16)
    om_s = dscr("om_s", [T, D], BF16)
    utb_s = dscr("utb_s", [L, 128, 128, 1024], BF16)
    vb_s = dscr("vb_s", [L, 16384, D], BF16)

    S = Sched(nc, es)

    uid = [0]

    def sb(name, shape, dt, stack=None):
        uid[0] += 1
        return TT((stack or es).enter_context(nc.sbuf_tensor("%s_%d" % (name, uid[0]), list(shape), dt)))

    ps = [TT(es.enter_context(nc.psum_tensor("ps%d" % i, [128, 512], F32))) for i in range(8)]

    def psb(i):
        return ps[i][:].bitcast(BF16)

    ident_f = sb("ident_f", [128, 128], F32)
    ident_b = sb("ident_b", [128, 128], BF16)
    iota_f = sb("iota_f", [128, 128], F32)
    iota_b = sb("iota_b", [128, 128], BF16)
    tri_f = sb("tri_f", [128, 2, 128], F32)
    tri_b = sb("tri_b", [128, 2, 128], BF16)
    sel_f = sb("sel_f", [2, 256], F32)
    epsc = sb("epsc", [128, 1], F32)
    S.dma(ident_f[:], ident_d[:, :], W=[ident_f.b])
    S.dma(iota_f[:], iota_d[:, :], W=[iota_f.b])
    S.dma(tri_f[:], tri_d.rearrange("a p q -> p a q"), W=[tri_f.b])
    S.dma(sel_f[:], sel_d[:, :], W=[sel_f.b])
    S.op("dve", lambda e: e.tensor_copy(ident_b[:], ident_f[:]), R=[ident_f.b], W=[ident_b.b])
    S.op("dve", lambda e: e.tensor_copy(iota_b[:], iota_f[:]), R=[iota_f.b], W=[iota_b.b])
    S.op("dve", lambda e: e.tensor_copy(tri_b[:], tri_f[:]), R=[tri_f.b], W=[tri_b.b])
    S.op("dve", lambda e: e.memset(epsc[:], EPS), W=[epsc.b])

    modT = sb("modT", [128, 4, 8, 2], F32)
    grow = sb("grow", [2, 2 * D], F32)
    sT = sb("sT", [128, 8, 2], F32)
    esink = sb("esink", [128, 8], F32)
    tmp_es = contextlib.ExitStack()
    s2row = sb("s2row", [2, D], F32, tmp_es)

    S.dma(s2row[:], cvec_d[:, :], W=[s2row.b])
    S.op("act", lambda e: e.activation(out=s2row[:], in_=s2row[:], func=AF.Silu), R=[s2row.b], W=[s2row.b])
    for dc in range(8):
        S.op("pe", lambda e, dc=dc: e.transpose(ps[0][:, dc * 2:dc * 2 + 2], s2row[0:2, dc * 128:(dc + 1) * 128], ident_f[0:2, 0:2]),
             R=[s2row.b, ident_f.b], W=[ps[0].b])
    S.op("dve", lambda e: e.tensor_copy(sT[:].rearrange("p a b -> p (a b)"), ps[0][:, 0:16]), R=[ps[0].b], W=[sT.b])
    S.barrier()
    tmp_es.close()

    bg_list = []
    for l in range(L):
        for c in range(0, 128, 8):
            bg_list.append((utb_s[l, c:c + 8].rearrange("c p n -> (c p) n"), ut_d[l, c:c + 8].rearrange("c p n -> (c p) n")))
            bg_list.append((vb_s[l, c * 128:(c + 8) * 128, :], v_d[l, c * 128:(c + 8) * 128, :]))

    def bg_issue(n):
        for _ in range(n):
            if bg_list:
                o_, i_ = bg_list.pop(0)
                S.dma_untracked(o_, i_, "pool")

    def rstd_of(ph, x3, xb, H, Dh, tmp, ssq, rstd):
        tv = tmp[:, 0:H * Dh].rearrange("p (h d) -> p h d", h=H)
        S.op("dve", lambda e: e.tensor_tensor(out=tv, in0=x3, in1=x3, op=ALU.mult), R=[xb], W=[tmp.b])
        S.op("dve", lambda e: e.tensor_reduce(out=ssq[:, 0:H], in_=tv, axis=AX.X, op=ALU.add), R=[tmp.b], W=[ssq.b])
        S.op("act", lambda e: e.activation(out=rstd[:, 0:H], in_=ssq[:, 0:H], func=AF.Sqrt, scale=1.0 / Dh, bias=epsc[:, 0:1]),
             R=[ssq.b, epsc.b], W=[rstd.b])
        S.op("dve", lambda e: e.reciprocal(out=rstd[:, 0:H], in_=rstd[:, 0:H]), R=[rstd.b], W=[rstd.b])

    def rope(x5, xb, cst, cstb, H, q, t1, t2, out5, outb):
        cos4 = cst[:, 0, :].rearrange("p (a b q) -> p a b q", a=2, b=2)
        sin4 = cst[:, 1, :].rearrange("p (a b q) -> p a b q", a=2, b=2)
        n = H * 4 * q
        t1v = t1[:, 0:n].rearrange("p (h a b q) -> p h a b q", h=H, a=2, b=2)
        t2v = t2[:, 0:n].rearrange("p (h a b q) -> p h a b q", h=H, a=2, b=2)
        for b_ in range(2):
            cb = cos4[:, :, b_, :].unsqueeze(1).to_broadcast([128, H, 2, q])
            sbn = sin4[:, :, b_, :].unsqueeze(1).to_broadcast([128, H, 2, q])
            S.op("dve", lambda e, b_=b_, cb=cb: e.tensor_tensor(out=t1v[:, :, :, b_, :], in0=x5[:, :, :, b_, :], in1=cb, op=ALU.mult),
                 R=[xb, cstb], W=[t1.b])
            S.op("dve", lambda e, b_=b_, sbn=sbn: e.tensor_tensor(out=t2v[:, :, :, b_, :], in0=x5[:, :, :, 1 - b_, :], in1=sbn, op=ALU.mult),
                 R=[xb, cstb], W=[t2.b])
        for b_ in range(2):
            S.op("dve", lambda e, b_=b_: e.tensor_tensor(out=out5[:, :, :, b_, :], in0=t1v[:, :, :, b_, :], in1=t2v[:, :, :, b_, :], op=ALU.add),
                 R=[t1.b, t2.b], W=[outb])

    def load_w_bf16(ph, dst2d, src2d, ncols, stg, scale_ap=None, scale_b=None, k=[0]):
        for c0 in range(0, ncols, 2048):
            c1 = min(ncols, c0 + 2048)
            st = stg[k[0] % 2]
            k[0] += 1
            S.dma(st[:, 0:c1 - c0], src2d[:, c0:c1], W=[st.b])
            if scale_ap is None:
                S.op("act", lambda e, st=st, c0=c0, c1=c1: e.activation(out=dst2d[0][:, c0:c1], in_=st[:, 0:c1 - c0], func=AF.Copy),
                     R=[st.b], W=[dst2d[1]])
            else:
                S.op("dve", lambda e, st=st, c0=c0, c1=c1: e.tensor_scalar(out=dst2d[0][:, c0:c1], in0=st[:, 0:c1 - c0], scalar1=scale_ap,
                                                                         scalar2=None, op0=ALU.mult),
                     R=[st.b, scale_b], W=[dst2d[1]])

    def chk(l, phn):
        if stop is not None and stop == (l, phn):
            raise _Stop()

    for l in (range(L) if stop is None else range(stop[0] + 1)):
        last = l == L - 1
        with contextlib.ExitStack() as ph:
            aw = [sb("aw%d" % i, [128, 3072], F32, ph) for i in range(4)]
            modrow = sb("modrow", [2, 6 * D], F32, ph)
            abr = sb("abr", [2, 6 * D], F32, ph)
            ng = sb("ng", [2, 2, D], F32, ph)
            vrow = sb("vrow", [2, 4, D], F32, ph)
            S.dma(abr[:], ada_b[l:l + 1, :].to_broadcast([2, 6 * D]), W=[abr.b])
            S.dma(ng[:, 0, :], n1g[l:l + 1, :].to_broadcast([2, D]), W=[ng.b])
            S.dma(ng[:, 1, :], n2g[l:l + 1, :].to_broadcast([2, D]), W=[ng.b])
            S.dma(esink[:], sink_d[l:l + 1, :].to_broadcast([128, 8]), W=[esink.b])
            S.op("act", lambda e: e.activation(out=esink[:], in_=esink[:], func=AF.Exp), R=[esink.b], W=[esink.b])
            k = 0
            for half in range(2):
                for dc in range(8):
                    a = aw[k % 4]
                    k += 1
                    S.dma(a[:], ada_w[l, dc * 128:(dc + 1) * 128, half * 3072:(half + 1) * 3072], W=[a.b])
                    for cb in range(6):
                        S.op("pe", lambda e, a=a, cb=cb, dc=dc: e.matmul(ps[cb][0:2, :], lhsT=sT[:, dc, :], rhs=a[:, cb * 512:(cb + 1) * 512],
                                                                       start=(dc == 0), stop=(dc == 7)),
                             R=[a.b, sT.b], W=[ps[cb].b])
                for cb in range(6):
                    c0 = half * 3072 + cb * 512
                    S.op("dve", lambda e, cb=cb, c0=c0: e.tensor_tensor(out=modrow[:, c0:c0 + 512], in0=ps[cb][0:2, :], in1=abr[:, c0:c0 + 512], op=ALU.add),
                         R=[ps[cb].b, abr.b], W=[modrow.b])
            for j, (sci, shi) in enumerate(((1, 0), (4, 3))):
                S.op("dve", lambda e, j=j, sci=sci: e.scalar_tensor_tensor(out=vrow[:, 2 * j, :], in0=modrow[:, sci * D:(sci + 1) * D], scalar=1.0,
                                                                          in1=ng[:, j, :], op0=ALU.add, op1=ALU.mult),
                     R=[modrow.b, ng.b], W=[vrow.b])
                S.op("dve", lambda e, j=j, shi=shi: e.tensor_copy(vrow[:, 2 * j + 1, :], modrow[:, shi * D:(shi + 1) * D]),
                     R=[modrow.b], W=[vrow.b])
            S.op("dve", lambda e: e.tensor_copy(grow[:, 0:D], modrow[:, 2 * D:3 * D]), R=[modrow.b], W=[grow.b])
            S.op("dve", lambda e: e.tensor_copy(grow[:, D:2 * D], modrow[:, 5 * D:6 * D]), R=[modrow.b], W=[grow.b])
            for v in range(4):
                for dc in range(8):
                    o = (v * 8 + dc) * 2
                    S.op("pe", lambda e, v=v, dc=dc, o=o: e.transpose(ps[6][:, o:o + 2], vrow[0:2, v, dc * 128:(dc + 1) * 128], ident_f[0:2, 0:2]),
                         R=[vrow.b, ident_f.b], W=[ps[6].b])
            S.op("dve", lambda e: e.tensor_copy(modT[:].rearrange("p a b c -> p (a b c)"), ps[6][:, 0:64]), R=[ps[6].b], W=[modT.b])
            S.barrier()
        chk(l, "A")

        def gate_bcast(dst, gi, j):
            for hf in range(2):
                S.op("pe", lambda e, hf=hf: e.matmul(ps[6 + hf][:, :], lhsT=sel_f[0:2, j * 128:(j + 1) * 128],
                                                    rhs=grow[0:2, gi * D + hf * 512: gi * D + (hf + 1) * 512], start=True, stop=True),
                     R=[sel_f.b, grow.b], W=[ps[6 + hf].b])
                S.op("act", lambda e, hf=hf: e.activation(out=dst[:, hf * 512:(hf + 1) * 512], in_=ps[6 + hf][:, :], func=AF.Copy),
                     R=[ps[6 + hf].b], W=[dst.b])

        def modulate_T(ht, xn, sqj, ssq, rstd, aT_ap, aT_b, vA, j, psbank):
            rstd_of(None, ht[:].rearrange("p (h d) -> p h d", h=1), ht.b, 1, D, sqj, ssq, rstd)
            S.op("dve", lambda e: e.tensor_scalar(out=xn[:], in0=ht[:], scalar1=rstd[:, 0:1], scalar2=None, op0=ALU.mult),
                 R=[ht.b, rstd.b], W=[xn.b])
            pv = psb(psbank)
            for dc in range(8):
                S.op("pe", lambda e, dc=dc: e.transpose(pv[:, dc * 128:(dc + 1) * 128], xn[:, dc * 128:(dc + 1) * 128], ident_b[:]),
                     R=[xn.b, ident_b.b], W=[ps[psbank].b])
            for dc in range(8):
                S.op("dve", lambda e, dc=dc: e.tensor_scalar(out=aT_ap[:, dc, :], in0=pv[:, dc * 128:(dc + 1) * 128],
                                                            scalar1=modT[:, vA, dc, j:j + 1], scalar2=modT[:, vA + 1, dc, j:j + 1],
                                                            op0=ALU.mult, op1=ALU.add),
                     R=[ps[psbank].b, modT.b], W=[aT_b])

        with contextlib.ExitStack() as ph:
            stg = [sb("stg%d" % i, [128, 2048], F32, ph) for i in range(2)]
            w_in_sb = sb("w_in_sb", [128, 8, DIN], BF16, ph)
            wuq_sb = sb("wuq_sb", [128, 3, 768], BF16, ph)
            wukv_sb = sb("wukv_sb", [128, 2, 1024], BF16, ph)
            gq = sb("gq", [128, 3], F32, ph)
            gkv = sb("gkv", [128, 2], F32, ph)
            g_mq = sb("g_mq", [128, 96], F32, ph)
            g_mk = sb("g_mk", [128, 96], F32, ph)
            g_sq = sb("g_sq", [128, 64], F32, ph)
            g_sk = sb("g_sk", [128, 64], F32, ph)
            S.dma(gq[:], qa_g[l], W=[gq.b])
            S.dma(gkv[:], kva_g[l], W=[gkv.b])
            S.dma(g_mq[:], mqn_g[l:l + 1, :].to_broadcast([128, 96]), W=[g_mq.b])
            S.dma(g_mk[:], mkn_g[l:l + 1, :].to_broadcast([128, 96]), W=[g_mk.b])
            S.dma(g_sq[:], sqn_g[l:l + 1, :].to_broadcast([128, 64]), W=[g_sq.b])
            S.dma(g_sk[:], skn_g[l:l + 1, :].to_broadcast([128, 64]), W=[g_sk.b])
            for dc in range(8):
                load_w_bf16(ph, (w_in_sb[:, dc, :], w_in_sb.b), w_in[l, dc * 128:(dc + 1) * 128, :], DIN, stg)
            for kc in range(3):
                load_w_bf16(ph, (wuq_sb[:, kc, :], wuq_sb.b), wuq[l, kc * 128:(kc + 1) * 128, :], 768, stg, gq[:, kc:kc + 1], gq.b)
            for kc in range(2):
                load_w_bf16(ph, (wukv_sb[:, kc, :], wukv_sb.b), wukv[l, kc * 128:(kc + 1) * 128, :], 1024, stg, gkv[:, kc:kc + 1], gkv.b)

            ht = [sb("ht%d" % i, [128, D], F32, ph) for i in range(2)]
            cstm = [sb("cstm%d" % i, [128, 2, 32], F32, ph) for i in range(2)]
            csts = [sb("csts%d" % i, [128, 2, 64], F32, ph) for i in range(2)]
            def mkset(par):
                d_ = {}
                d_["sqj"] = sb("sqj%d" % par, [128, D], F32, ph)
                d_["t1"] = sb("t1%d" % par, [128, D], F32, ph)
                d_["t2"] = sb("t2%d" % par, [128, D], F32, ph)
                d_["xn"] = sb("xn%d" % par, [128, D], BF16, ph)
                d_["aT"] = sb("aT%d" % par, [128, 8, 128], BF16, ph)
                d_["p_sb"] = sb("p_sb%d" % par, [128, DIN], F32, ph)
                d_["qan"] = sb("qan%d" % par, [128, 384], BF16, ph)
                d_["kvan"] = sb("kvan%d" % par, [128, 256], BF16, ph)
                d_["lT"] = sb("lT%d" % par, [128, 5, 128], BF16, ph)
                d_["qm"] = sb("qm%d" % par, [128, 8, 96], F32, ph)
                d_["kv"] = sb("kv%d" % par, [128, 8, 128], F32, ph)
                d_["kf"] = sb("kf%d" % par, [128, 8, 96], F32, ph)
                d_["qn"] = sb("qn%d" % par, [128, 8, 96], F32, ph)
                d_["kn"] = sb("kn%d" % par, [128, 8, 96], F32, ph)
                d_["sqn"] = sb("sqn%d" % par, [128, 8, 64], F32, ph)
                d_["skn"] = sb("skn%d" % par, [128, 2, 64], F32, ph)
                d_["qo"] = sb("qo%d" % par, [128, 8, 96], BF16, ph)
                d_["ko"] = sb("ko%d" % par, [128, 8, 96], BF16, ph)
                d_["sqo"] = sb("sqo%d" % par, [128, 8, 64], BF16, ph)
                d_["sko"] = sb("sko%d" % par, [128, 2, 64], BF16, ph)
                d_["qT_sb"] = sb("qT_sb%d" % par, [128, 8, 128], BF16, ph)
                d_["kT_sb"] = sb("kT_sb%d" % par, [128, 8, 128], BF16, ph)
                d_["sqT_sb"] = sb("sqT_sb%d" % par, [128, 8, 128], BF16, ph)
                d_["skT_sb"] = sb("skT_sb%d" % par, [128, 2, 128], BF16, ph)
                return d_

            BS = [mkset(0), mkset(1)]
            Vm_sb = [sb("Vm_sb%d" % i, [128, 8, 65], BF16, ph) for i in range(2)]
            Vs_sb = [sb("Vs_sb%d" % i, [128, 2, 65], BF16, ph) for i in range(2)]
            for par_ in range(2):
                BS[par_]["ssq"] = sb("ssq%d" % par_, [128, 16], F32, ph)
                BS[par_]["rstd"] = sb("rstd%d" % par_, [128, 16], F32, ph)
            for i in range(2):
                S.op("pool", lambda e, i=i: e.memset(Vm_sb[i][:], 1.0), W=[Vm_sb[i].b])
                S.op("pool", lambda e, i=i: e.memset(Vs_sb[i][:], 1.0), W=[Vs_sb[i].b])

            def loads(i):
                hb = ht[i % 2]
                if l == 0:
                    src = ctx_d[i * 128:(i + 1) * 128, :] if i < 2 else x_d[(i - 2) * 128:(i - 1) * 128, :]
                else:
                    src = h_s[i * 128:(i + 1) * 128, :]
                S.dma(hb[:], src, W=[hb.b])
                S.dma(cstm[i % 2][:].rearrange("p a b -> p (a b)"), csm_d[i * 128:(i + 1) * 128, :], W=[cstm[i % 2].b])
                S.dma(csts[i % 2][:].rearrange("p a b -> p (a b)"), css_d[i * 128:(i + 1) * 128, :], W=[csts[i % 2].b])

            def tile_body(i, par):
                d_ = BS[par]
                sqj, t1, t2, xn, aT, p_sb, qan, kvan, lT, qm, kv, kf, qn, kn, sqn, skn, qo, ko, sqo, sko, qT_sb, kT_sb, sqT_sb, skT_sb, ssq, rstd = d_["sqj"], d_["t1"], d_["t2"], d_["xn"], d_["aT"], d_["p_sb"], d_["qan"], d_["kvan"], d_["lT"], d_["qm"], d_["kv"], d_["kf"], d_["qn"], d_["kn"], d_["sqn"], d_["skn"], d_["qo"], d_["ko"], d_["sqo"], d_["sko"], d_["qT_sb"], d_["kT_sb"], d_["sqT_sb"], d_["skT_sb"], d_["ssq"], d_["rstd"]
                ps_ = ps[4 * par:] + ps[:4 * par]
                psb_ = lambda k_: psb((k_ + 4 * par) % 8)
                j = 1 if i < 2 else 0
                hb = ht[i % 2]
                cm = cstm[i % 2]
                cs_ = csts[i % 2]
                tsl = slice(i * 128, (i + 1) * 128)
                modulate_T(hb, xn, sqj, ssq, rstd, aT[:], aT.b, 0, j, (4 * par) % 8)
                for cb, (c0, c1) in enumerate(((0, 512), (512, 1024), (1024, DIN))):
                    for dc in range(8):
                        S.op("pe", lambda e, cb=cb, c0=c0, c1=c1, dc=dc: e.matmul(ps_[1 + cb][:, 0:c1 - c0], lhsT=aT[:, dc, :], rhs=w_in_sb[:, dc, c0:c1],
                                                                              start=(dc == 0), stop=(dc == 7)),
                             R=[aT.b, w_in_sb.b], W=[ps_[1 + cb].b], inc=(dc == 7))
                    S.op("act", lambda e, cb=cb, c0=c0, c1=c1: e.activation(out=p_sb[:, c0:c1], in_=ps_[1 + cb][:, 0:c1 - c0], func=AF.Copy),
                         R=[ps_[1 + cb].b], W=[p_sb.b])
                rstd_of(ph, p_sb[:, 0:384].rearrange("p (h d) -> p h d", h=1), p_sb.b, 1, 384, sqj, ssq, rstd)
                S.op("dve", lambda e: e.tensor_scalar(out=qan[:], in0=p_sb[:, 0:384], scalar1=rstd[:, 0:1], scalar2=None, op0=ALU.mult),
                     R=[p_sb.b, rstd.b], W=[qan.b])
                rstd_of(ph, p_sb[:, 896:1152].rearrange("p (h d) -> p h d", h=1), p_sb.b, 1, 256, sqj, ssq, rstd)
                S.op("dve", lambda e: e.tensor_scalar(out=kvan[:], in0=p_sb[:, 896:1152], scalar1=rstd[:, 0:1], scalar2=None, op0=ALU.mult),
                     R=[p_sb.b, rstd.b], W=[kvan.b])
                pv4 = psb_(4)
                for kc in range(3):
                    S.op("pe", lambda e, kc=kc: e.transpose(pv4[:, kc * 128:(kc + 1) * 128], qan[:, kc * 128:(kc + 1) * 128], ident_b[:]),
                         R=[qan.b, ident_b.b], W=[ps_[4].b])
                for kc in range(2):
                    S.op("pe", lambda e, kc=kc: e.transpose(pv4[:, (3 + kc) * 128:(4 + kc) * 128], kvan[:, kc * 128:(kc + 1) * 128], ident_b[:]),
                         R=[kvan.b, ident_b.b], W=[ps_[4].b])
                S.op("act", lambda e: e.activation(out=lT[:].rearrange("p a b -> p (a b)"), in_=pv4[:, 0:640], func=AF.Copy), R=[ps_[4].b], W=[lT.b])
                for cb, (c0, c1) in enumerate(((0, 512), (512, 768))):
                    for kc in range(3):
                        S.op("pe", lambda e, cb=cb, c0=c0, c1=c1, kc=kc: e.matmul(ps_[5 + cb][:, 0:c1 - c0], lhsT=lT[:, kc, :], rhs=wuq_sb[:, kc, c0:c1],
                                                                              start=(kc == 0), stop=(kc == 2)),
                             R=[lT.b, wuq_sb.b], W=[ps_[5 + cb].b], inc=(kc == 2))
                    S.op("act", lambda e, cb=cb, c0=c0, c1=c1: e.activation(out=qm[:].rearrange("p a b -> p (a b)")[:, c0:c1], in_=ps_[5 + cb][:, 0:c1 - c0], func=AF.Copy),
                         R=[ps_[5 + cb].b], W=[qm.b])
                for cb in range(2):
                    for kc in range(2):
                        S.op("pe", lambda e, cb=cb, kc=kc: e.matmul(ps_[1 + cb][:, :], lhsT=lT[:, 3 + kc, :], rhs=wukv_sb[:, kc, cb * 512:(cb + 1) * 512],
                                                                start=(kc == 0), stop=(kc == 1)),
                             R=[lT.b, wukv_sb.b], W=[ps_[1 + cb].b], inc=(kc == 1))
                    S.op("act", lambda e, cb=cb: e.activation(out=kv[:].rearrange("p a b -> p (a b)")[:, cb * 512:(cb + 1) * 512], in_=ps_[1 + cb][:, :], func=AF.Copy),
                         R=[ps_[1 + cb].b], W=[kv.b])
                rstd_of(ph, qm[:], qm.b, 8, 96, sqj, ssq, rstd)
                S.op("dve", lambda e: e.tensor_tensor(out=qn[:], in0=qm[:], in1=rstd[:, 0:8].unsqueeze(2).to_broadcast([128, 8, 96]), op=ALU.mult),
                     R=[qm.b, rstd.b], W=[qn.b])
                S.op("dve", lambda e: e.tensor_tensor(out=qn[:], in0=qn[:], in1=g_mq[:].unsqueeze(1).to_broadcast([128, 8, 96]), op=ALU.mult),
                     R=[qn.b, g_mq.b], W=[qn.b])
                S.op("dve", lambda e: e.tensor_copy(qo[:, :, 0:64], qn[:, :, 0:64]), R=[qn.b], W=[qo.b])
                rope(qn[:, :, 64:96].rearrange("p h (a b q) -> p h a b q", a=2, b=2), qn.b, cm, cm.b, 8, 8, t1, t2,
                     qo[:, :, 64:96].rearrange("p h (a b q) -> p h a b q", a=2, b=2), qo.b)
                S.op("dve", lambda e: e.tensor_copy(kf[:, :, 0:64], kv[:, :, 0:64]), R=[kv.b], W=[kf.b])
                S.op("dve", lambda e: e.tensor_copy(kf[:, :, 64:96], p_sb[:, 1152:1184].unsqueeze(1).to_broadcast([128, 8, 32])), R=[p_sb.b], W=[kf.b])
                rstd_of(ph, kf[:], kf.b, 8, 96, sqj, ssq, rstd)
                S.op("dve", lambda e: e.tensor_tensor(out=kn[:], in0=kf[:], in1=rstd[:, 0:8].unsqueeze(2).to_broadcast([128, 8, 96]), op=ALU.mult),
                     R=[kf.b, rstd.b], W=[kn.b])
                S.op("dve", lambda e: e.tensor_tensor(out=kn[:], in0=kn[:], in1=g_mk[:].unsqueeze(1).to_broadcast([128, 8, 96]), op=ALU.mult),
                     R=[kn.b, g_mk.b], W=[kn.b])
                S.op("dve", lambda e: e.tensor_copy(ko[:, :, 0:64], kn[:, :, 0:64]), R=[kn.b], W=[ko.b])
                rope(kn[:, :, 64:96].rearrange("p h (a b q) -> p h a b q", a=2, b=2), kn.b, cm, cm.b, 8, 8, t1, t2,
                     ko[:, :, 64:96].rearrange("p h (a b q) -> p h a b q", a=2, b=2), ko.b)
                vmb = Vm_sb[i % 2]
                S.op("act", lambda e, vmb=vmb: e.activation(out=vmb[:, :, 0:64], in_=kv[:, :, 64:128], func=AF.Copy), R=[kv.b], W=[vmb.b])
                S.dma(Vm_s[tsl, :], vmb[:].rearrange("p a b -> p (a b)"), R=[vmb.b])
                sq3 = p_sb[:, 384:896].rearrange("p (h d) -> p h d", h=8)
                rstd_of(ph, sq3, p_sb.b, 8, 64, sqj, ssq, rstd)
                S.op("dve", lambda e: e.tensor_tensor(out=sqn[:], in0=sq3, in1=rstd[:, 0:8].unsqueeze(2).to_broadcast([128, 8, 64]), op=ALU.mult),
                     R=[p_sb.b, rstd.b], W=[sqn.b])
                S.op("dve", lambda e: e.tensor_tensor(out=sqn[:], in0=sqn[:], in1=g_sq[:].unsqueeze(1).to_broadcast([128, 8, 64]), op=ALU.mult),
                     R=[sqn.b, g_sq.b], W=[sqn.b])
                rope(sqn[:].rearrange("p h (a b q) -> p h a b q", a=2, b=2), sqn.b, cs_, cs_.b, 8, 16, t1, t2,
                     sqo[:].rearrange("p h (a b q) -> p h a b q", a=2, b=2), sqo.b)
                sk3 = p_sb[:, 1184:1312].rearrange("p (h d) -> p h d", h=2)
                rstd_of(ph, sk3, p_sb.b, 2, 64, sqj, ssq, rstd)
                S.op("dve", lambda e: e.tensor_tensor(out=skn[:], in0=sk3, in1=rstd[:, 0:2].unsqueeze(2).to_broadcast([128, 2, 64]), op=ALU.mult),
                     R=[p_sb.b, rstd.b], W=[skn.b])
                S.op("dve", lambda e: e.tensor_tensor(out=skn[:], in0=skn[:], in1=g_sk[:].unsqueeze(1).to_broadcast([128, 2, 64]), op=ALU.mult),
                     R=[skn.b, g_sk.b], W=[skn.b])
                rope(skn[:].rearrange("p h (a b q) -> p h a b q", a=2, b=2), skn.b, cs_, cs_.b, 2, 16, t1, t2,
                     sko[:].rearrange("p h (a b q) -> p h a b q", a=2, b=2), sko.b)
                vsb = Vs_sb[i % 2]
                S.op("act", lambda e, vsb=vsb: e.activation(out=vsb[:, :, 0:64], in_=p_sb[:, 1312:1440].rearrange("p (h d) -> p h d", h=2), func=AF.Copy),
                     R=[p_sb.b], W=[vsb.b])
                S.dma(Vs_s[tsl, :], vsb[:].rearrange("p a b -> p (a b)"), R=[vsb.b])
                for (src, dstT, nh, dh, bank, scr) in ((qo, qT_sb, 8, 96, 7, qTm_s), (ko, kT_sb, 8, 96, 0, kTm_s),
                                                       (sqo, sqT_sb, 8, 64, 4, qTs_s), (sko, skT_sb, 2, 64, 3, kTs_s)):
                    pvv = psb_(bank)
                    for h in range(nh):
                        S.op("pe", lambda e, src=src, h=h, dh=dh, pvv=pvv: e.transpose(pvv[0:dh, h * 128:(h + 1) * 128], src[:, h, :], ident_b[:]),
                             R=[src.b, ident_b.b], W=[ps_[bank].b])
                    S.op("act", lambda e, dstT=dstT, nh=nh, dh=dh, pvv=pvv: e.activation(out=dstT[0:dh, 0:nh, :].rearrange("p a b -> p (a b)"),
                                                                                       in_=pvv[0:dh, 0:nh * 128], func=AF.Copy),
                         R=[ps_[bank].b], W=[dstT.b])
                    S.dma(scr[:, :, tsl].rearrange("h d t -> d h t"), dstT[0:dh, 0:nh, :], R=[dstT.b])
                if i + 2 < NT:
                    loads(i + 2)

            loads(0)
            loads(1)
            for pr in range(NT // 2):
                bg_issue(2)
                recs = []
                for par in range(2):
                    S.rec = []
                    tile_body(2 * pr + par, par)
                    recs.append(S.rec)
                    S.rec = None
                for n_ in range(max(len(recs[0]), len(recs[1]))):
                    for par in range(2):
                        if n_ < len(recs[par]):
                            recs[par][n_]()
            S.barrier()
        chk(l, "B")

        with contextlib.ExitStack() as ph:
            kT_all = [sb("kTa%d" % h, [128, T], BF16, ph) for h in range(8)]
            Vall = sb("Vall", [128, NT, 520], BF16, ph)
            kTs_all = sb("kTs_all", [64, 2, T], BF16, ph)
            Vs_all = sb("Vs_all", [128, NT, 130], BF16, ph)
            qTg = [sb("qTg%d" % i, [128, 8, 512], BF16, ph) for i in range(2)]
            sqTg = [sb("sqTg%d" % i, [64, 8, 512], BF16, ph) for i in range(2)]
            PT = [sb("PT%d" % i, [128, 512], BF16, ph) for i in range(4)]
            obuf = [sb("obuf0", [128, 4, D], BF16, ph)]
            rden = sb("rden", [128, 4], F32, ph)
            for h in range(8):
                S.dma(kT_all[h][0:96, :], kTm_s[h], W=[kT_all[h].b])
            S.dma(Vall[:], Vm_s.rearrange("(k p) e -> p k e", p=128), W=[Vall.b])
            S.dma(kTs_all[:], kTs_s.rearrange("g d t -> d g t"), W=[kTs_all.b])
            S.dma(Vs_all[:], Vs_s.rearrange("(k p) e -> p k e", p=128), W=[Vs_all.b])
            groups = ([] if last else [(0, 256)]) + [(256 + 512 * g, 512) for g in range(8)]
            ctr = {"s": 0, "p": 0, "o": 0}

            def qloads(gi):
                t0, nq = groups[gi]
                S.dma(qTg[gi % 2][0:96, :, 0:nq], qTm_s[:, :, t0:t0 + nq].rearrange("h d t -> d h t"), W=[qTg[gi % 2].b])
                S.dma(sqTg[gi % 2][:, :, 0:nq], qTs_s[:, :, t0:t0 + nq].rearrange("h d t -> d h t"), W=[sqTg[gi % 2].b])

            qloads(0)
            for gi, (t0, nq) in enumerate(groups):
                bg_issue(4)
                if gi + 1 < len(groups):
                    qloads(gi + 1)
                isctx = t0 < 256
                nblk = nq // 128
                qg = qTg[gi % 2]
                sg = sqTg[gi % 2]
                ob = obuf[0]
                kts = [0, 1] if isctx else list(range(NT))
                its = []
                for h in range(8):
                    for ki, kt in enumerate(kts):
                        its.append(("m", h, ki, kt, len(kts), None, None))
                for blk in range(nblk):
                    ti = t0 // 128 + blk
                    if isctx:
                        kl = [0, 1]
                    else:
                        kl = [0, 1] + ([ti - 1] if ti - 1 >= 2 else []) + [ti] + ([ti + 1] if ti + 1 < NT else [])
                    for g2 in range(2):
                        for ki, kt in enumerate(kl):
                            its.append(("s", g2, ki, kt, len(kl), blk, ti))

                def emitS(n):
                    kind, a, ki, kt, nk, blk, ti = its[n]
                    pS = ps[n % 4]
                    if kind == "m":
                        S.op("pe", lambda e: e.matmul(pS[:, 0:nq], lhsT=kT_all[a][0:96, kt * 128:(kt + 1) * 128], rhs=qg[0:96, a, 0:nq], start=True, stop=True),
                             R=[kT_all[a].b, qg.b], W=[pS.b])
                    else:
                        S.op("pe", lambda e: e.matmul(pS[:, :].rearrange("p (a b) -> p a b", a=4), lhsT=kTs_all[:, a, kt * 128:(kt + 1) * 128],
                                                      rhs=sg[:, 4 * a:4 * a + 4, blk * 128:(blk + 1) * 128], start=True, stop=True),
                             R=[kTs_all.b, sg.b], W=[pS.b])

                po_of = {}

                def emitRest(n):
                    kind, a, ki, kt, nk, blk, ti = its[n]
                    pS = ps[n % 4]
                    pt = PT[n % 4]
                    if ki == 0:
                        po_of["cur"] = ps[4 + ctr["o"] % 2]
                        ctr["o"] += 1
                    po = po_of["cur"]
                    if kind == "m":
                        S.op("act", lambda e: e.activation(out=pt[:, 0:nq], in_=pS[:, 0:nq], func=AF.Exp, scale=96.0 ** -0.5), R=[pS.b], W=[pt.b])
                        for qs in range(nblk):
                            S.op("pe", lambda e, qs=qs: e.matmul(po[:, qs * 65:(qs + 1) * 65], lhsT=pt[:, qs * 128:(qs + 1) * 128], rhs=Vall[:, kt, a * 65:(a + 1) * 65],
                                                                start=(ki == 0 and qs == 0), stop=(ki == nk - 1), skip_group_check=True),
                                 R=[pt.b, Vall.b], W=[po.b], inc=(qs == nblk - 1))
                        if ki == nk - 1:
                            po3 = po[:, 0:nblk * 65].rearrange("p (a b) -> p a b", b=65)
                            S.op("dve", lambda e: e.reciprocal(out=rden[:, 0:nblk], in_=po3[:, :, 64]), R=[po.b], W=[rden.b])
                            S.op("dve", lambda e: e.tensor_tensor(out=ob[:, 0:nblk, a * 64:(a + 1) * 64], in0=po3[:, :, 0:64],
                                                                  in1=rden[:, 0:nblk].unsqueeze(2).to_broadcast([128, nblk, 64]), op=ALU.mult),
                                 R=[po.b, rden.b], W=[ob.b])
                    else:
                        S.op("act", lambda e: e.activation(out=pt[:, :], in_=pS[:, :], func=AF.Exp, scale=0.125), R=[pS.b], W=[pt.b])
                        if (not isctx) and kt >= 2 and kt != ti:
                            mi = 0 if kt == ti - 1 else 1
                            S.op("pool", lambda e: e.tensor_tensor(out=pt[:, :].rearrange("p (a b) -> p a b", a=4), in0=pt[:, :].rearrange("p (a b) -> p a b", a=4),
                                                                   in1=tri_b[:, mi, :].unsqueeze(1).to_broadcast([128, 4, 128]), op=ALU.mult),
                                 R=[pt.b, tri_b.b], W=[pt.b])
                        for r in range(4):
                            S.op("pe", lambda e, r=r: e.matmul(po[:, r * 65:(r + 1) * 65], lhsT=pt[:, r * 128:(r + 1) * 128], rhs=Vs_all[:, kt, a * 65:(a + 1) * 65],
                                                              start=(ki == 0 and r == 0), stop=(ki == nk - 1), skip_group_check=True),
                                 R=[pt.b, Vs_all.b], W=[po.b], inc=(r == 3))
                        if ki == nk - 1:
                            po3 = po[:, 0:260].rearrange("p (a b) -> p a b", b=65)
                            S.op("dve", lambda e: e.tensor_tensor(out=rden[:, 0:4], in0=po3[:, :, 64], in1=esink[:, 4 * a:4 * a + 4], op=ALU.add),
                                 R=[po.b, esink.b], W=[rden.b])
                            S.op("dve", lambda e: e.reciprocal(out=rden[:, 0:4], in_=rden[:, 0:4]), R=[rden.b], W=[rden.b])
                            S.op("dve", lambda e: e.tensor_tensor(out=ob[:, blk, 512 + a * 256:512 + (a + 1) * 256].rearrange("p (a b) -> p a b", a=4), in0=po3[:, :, 0:64],
                                                                  in1=rden[:, 0:4].unsqueeze(2).to_broadcast([128, 4, 64]), op=ALU.mult),
                                 R=[po.b, rden.b], W=[ob.b])

                LA = 3
                for n in range(min(LA, len(its))):
                    emitS(n)
                for n in range(len(its)):
                    if n + LA < len(its):
                        emitS(n + LA)
                    emitRest(n)
                S.dma(om_s[t0:t0 + nq, :].rearrange("(b p) c -> p b c", p=128), ob[:, 0:nblk, :], R=[ob.b])
            S.barrier()
        chk(l, "C")

        with contextlib.ExitStack() as ph:
            stg = [sb("stgd%d" % i, [128, 2048], F32, ph) for i in range(2)]
            w_out_sb = sb("w_out_sb", [128, 8, D], BF16, ph)
            for kc in range(8):
                load_w_bf16(ph, (w_out_sb[:, kc, :], w_out_sb.b), w_out[l, kc * 128:(kc + 1) * 128, :], D, stg)
            gb = [sb("gbd%d" % j, [128, D], F32, ph) for j in range(2)]
            gate_bcast(gb[0], 0, 0)
            gate_bcast(gb[1], 0, 1)
            htd = [sb("htd%d" % i, [128, D], F32, ph) for i in range(2)]
            omb = [sb("omb%d" % i, [128, D], BF16, ph) for i in range(2)]
            oT = sb("oT", [128, 8, 128], BF16, ph)
            h1 = [sb("h1d%d" % i, [128, D], F32, ph) for i in range(2)]
            tiles = list(range(2 if last else 0, NT))

            def loadsD(i):
                if l == 0:
                    src = ctx_d[i * 128:(i + 1) * 128, :] if i < 2 else x_d[(i - 2) * 128:(i - 1) * 128, :]
                else:
                    src = h_s[i * 128:(i + 1) * 128, :]
                S.dma(htd[i % 2][:], src, W=[htd[i % 2].b])
                S.dma(omb[i % 2][:], om_s[i * 128:(i + 1) * 128, :], W=[omb[i % 2].b])

            loadsD(tiles[0])
            for n_, i in enumerate(tiles):
                if n_ + 1 < len(tiles):
                    loadsD(tiles[n_ + 1])
                j = 1 if i < 2 else 0
                pv = psb(0)
                for kc in range(8):
                    S.op("pe", lambda e, kc=kc, i=i: e.transpose(pv[:, kc * 128:(kc + 1) * 128], omb[i % 2][:, kc * 128:(kc + 1) * 128], ident_b[:]),
                         R=[omb[i % 2].b, ident_b.b], W=[ps[0].b])
                S.op("act", lambda e: e.activation(out=oT[:].rearrange("p a b -> p (a b)"), in_=pv[:, :], func=AF.Copy), R=[ps[0].b], W=[oT.b])
                hh = h1[i % 2]
                for hf in range(2):
                    for kc in range(8):
                        S.op("pe", lambda e, hf=hf, kc=kc: e.matmul(ps[1 + hf][:, :], lhsT=oT[:, kc, :], rhs=w_out_sb[:, kc, hf * 512:(hf + 1) * 512],
                                                                start=(kc == 0), stop=(kc == 7)),
                             R=[oT.b, w_out_sb.b], W=[ps[1 + hf].b], inc=(kc == 7))
                    S.op("dve", lambda e, hf=hf, j=j, hh=hh: e.tensor_tensor(out=hh[:, hf * 512:(hf + 1) * 512], in0=ps[1 + hf][:, :],
                                                                          in1=gb[j][:, hf * 512:(hf + 1) * 512], op=ALU.mult),
                         R=[ps[1 + hf].b, gb[j].b], W=[hh.b])
                S.op("pool", lambda e, hh=hh, i=i: e.tensor_tensor(out=hh[:], in0=hh[:], in1=htd[i % 2][:], op=ALU.add), R=[hh.b, htd[i % 2].b], W=[hh.b])
                S.dma(h_s[i * 128:(i + 1) * 128, :], hh[:], R=[hh.b])
            bg_issue(len(bg_list))
            S.barrier(bg=True)
        chk(l, "D")

        with contextlib.ExitStack() as ph:
            s_sb = sb("s_sb", [128, 2048], F32, ph)
            cand = sb("cand", [128, 2048], F32, ph)
            stg = [s_sb, cand]
            wq_sb = sb("wq_sb", [128, 8, 2048], BF16, ph)
            keysT_sb = sb("keysT_sb", [128, 2048], BF16, ph)
            for dc in range(8):
                load_w_bf16(ph, (wq_sb[:, dc, :], wq_sb.b), wq_d[l, dc * 128:(dc + 1) * 128, :], 2048, stg)
            load_w_bf16(ph, (keysT_sb[:, :], keysT_sb.b), keysT_d[l], 2048, stg)
            gbe = sb("gbe", [128, D], F32, ph)
            gate_bcast(gbe, 1, 0 if last else 1)
            WtS = sb("WtS", [128, 256, 128], BF16, ph)
            Ab = [sb("Ab%d" % i, [128, 8, 128], BF16, ph) for i in range(2)]
            Bb = [sb("Bb%d" % i, [128, 8, 128], BF16, ph) for i in range(2)]
            utc = [sb("utc%d" % i, [128, 8, 128], BF16, ph) for i in range(6)]
            vc = [sb("vc%d" % i, [128, D], BF16, ph) for i in range(4)]
            h1e = [[sb("h1e%d_%d" % (p_, i), [128, D], F32, ph) for i in range(2)] for p_ in range(2)]
            xn = sb("xne", [128, D], BF16, ph)
            bT = [sb("bT%d" % p_, [128, 8, 256], BF16, ph) for p_ in range(2)]
            qTe = sb("qTe", [128, 16, 256], BF16, ph)
            sv = sb("sv", [128, 16, 16], F32, ph)
            si_u = sb("si_u", [128, 16, 16], U32, ph)
            si_f = sb("si_f", [128, 16, 16], F32, ph)
            cv = sb("cv", [128, 8, 16], F32, ph)
            ci_u = sb("ci_u", [128, 8, 16], U32, ph)
            k_u = sb("k_u", [128, 2, 128], U32, ph)
            k_f = sb("k_f", [128, 2, 128], F32, ph)
            IG = sb("IG", [128, 3, 128], F32, ph)
            IGT = [sb("IGT%d" % p_, [128, 3, 256], BF16, ph) for p_ in range(2)]
            gsum = sb("gsum", [128, 8], F32, ph)
            ssq = sb("ssqe", [128, 16], F32, ph)
            rstd = sb("rstde", [128, 16], F32, ph)
            ga = [sb("ga%d" % i, [128, 256], BF16, ph) for i in range(2)]
            GT = [sb("GT%d" % i, [128, 256], BF16, ph) for i in range(3)]
            iota16 = iota_f[:, 0:16]
            S.barrier()
            s_b = [Buf() for _ in range(16)]
            c_b = [Buf() for _ in range(16)]
            sv_b = [Buf() for _ in range(16)]
            siu_b = [Buf() for _ in range(16)]
            cv_b = [Buf() for _ in range(8)]
            ciu_b = [Buf() for _ in range(8)]
            s3 = s_sb[:].rearrange("p (a b) -> p a b", a=16)
            c3s = cand[:].rearrange("p (a b) -> p a b", a=16)
            cg3 = cand[:].rearrange("p (h a) -> p h a", h=8)
            sg3 = s_sb[:].rearrange("p (h a) -> p h a", h=8)

            def front(g, par):
                j = 1 if g == 0 else 0
                bTp = bT[par]
                for tt in range(2):
                    i = g * 2 + tt
                    S.dma(h1e[par][tt][:], h_s[i * 128:(i + 1) * 128, :], W=[h1e[par][tt].b])
                    modulate_T(h1e[par][tt], xn, xn, ssq, rstd, bTp[:, :, tt * 128:(tt + 1) * 128], bTp.b, 2, j, 7)
                    yield
                for hp in range(16):
                    pq = ps[7]
                    for dc in range(8):
                        S.op("pe", lambda e, dc=dc: e.matmul(pq[:, 0:256], lhsT=wq_sb[:, dc, hp * 128:(hp + 1) * 128], rhs=bTp[:, dc, :],
                                                            start=(dc == 0), stop=(dc == 7)),
                             R=[wq_sb.b, bTp.b], W=[pq.b], inc=(dc == 7))
                    if hp % 2 == 0:
                        S.op("act", lambda e: e.activation(out=qTe[:, hp, :], in_=pq[:, 0:256], func=AF.Copy), R=[pq.b], W=[qTe.b])
                    else:
                        S.op("dve", lambda e: e.tensor_copy(qTe[:, hp, :], pq[:, 0:256]), R=[pq.b], W=[qTe.b])
                    yield
                for tt in range(2):
                    for qd in range(4):
                        pb = ps[7]
                        for k4 in range(4):
                            hp = qd * 4 + k4
                            S.op("pe", lambda e, hp=hp, k4=k4: e.matmul(pb[:, k4 * 128:(k4 + 1) * 128], lhsT=qTe[:, hp, tt * 128:(tt + 1) * 128],
                                                                      rhs=keysT_sb[:, hp * 128:(hp + 1) * 128], start=True, stop=True),
                                 R=[qTe.b, keysT_sb.b], W=[pb.b], inc=(k4 == 3))
                        S.op("act", lambda e: e.activation(out=s_sb[:, qd * 512:(qd + 1) * 512], in_=pb[:, :], func=AF.Copy), R=[pb.b], W=s_b[qd * 4:qd * 4 + 4])
                        yield
                    for hp in range(16):
                        S.op("dve", lambda e, hp=hp: e.max(out=sv[:, hp, 0:8], in_=s3[:, hp, :]), R=[s_b[hp]], W=[sv_b[hp]])
                        if hp % 2 == 1:
                            yield
                    for hp in range(16):
                        S.op("dve", lambda e, hp=hp: e.max_index(out=si_u[:, hp, 0:8], in_max=sv[:, hp, 0:8], in_values=s3[:, hp, :]), R=[s_b[hp], sv_b[hp]], W=[siu_b[hp]])
                        if hp % 2 == 1:
                            yield
                    for hp in range(16):
                        S.op("dve", lambda e, hp=hp: e.match_replace(out=c3s[:, hp, :], in_to_replace=sv[:, hp, 0:8], in_values=s3[:, hp, :], imm_value=NEG),
                             R=[s_b[hp], sv_b[hp]], W=[c_b[hp]])
                        if hp % 2 == 1:
                            yield
                    for hp in range(16):
                        S.op("dve", lambda e, hp=hp: e.max(out=sv[:, hp, 8:16], in_=c3s[:, hp, :]), R=[c_b[hp]], W=[sv_b[hp]])
                        if hp % 2 == 1:
                            yield
                    for hp in range(16):
                        S.op("dve", lambda e, hp=hp: e.max_index(out=si_u[:, hp, 8:16], in_max=sv[:, hp, 8:16], in_values=c3s[:, hp, :]), R=[c_b[hp], sv_b[hp]], W=[siu_b[hp]])
                        if hp % 2 == 1:
                            yield
                    S.op("dve", lambda e: e.tensor_copy(si_f[:], si_u[:]), R=siu_b, W=[si_f.b])
                    sv4 = sv[:].rearrange("p (h a) k -> p h a k", a=2)
                    si4 = si_f[:].rearrange("p (h a) k -> p h a k", a=2)
                    c4 = cand[:].rearrange("p (h a b) -> p h a b", h=8, a=16)
                    S.op("dve", lambda e: e.tensor_tensor(out=c4, in0=sv4[:, :, 0, :].unsqueeze(3).to_broadcast([128, 8, 16, 16]),
                                                          in1=sv4[:, :, 1, :].unsqueeze(2).to_broadcast([128, 8, 16, 16]), op=ALU.add),
                         R=sv_b, W=c_b)
                    yield
                    hb = lambda lst, h: [lst[2 * h], lst[2 * h + 1]]
                    for h in range(8):
                        S.op("dve", lambda e, h=h: e.max(out=cv[:, h, 0:8], in_=cg3[:, h, :]), R=hb(c_b, h), W=[cv_b[h]])
                        if h % 2 == 1:
                            yield
                    for h in range(8):
                        S.op("dve", lambda e, h=h: e.max_index(out=ci_u[:, h, 0:8], in_max=cv[:, h, 0:8], in_values=cg3[:, h, :]), R=hb(c_b, h) + [cv_b[h]], W=[ciu_b[h]])
                        if h % 2 == 1:
                            yield
                    for h in range(8):
                        S.op("dve", lambda e, h=h: e.match_replace(out=sg3[:, h, :], in_to_replace=cv[:, h, 0:8], in_values=cg3[:, h, :], imm_value=NEG),
                             R=hb(c_b, h) + [cv_b[h]], W=hb(s_b, h))
                        if h % 2 == 1:
                            yield
                    for h in range(8):
                        S.op("dve", lambda e, h=h: e.max(out=cv[:, h, 8:16], in_=sg3[:, h, :]), R=hb(s_b, h), W=[cv_b[h]])
                        if h % 2 == 1:
                            yield
                    for h in range(8):
                        S.op("dve", lambda e, h=h: e.max_index(out=ci_u[:, h, 8:16], in_max=cv[:, h, 8:16], in_values=sg3[:, h, :]), R=hb(s_b, h) + [cv_b[h]], W=[ciu_b[h]])
                        if h % 2 == 1:
                            yield
                    ciu2 = ci_u[:].rearrange("p a b -> p (a b)")
                    S.op("dve", lambda e: e.tensor_scalar(out=k_u[:, 0, :], in0=ciu2, scalar1=4, scalar2=None, op0=ALU.logical_shift_right), R=ciu_b, W=[k_u.b])
                    S.op("dve", lambda e: e.tensor_scalar(out=k_u[:, 1, :], in0=ciu2, scalar1=15, scalar2=None, op0=ALU.bitwise_and), R=ciu_b, W=[k_u.b])
                    S.op("dve", lambda e: e.tensor_copy(k_f[:], k_u[:]), R=[k_u.b], W=[k_f.b])
                    yield
                    for a in range(2):
                        kk = k_f[:, a, :].rearrange("p (h k) -> p h k", h=8)
                        e4 = s_sb[:].rearrange("p (h a b) -> p h a b", h=8, a=16)
                        S.op("dve", lambda e: e.tensor_tensor(out=e4, in0=kk.unsqueeze(3).to_broadcast([128, 8, 16, 16]),
                                                              in1=iota16.unsqueeze(1).unsqueeze(1).to_broadcast([128, 8, 16, 16]), op=ALU.is_equal),
                             R=[k_f.b, iota_f.b], W=s_b)
                        yield
                        S.op("dve", lambda e: e.tensor_tensor(out=e4, in0=e4, in1=si4[:, :, a, :].unsqueeze(2).to_broadcast([128, 8, 16, 16]), op=ALU.mult),
                             R=s_b + [si_f.b], W=s_b)
                        yield
                        S.op("dve", lambda e: e.tensor_reduce(out=IG[:, a, :], in_=s_sb[:].rearrange("p (a b) -> p a b", b=16), axis=AX.X, op=ALU.add),
                             R=s_b, W=[IG.b])
                        yield
                    g3 = IG[:, 2, :].rearrange("p (h k) -> p h k", h=8)
                    S.op("dve", lambda e: e.tensor_tensor(out=g3, in0=cv[:], in1=cv[:, :, 0:1].to_broadcast([128, 8, 16]), op=ALU.subtract), R=cv_b, W=[IG.b])
                    S.op("act", lambda e: e.activation(out=IG[:, 2, :], in_=IG[:, 2, :], func=AF.Exp), R=[IG.b], W=[IG.b])
                    S.op("dve", lambda e: e.tensor_reduce(out=gsum[:], in_=g3, axis=AX.X, op=ALU.add), R=[IG.b], W=[gsum.b])
                    S.op("dve", lambda e: e.reciprocal(out=gsum[:], in_=gsum[:]), R=[gsum.b], W=[gsum.b])
                    S.op("dve", lambda e: e.tensor_tensor(out=g3, in0=g3, in1=gsum[:].unsqueeze(2).to_broadcast([128, 8, 16]), op=ALU.mult), R=[IG.b, gsum.b], W=[IG.b])
                    yield
                    pbt = ps[7]
                    for a in range(3):
                        S.op("pe", lambda e, a=a: e.transpose(pbt[:, a * 128:(a + 1) * 128], IG[:, a, :], ident_f[:]), R=[IG.b, ident_f.b], W=[pbt.b])
                    S.op("act", lambda e: e.activation(out=IGT[par][:, :, tt * 128:(tt + 1) * 128], in_=pbt[:, 0:384].rearrange("p (a b) -> p a b", a=3), func=AF.Copy),
                         R=[pbt.b], W=[IGT[par].b])
                    yield

            def build_wt(par):
                IGTp = IGT[par]
                for tb in range(32):
                    A_ = Ab[tb % 2]
                    B_ = Bb[tb % 2]
                    ab_ = AB_b[tb % 2]
                    for t8 in range(8):
                        t = tb * 8 + t8
                        S.op("dve", lambda e, t8=t8, t=t: e.tensor_scalar(out=A_[:, t8, :], in0=iota_b[:], scalar1=IGTp[:, 0, t:t + 1], scalar2=IGTp[:, 2, t:t + 1],
                                                                        op0=ALU.is_equal, op1=ALU.mult),
                             R=[IGTp.b, iota_b.b], W=[ab_[0][t8]])
                        S.op("dve", lambda e, t8=t8, t=t: e.tensor_scalar(out=B_[:, t8, :], in0=iota_b[:], scalar1=IGTp[:, 1, t:t + 1], scalar2=None, op0=ALU.is_equal),
                             R=[IGTp.b, iota_b.b], W=[ab_[1][t8]])
                    for q4 in range(2):
                        pw = ps[6 + (tb * 2 + q4) % 2]
                        for t4 in range(4):
                            t = q4 * 4 + t4
                            S.op("pe", lambda e, t=t, t4=t4: e.matmul(pw[:, t4 * 128:(t4 + 1) * 128], lhsT=B_[:, t, :], rhs=A_[:, t, :], start=True, stop=True),
                                 R=[ab_[0][t], ab_[1][t]], W=[pw.b], inc=(t4 == 3))
                        tk0 = tb * 8 + q4 * 4
                        S.op("act", lambda e: e.activation(out=WtS[:, tk0:tk0 + 4, :].rearrange("p a b -> p (a b)"), in_=pw[:, :], func=AF.Copy),
                             R=[pw.b], W=[WtS.b])

            AB_b = [[[Buf() for _ in range(8)] for _ in range(2)] for _ in range(2)]
            ngrp = T // 256
            glist = list(range(1 if last else 0, ngrp))
            for _ in front(glist[0], 0):
                pass
            for gi_, g in enumerate(glist):
                par = gi_ % 2
                j = 1 if g == 0 else 0
                bTp = bT[par]
                build_wt(par)
                nxt = front(glist[gi_ + 1], 1 - par) if gi_ + 1 < len(glist) else iter(())

                def uload(c):
                    S.dma(utc[c % 6][:].rearrange("p a b -> p (a b)"), utb_s[l, c], W=[utc[c % 6].b])

                def vload(c):
                    S.dma(vc[c % 4][:], vb_s[l, c * 128:(c + 1) * 128, :], W=[vc[c % 4].b])

                def emitA(c):
                    u_ = utc[c % 6]
                    pa = ps[4 + c % 3]
                    for dc in range(8):
                        S.op("pe", lambda e, dc=dc: e.matmul(pa[:, 0:256], lhsT=u_[:, dc, :], rhs=bTp[:, dc, :], start=(dc == 0), stop=(dc == 7)),
                             R=[u_.b, bTp.b], W=[pa.b], inc=(dc == 7))

                for c in range(5):
                    uload(c)
                for c in range(3):
                    vload(c)
                def emitMid(c):
                    pa = ps[4 + c % 3]
                    S.op("act", lambda e: e.activation(out=ga[c % 2][:], in_=pa[:, 0:256], func=AF.Gelu), R=[pa.b], W=[ga[c % 2].b])
                    S.op("pool", lambda e: e.tensor_tensor(out=GT[c % 3][:], in0=ga[c % 2][:], in1=WtS[:, :, c], op=ALU.mult), R=[ga[c % 2].b, WtS.b], W=[GT[c % 3].b])

                emitA(0)
                emitA(1)
                emitMid(0)
                for c in range(128):
                    if c + 5 < 128:
                        uload(c + 5)
                    if c + 3 < 128:
                        vload(c + 3)
                    if c + 2 < 128:
                        emitA(c + 2)
                    if c + 1 < 128:
                        emitMid(c + 1)
                    v_ = vc[c % 4]
                    for tt in range(2):
                        for hf in range(2):
                            S.op("pe", lambda e, tt=tt, hf=hf: e.matmul(ps[tt * 2 + hf][:, :], lhsT=GT[c % 3][:, tt * 128:(tt + 1) * 128], rhs=v_[:, hf * 512:(hf + 1) * 512],
                                                                      start=(c == 0), stop=(c == 127), skip_group_check=True),
                                 R=[GT[c % 3].b, v_.b], W=[ps[tt * 2 + hf].b], inc=(tt == 1 and hf == 1))
                    if c >= 1:
                        next(nxt, None)
                        if c % 2 == 0 or c % 8 == 1:
                            next(nxt, None)
                for _ in nxt:
                    pass
                for tt in range(2):
                    i = g * 2 + tt
                    y_ = h1e[par][tt]
                    for hf in range(2):
                        pb_ = ps[tt * 2 + hf]
                        S.op("dve", lambda e, hf=hf, pb_=pb_: e.tensor_tensor(out=pb_[:, :], in0=pb_[:, :], in1=gbe[:, hf * 512:(hf + 1) * 512], op=ALU.mult),
                             R=[pb_.b, gbe.b], W=[pb_.b])
                        S.op("dve", lambda e, hf=hf, pb_=pb_: e.tensor_tensor(out=y_[:, hf * 512:(hf + 1) * 512], in0=pb_[:, :], in1=y_[:, hf * 512:(hf + 1) * 512], op=ALU.add),
                             R=[pb_.b, y_.b], W=[y_.b])
                    if last:
                        S.dma(y_d[(i - 2) * 128:(i - 1) * 128, :], y_[:], R=[y_.b])
                    else:
                        S.dma(h_s[i * 128:(i + 1) * 128, :], y_[:], R=[y_.b])
                if g == 0:
                    gate_bcast(gbe, 1, 0)
            S.barrier()
        chk(l, "E")

    S.barrier()
    es.close()
    return nc


def _consts():
    ident = np.eye(128, dtype=np.float32)
    iota = np.tile(np.arange(128, dtype=np.float32)[None, :], (128, 1))
    jj = np.arange(128)[:, None]
    ii = np.arange(128)[None, :]
    tri = np.stack([(ii <= jj), (jj <= ii)]).astype(np.float32)
    sel = np.zeros((2, 256), np.float32)
    sel[0, 0:128] = 1.0
    sel[1, 128:256] = 1.0

    def tables(rot):
        qd = rot // 4
        inv = (np.float32(10000.0) ** (-np.arange(qd, dtype=np.float32) / np.float32(qd))).astype(np.float32)
        t = np.arange(SEQ)
        rows = (t // 64).astype(np.float32)
        cols = (t % 64).astype(np.float32)
        ar = rows[:, None] * inv
        ac = cols[:, None] * inv
        ang = np.concatenate([ar, ar, ac, ac], -1).astype(np.float32)
        cos = np.cos(ang).astype(np.float32)
        sin = np.sin(ang).astype(np.float32)
        sgn = np.concatenate([-np.ones(qd), np.ones(qd), -np.ones(qd), np.ones(qd)]).astype(np.float32)
        cs = np.zeros((T, 2, rot), np.float32)
        cs[:CTX, 0, :] = 1.0
        cs[CTX:, 0, :] = cos
        cs[CTX:, 1, :] = sin * sgn
        return cs.reshape(T, 2 * rot)

    return dict(ident=ident, iota=iota, tri=tri, sel=sel, cs_mla=tables(32), cs_swa=tables(64))


def _in_maps(inp):
    f = lambda a: np.ascontiguousarray(np.asarray(a, dtype=np.float32))
    shared = dict(_consts())
    for k in ("ada_w", "ada_b", "norm1_g", "norm2_g", "w_in", "mla_wuq", "mla_wukv", "mla_qn_g", "mla_kn_g",
              "swa_qn_g", "swa_kn_g", "swa_sink", "w_out", "peer_wq", "peer_v"):
        shared[k] = f(inp[k])
    shared["qa_g_t"] = f(np.asarray(inp["mla_qa_g"]).reshape(L, 3, 128).transpose(0, 2, 1))
    shared["kva_g_t"] = f(np.asarray(inp["mla_kva_g"]).reshape(L, 2, 128).transpose(0, 2, 1))
    shared["keysT"] = f(np.asarray(inp["peer_keys"]).transpose(0, 4, 1, 2, 3).reshape(L, 128, 2048))
    u = np.asarray(inp["peer_u"], dtype=np.float32).reshape(L, 128, 128, 8, 128)
    shared["peer_uT"] = np.ascontiguousarray(u.transpose(0, 1, 4, 3, 2)).reshape(L, 128, 128, 1024)
    x = np.asarray(inp["x"], dtype=np.float32)
    c = np.asarray(inp["c"], dtype=np.float32)
    ctx = np.asarray(inp["ctx"], dtype=np.float32)
    cc = np.asarray(inp["c_ctx"], dtype=np.float32)
    maps = []
    for b in range(8):
        m = dict(shared)
        m["x"] = np.ascontiguousarray(x[b])
        m["ctx"] = np.ascontiguousarray(ctx[b])
        m["cvec"] = np.ascontiguousarray(np.stack([c[b], cc]))
        maps.append(m)
    return maps


def kernel(**inputs):
    nc = build()
    res = run_bass_kernel_spmd(nc, _in_maps(inputs), core_ids=list(range(8)))
    return np.stack([np.asarray(r["y"], dtype=np.float32) for r in res.results], axis=0)
```
